# Optimizing a Trainium2 kernel written in Bass

```python
import jax, jax.numpy as jnp
from jax import lax
import numpy as np

D_MODEL = 1024
BATCH = 16
SEQ = 4096
DEPTH = 4

MIX_WIDTH = D_MODEL
DN_HEADS = 4
DN_HEAD_DIM = MIX_WIDTH // (2 * DN_HEADS)
DN_WIDTH = DN_HEADS * DN_HEAD_DIM
DN_CHUNK = 64
CONV_WIDTH = 4
GM_GROUPS = 4
GM_WIDTH = MIX_WIDTH - DN_WIDTH
GM_GROUP_DIM = GM_WIDTH // GM_GROUPS
GM_CHUNK = 128
OFF_QKV = 0
OFF_Z = 3 * DN_WIDTH
OFF_BETA = 4 * DN_WIDTH
OFF_ALPHA = OFF_BETA + DN_HEADS
OFF_GU = OFF_ALPHA + DN_HEADS
OFF_GV = OFF_GU + GM_WIDTH
D_IN = OFF_GV + GM_WIDTH
D_FF_DENSE = ((8 * D_MODEL // 3 + 127) // 128) * 128
N_EXPERTS = 8
TOP_K = 2
D_FF_EXPERT = 7 * D_MODEL // 2
N_DENSE = (DEPTH + 1) // 2
N_MOE = DEPTH // 2
N_MOD = 6
EPS = 1e-6

kernel_name = 'hybrid_deltanet_gmlp_moe_adaln'


def rms_norm(x, w):
    xf = x.astype(jnp.float32)
    y = xf * lax.rsqrt(jnp.mean(xf * xf, axis=-1, keepdims=True) + EPS)
    return (y * w.astype(jnp.float32)).astype(x.dtype)


def layer_norm(x, w, b):
    xf = x.astype(jnp.float32)
    mu = jnp.mean(xf, axis=-1, keepdims=True)
    var = jnp.mean(jnp.square(xf - mu), axis=-1, keepdims=True)
    y = (xf - mu) * lax.rsqrt(var + EPS)
    return (y * w.astype(jnp.float32) + b.astype(jnp.float32)).astype(x.dtype)


def l2_normalize(x):
    xf = x.astype(jnp.float32)
    return (xf * lax.rsqrt(jnp.sum(xf * xf, axis=-1, keepdims=True) + EPS)).astype(x.dtype)


def modulate(h, shift, scale):
    return h * (1.0 + scale[:, None, :]) + shift[:, None, :]


def causal_depthwise_conv(x, w):
    C = x.shape[-1]
    return lax.conv_general_dilated(
        x, w[:, None, :].astype(x.dtype), window_strides=(1,),
        padding=[(CONV_WIDTH - 1, 0)], dimension_numbers=('NWC', 'WIO', 'NWC'),
        feature_group_count=C)


def gated_delta_rule(q, k, v, g, beta):
    B, L, H, Dk = q.shape
    Dv = v.shape[-1]
    C = DN_CHUNK
    N = L // C
    f32 = jnp.float32

    def to_chunks(t):
        t = t.astype(f32).reshape(B, N, C, H, *t.shape[3:])
        return jnp.moveaxis(t, (1, 3), (0, 2))

    q, k, v = to_chunks(q), to_chunks(k), to_chunks(v)
    g, beta = to_chunks(g), to_chunks(beta)
    g_cum = jnp.cumsum(g, axis=-1)
    tri = jnp.tril(jnp.ones((C, C), dtype=bool))
    strict = jnp.tril(jnp.ones((C, C), dtype=bool), -1)
    diff = g_cum[..., :, None] - g_cum[..., None, :]
    decay = jnp.exp(jnp.where(tri, diff, -jnp.inf))
    k_beta = k * beta[..., None]
    v_beta = v * beta[..., None]
    a_mat = jnp.where(strict, jnp.einsum('nbhcd,nbhsd->nbhcs', k_beta, k) * decay, 0.0)
    eye = jnp.eye(C, dtype=f32)
    t_mat = lax.linalg.triangular_solve(a_mat + eye, jnp.broadcast_to(eye, a_mat.shape),
                                        left_side=True, lower=True)
    u = jnp.einsum('nbhcs,nbhse->nbhce', t_mat, v_beta)
    w = jnp.einsum('nbhcs,nbhsd->nbhcd', t_mat, k_beta * jnp.exp(g_cum)[..., None])
    attn = jnp.einsum('nbhcd,nbhsd->nbhcs', q, k) * decay
    q_dec = q * jnp.exp(g_cum)[..., None]
    g_last = g_cum[..., -1]
    k_dec = k * jnp.exp(g_last[..., None] - g_cum)[..., None]

    def step(S, xs):
        u_n, w_n, q_n, k_n, attn_n, gl_n = xs
        v_new = u_n - jnp.einsum('bhcd,bhde->bhce', w_n, S)
        o_n = (jnp.einsum('bhcd,bhde->bhce', q_n, S)
               + jnp.einsum('bhcs,bhse->bhce', attn_n, v_new))
        S = S * jnp.exp(gl_n)[..., None, None] + jnp.einsum('bhcd,bhce->bhde', k_n, v_new)
        return S, o_n

    S0 = jnp.zeros((B, H, Dk, Dv), f32)
    _, o = lax.scan(step, S0, (u, w, q_dec, k_dec, attn, g_last))
    return jnp.moveaxis(o, (0, 2), (1, 3)).reshape(B, L, H, Dv)


def hybrid_mixer(h, w_in, conv_w, a_log, dt_bias, dn_norm_w, gm_ln_w, gm_ln_b,
                 gm_spatial_w, gm_spatial_b, w_out):
    B, L, _ = h.shape
    proj = h @ w_in
    qkv = jax.nn.silu(causal_depthwise_conv(proj[..., OFF_QKV:OFF_Z], conv_w))
    q = qkv[..., 0:DN_WIDTH].reshape(B, L, DN_HEADS, DN_HEAD_DIM)
    k = qkv[..., DN_WIDTH:2 * DN_WIDTH].reshape(B, L, DN_HEADS, DN_HEAD_DIM)
    v = qkv[..., 2 * DN_WIDTH:3 * DN_WIDTH].reshape(B, L, DN_HEADS, DN_HEAD_DIM)
    z = proj[..., OFF_Z:OFF_BETA].reshape(B, L, DN_HEADS, DN_HEAD_DIM)
    beta = jax.nn.sigmoid(proj[..., OFF_BETA:OFF_ALPHA].astype(jnp.float32))
    alpha = proj[..., OFF_ALPHA:OFF_GU].astype(jnp.float32)
    g = -jnp.exp(a_log.astype(jnp.float32)) * jax.nn.softplus(alpha + dt_bias.astype(jnp.float32))
    q = l2_normalize(q) * (DN_HEAD_DIM ** -0.5)
    k = l2_normalize(k)
    o = gated_delta_rule(q, k, v, g, beta).astype(h.dtype)
    o_dn = (rms_norm(o, dn_norm_w) * jax.nn.silu(z)).reshape(B, L, DN_WIDTH)
    gu = jax.nn.gelu(proj[..., OFF_GU:OFF_GV])
    gv = layer_norm(jax.nn.gelu(proj[..., OFF_GV:D_IN]), gm_ln_w, gm_ln_b)
    n_chunks = L // GM_CHUNK
    gv = gv.reshape(B, n_chunks, GM_CHUNK, GM_GROUPS, GM_GROUP_DIM)
    causal = jnp.tril(jnp.ones((GM_CHUNK, GM_CHUNK), dtype=bool))
    w_s = jnp.where(causal, gm_spatial_w, 0.0).astype(h.dtype)
    s = jnp.einsum('gts,bnsgd->bntgd', w_s, gv) + gm_spatial_b.T[None, None, :, :, None]
    o_gm = (gu.reshape(B, n_chunks, GM_CHUNK, GM_GROUPS, GM_GROUP_DIM) * s).reshape(B, L, GM_WIDTH)
    return jnp.concatenate([o_dn, o_gm], axis=-1) @ w_out


def swiglu(h, wg, wu, wd):
    return (jax.nn.silu(h @ wg) * (h @ wu)) @ wd


def moe_swiglu(h, w_router, w_gate, w_up, w_down):
    logits = (h @ w_router).astype(jnp.float32)
    top_vals, top_idx = lax.top_k(logits, TOP_K)
    top_w = jax.nn.softmax(top_vals, axis=-1)
    combine = jnp.sum(jax.nn.one_hot(top_idx, N_EXPERTS, dtype=jnp.float32) * top_w[..., None],
                      axis=-2).astype(h.dtype)
    y = jnp.zeros_like(h)
    for e in range(N_EXPERTS):
        y = y + combine[..., e:e + 1] * swiglu(h, w_gate[e], w_up[e], w_down[e])
    return y


def setup_inputs(seed: int = 0) -> dict:
    key = jax.random.key(seed)
    ks = jax.random.split(key, 32)
    f32 = jnp.float32
    D = D_MODEL

    def nrm(k, shape, fan_in, gain=1.0):
        return jax.random.normal(k, shape, f32) * (gain * fan_in ** -0.5)

    x = jax.random.normal(ks[0], (BATCH, SEQ, D), f32)
    c = jax.random.normal(ks[1], (BATCH, D), f32)
    ada_w = nrm(ks[2], (DEPTH, D, N_MOD * D), D, 0.5)
    ada_b = 0.02 * jax.random.normal(ks[3], (DEPTH, N_MOD * D), f32)
    norm1_w = 1.0 + 0.05 * jax.random.normal(ks[4], (DEPTH, D), f32)
    norm2_w = 1.0 + 0.05 * jax.random.normal(ks[5], (DEPTH, D), f32)
    w_in = nrm(ks[6], (DEPTH, D, D_IN), D)
    conv_w = nrm(ks[7], (DEPTH, CONV_WIDTH, 3 * DN_WIDTH), CONV_WIDTH)
    a_log = jnp.log(jax.random.uniform(ks[8], (DEPTH, DN_HEADS), f32, 1.0, 16.0))
    dt = jnp.exp(jax.random.uniform(ks[9], (DEPTH, DN_HEADS), f32, np.log(1e-3), np.log(1e-1)))
    dt_bias = dt + jnp.log(-jnp.expm1(-dt))
    dn_norm_w = 1.0 + 0.05 * jax.random.normal(ks[10], (DEPTH, DN_HEAD_DIM), f32)
    gm_ln_w = 1.0 + 0.05 * jax.random.normal(ks[11], (DEPTH, GM_WIDTH), f32)
    gm_ln_b = 0.02 * jax.random.normal(ks[12], (DEPTH, GM_WIDTH), f32)
    gm_spatial_w = nrm(ks[13], (DEPTH, GM_GROUPS, GM_CHUNK, GM_CHUNK), GM_CHUNK, 0.5)
    gm_spatial_b = 1.0 + 0.1 * jax.random.normal(ks[14], (DEPTH, GM_GROUPS, GM_CHUNK), f32)
    w_out = nrm(ks[15], (DEPTH, MIX_WIDTH, D), MIX_WIDTH)
    ffn_w_gate = nrm(ks[16], (N_DENSE, D, D_FF_DENSE), D)
    ffn_w_up = nrm(ks[17], (N_DENSE, D, D_FF_DENSE), D)
    ffn_w_down = nrm(ks[18], (N_DENSE, D_FF_DENSE, D), D_FF_DENSE)
    moe_router = nrm(ks[19], (N_MOE, D, N_EXPERTS), D)
    moe_w_gate = nrm(ks[20], (N_MOE, N_EXPERTS, D, D_FF_EXPERT), D)
    moe_w_up = nrm(ks[21], (N_MOE, N_EXPERTS, D, D_FF_EXPERT), D)
    moe_w_down = nrm(ks[22], (N_MOE, N_EXPERTS, D_FF_EXPERT, D), D_FF_EXPERT)
    final_ada_w = nrm(ks[23], (D, 2 * D), D, 0.5)
    final_ada_b = 0.02 * jax.random.normal(ks[24], (2 * D,), f32)
    final_norm_w = 1.0 + 0.05 * jax.random.normal(ks[25], (D,), f32)
    return {'x': x, 'c': c, 'ada_w': ada_w, 'ada_b': ada_b, 'norm1_w': norm1_w,
            'norm2_w': norm2_w, 'w_in': w_in, 'conv_w': conv_w, 'a_log': a_log,
            'dt_bias': dt_bias, 'dn_norm_w': dn_norm_w, 'gm_ln_w': gm_ln_w,
            'gm_ln_b': gm_ln_b, 'gm_spatial_w': gm_spatial_w, 'gm_spatial_b': gm_spatial_b,
            'w_out': w_out, 'ffn_w_gate': ffn_w_gate, 'ffn_w_up': ffn_w_up,
            'ffn_w_down': ffn_w_down, 'moe_router': moe_router, 'moe_w_gate': moe_w_gate,
            'moe_w_up': moe_w_up, 'moe_w_down': moe_w_down, 'final_ada_w': final_ada_w,
            'final_ada_b': final_ada_b, 'final_norm_w': final_norm_w}


def reference(x, c, ada_w, ada_b, norm1_w, norm2_w, w_in, conv_w, a_log, dt_bias,
              dn_norm_w, gm_ln_w, gm_ln_b, gm_spatial_w, gm_spatial_b, w_out,
              ffn_w_gate, ffn_w_up, ffn_w_down, moe_router, moe_w_gate, moe_w_up,
              moe_w_down, final_ada_w, final_ada_b, final_norm_w):
    c_act = jax.nn.silu(c)
    for i in range(DEPTH):
        mod = c_act @ ada_w[i] + ada_b[i]
        sh1, sc1, g1, sh2, sc2, g2 = jnp.split(mod, N_MOD, axis=-1)
        h = modulate(rms_norm(x, norm1_w[i]), sh1, sc1)
        mix = hybrid_mixer(h, w_in[i], conv_w[i], a_log[i], dt_bias[i], dn_norm_w[i],
                           gm_ln_w[i], gm_ln_b[i], gm_spatial_w[i], gm_spatial_b[i], w_out[i])
        x = x + g1[:, None, :] * mix
        h = modulate(rms_norm(x, norm2_w[i]), sh2, sc2)
        j = i // 2
        if i % 2 == 0:
            f = swiglu(h, ffn_w_gate[j], ffn_w_up[j], ffn_w_down[j])
        else:
            f = moe_swiglu(h, moe_router[j], moe_w_gate[j], moe_w_up[j], moe_w_down[j])
        x = x + g2[:, None, :] * f
    fmod = c_act @ final_ada_w + final_ada_b
    f_shift, f_scale = jnp.split(fmod, 2, axis=-1)
    return modulate(rms_norm(x, final_norm_w), f_shift, f_scale)
```

```python
import contextlib
import numpy as np
import concourse.bass as bass
import concourse.mybir as mybir
from concourse.bass_utils import run_bass_kernel_spmd

F32 = mybir.dt.float32
BF16 = mybir.dt.bfloat16
AF = mybir.ActivationFunctionType
ALU = mybir.AluOpType
AX = mybir.AxisListType

D = 1024
KC = 8
DIN = 3080
OFF_Z, OFF_BA, OFF_GU, OFF_GV = 1536, 2048, 2056, 2568
FF_DENSE = 2816
FF_MOE = 3584
NE = 8
EPS = 1e-6
N_CORES = 8

CFG = {"L": 4096, "NB": 2, "layers": [0, 1, 2, 3], "n_cores": 8}

ENGS = ["pe", "act", "dve", "pool", "sp"]
KEYS = ["pe", "act", "dve", "pool", "sp_dma", "pool_dma", "act_dma"]


class Res:
    __slots__ = ("w", "r", "x")

    def __init__(self, excl=False):
        self.w = None
        self.r = {}
        self.x = excl


class Prog:
    def __init__(self, nc, st):
        self.nc = nc
        self.q = {e: [] for e in ENGS}
        self.cnt = {k: 0 for k in KEYS}
        self.seen = {e: {} for e in ENGS}
        self.signaled = {k: set() for k in KEYS}
        self.rank_base = {k: 0 for k in KEYS}
        self.sems = {k: st.enter_context(nc.semaphore("s_" + k)) for k in KEYS}
        self.nops = 0
        self.mute = False

    def op(self, eng, fn, reads=(), writes=(), dma=False):
        if self.mute:
            return
        if any(r.x for r in reads):
            writes = list(writes) + [r for r in reads if r.x]
            reads = [r for r in reads if not r.x]
        key = eng + "_dma" if dma else eng
        idx = self.cnt[key] + 1
        self.cnt[key] = idx
        waits = {}
        for r in reads:
            if r.w is not None:
                k, v = r.w
                if not (k == "pe" and eng == "pe") and waits.get(k, 0) < v:
                    waits[k] = v
        for w in writes:
            if w.w is not None:
                k, v = w.w
                if not (k == "pe" and eng == "pe") and waits.get(k, 0) < v:
                    waits[k] = v
            for k, v in w.r.items():
                if k == eng:
                    continue
                if waits.get(k, 0) < v:
                    waits[k] = v
        wl = []
        seen = self.seen[eng]
        for k, v in waits.items():
            if seen.get(k, 0) >= v:
                continue
            seen[k] = v
            wl.append((k, v))
            self.signaled[k].add(v)
        for r in reads:
            if r.r.get(key, 0) < idx:
                r.r[key] = idx
        for w in writes:
            w.w = (key, idx)
            w.r = {}
        if dma:
            self.signaled[key].add(idx)
        self.q[eng].append((wl, fn, key, idx))
        self.nops += 1

    def barrier(self):
        tot = dict(self.cnt)
        for e in ENGS:
            wl = []
            for k, v in tot.items():
                if v == 0 or k == e:
                    continue
                if self.seen[e].get(k, 0) >= v:
                    continue
                self.seen[e][k] = v
                wl.append((k, v))
                self.signaled[k].add(v)
            if wl:
                self.q[e].append((wl, None, None, None))

    def flush(self):
        self.barrier()
        ranks = {}
        for k in KEYS:
            s = sorted(self.signaled[k])
            base = self.rank_base[k]
            ranks[k] = {v: base + i + 1 for i, v in enumerate(s)}
            self.rank_base[k] = base + len(s)
            self.signaled[k] = set()
        sems = self.sems
        q = self.q
        self.q = {e: [] for e in ENGS}

        def run(e):
            def body(engine):
                for wl, fn, key, idx in q[e]:
                    for k, v in wl:
                        engine.wait_ge(sems[k], ranks[k][v] * (16 if k.endswith("_dma") else 1))
                    if fn is None:
                        continue
                    ins = fn(engine)
                    if idx in ranks[key]:
                        ins.then_inc(sems[key], 16 if key.endswith("_dma") else 1)
            return body

        with self.nc.Block() as block:
            block.tensor(run("pe"))
            block.scalar(run("act"))
            block.vector(run("dve"))
            block.gpsimd(run("pool"))
            block.sync(run("sp"))


class Ring:
    def __init__(self, items):
        self.items = items
        self.i = 0

    def get(self):
        it = self.items[self.i % len(self.items)]
        self.i += 1
        return it


C_ID, C_ONES, C_TRIL, C_TRILS, C_TRIU, C_M0 = 0, 1, 2, 3, 4, 5
NCONST = 12


def _consts():
    c = np.zeros((NCONST, 128, 128), np.float32)
    i = np.arange(128)[:, None]
    j = np.arange(128)[None, :]
    c[C_ID] = (i == j)
    c[C_ONES] = 1.0
    c[C_TRIL] = (i >= j)
    c[C_TRILS] = (i > j)
    c[C_TRIU] = (i <= j)
    for l in range(7):
        b = 1 << l
        c[C_M0 + l] = ((i // (2 * b)) == (j // (2 * b))) & ((i % (2 * b)) >= b) & ((j % (2 * b)) < b)
    cons = np.ascontiguousarray(c.transpose(1, 0, 2))
    sel = np.zeros((8, 8, 128), np.float32)
    for e in range(8):
        sel[e, e, :] = 1.0
    return cons, sel.reshape(8, 1024)


def build(L, NB, layers):
    T = NB * L
    nl = len(layers)
    nc = bass.Bass("TRN2", target_bir_lowering=False)

    def din(name, shape, dt=F32):
        return nc.dram_tensor(name, list(shape), dt, kind="ExternalInput").ap()

    xT_in = din("xT", [D, T])
    outT = nc.dram_tensor("outT", [D, T], F32, kind="ExternalOutput").ap()
    Xs = nc.dram_tensor("Xs", [D, T], F32, kind="Internal").ap()
    cT_d = din("cT", [128, KC * NB])
    consts_d = din("consts", [128, NCONST * 128])
    sel_d = din("sel", [8, 1024])
    ada_w_d = din("ada_w", [4, D, 6 * D])
    ada_bT_d = din("ada_bT", [128, 4 * 48])
    fada_w_d = din("final_ada_w", [D, 2 * D])
    fada_bT_d = din("fada_bT", [128, 16])
    nw_d = din("nw", [128, 4 * 16 + 8])
    w_in_d = din("w_in", [4, D, DIN])
    cw_d = din("cw", [128, 4 * 48])
    dnw_d = din("dnw", [128, 4])
    alog_d = din("alog_bc", [128, 16])
    dtb_d = din("dtb_bc", [128, 16])
    lnw_d = din("gm_ln_w", [4, 512])
    lnb_d = din("gm_ln_b", [4, 512])
    wsT_d = din("wsT", [4, 128, 512])
    gsb_d = din("gsb", [4, 512])
    w_out_d = din("w_out", [4, D, D])
    fg_d = din("ffn_w_gate", [2, D, FF_DENSE])
    fu_d = din("ffn_w_up", [2, D, FF_DENSE])
    fd_d = din("ffn_w_down", [2, FF_DENSE, D])
    rt_d = din("moe_router", [2, D, NE])
    has_moe = any(l % 2 == 1 for l in layers)
    mg_d = din("moe_w_gate", [2, NE, D, FF_MOE] if has_moe else [1, 1, 1, 1])
    mu_d = din("moe_w_up", [2, NE, D, FF_MOE] if has_moe else [1, 1, 1, 1])
    md_d = din("moe_w_down", [2, NE, FF_MOE, D] if has_moe else [1, 1, 1, 1])

    with contextlib.ExitStack() as st:
        uid = [0]

        def uname(name):
            uid[0] += 1
            return "sb%d_%s" % (uid[0], name)

        def sb(name, shape, dt=F32):
            return st.enter_context(nc.sbuf_tensor(uname(name), list(shape), dt))

        P = Prog(nc, st)
        banks = [st.enter_context(nc.psum_tensor("bank%d" % i, [128, 512], F32)) for i in range(8)]
        bank_r = [Res(True) for _ in range(8)]

        cons = sb("cons", [128, NCONST, 128])
        cons_b = sb("cons_b", [128, 2, 128], BF16)
        sel = sb("sel", [8, 1024])
        onesrow_b = sb("onesrow_b", [1, 128], BF16)
        cT = sb("cT", [128, KC * NB])
        cact_b = sb("cact_b", [128, KC * NB], BF16)
        mod = sb("mod", [128, NB, 4 * 48 + 16])
        ada_bT = sb("ada_bT", [128, 4 * 48 + 16])
        nw = sb("nw", [128, 4 * 16 + 8])
        AB = sb("AB", [128, NB, (4 * 2 + 1) * 8])
        cw = sb("cw", [128, 4 * 48])
        dnw = sb("dnw", [128, 4])
        nA = sb("nA", [128, 16])
        dtb = sb("dtb", [128, 16])
        r_const = Res()

        def DMA(eng, out, in_, R=(), W=()):
            P.op(eng, lambda e: e.dma_start(out=out, in_=in_), R, W, dma=True)

        def MM(out, lhsT, rhs, start, stop, R, W):
            P.op("pe", lambda e: e.matmul(out, lhsT, rhs, start=start, stop=stop), R, W)

        def ACT(out, in_, func, R, W, scale=1.0, bias=0.0):
            P.op("act", lambda e: e.activation(out, in_, func, bias=bias, scale=scale), R, W)

        def TT(out, a, b, op, R, W, eng="dve"):
            P.op(eng, lambda e: e.tensor_tensor(out, a, b, op), R, W)

        def TS(out, a, s1, s2, op0, op1, R, W, eng="dve"):
            if s2 is None:
                P.op(eng, lambda e: e.tensor_scalar(out, a, s1, None, op0), R, W)
            else:
                P.op(eng, lambda e: e.tensor_scalar(out, a, s1, s2, op0, op1), R, W)

        def STT(out, a, s, b, op0, op1, R, W, eng="dve"):
            P.op(eng, lambda e: e.scalar_tensor_tensor(out, a, s, b, op0, op1), R, W)

        def CP(out, in_, R, W, eng="dve"):
            if eng == "act":
                P.op("act", lambda e: e.copy(out, in_), R, W)
            else:
                P.op(eng, lambda e: e.tensor_copy(out, in_), R, W)

        def RECIP(out, in_, R, W):
            P.op("dve", lambda e: e.reciprocal(out, in_), R, W)

        def MEMSET(ap, val, W, eng="pool"):
            P.op(eng, lambda e: e.memset(ap, val), (), W)

        DMA("sp", cons[:], consts_d.rearrange("p (a b) -> p a b", a=NCONST), W=[r_const])
        DMA("pool", cons_b[:], consts_d[:, 0:256].rearrange("p (a b) -> p a b", a=2), W=[r_const])
        DMA("sp", sel[:], sel_d, W=[r_const])
        DMA("sp", cT[:], cT_d, W=[r_const])
        DMA("sp", ada_bT[:, 0:192], ada_bT_d, W=[r_const])
        DMA("sp", ada_bT[:, 192:208], fada_bT_d, W=[r_const])
        DMA("sp", nw[:], nw_d, W=[r_const])
        DMA("sp", cw[:], cw_d, W=[r_const])
        DMA("sp", dnw[:], dnw_d, W=[r_const])
        DMA("sp", nA[:], alog_d, W=[r_const])
        DMA("sp", dtb[:], dtb_d, W=[r_const])
        MEMSET(onesrow_b[:], 1.0, [r_const])
        ACT(nA[:], nA[:], AF.Exp, [r_const], [r_const])
        TS(nA[:], nA[:], -1.0, None, ALU.mult, None, [r_const], [r_const])
        ACT(cact_b[:], cT[:], AF.Silu, [r_const], [r_const])

        ident_f = cons[:, C_ID, :]
        ones_f = cons[:, C_ONES, :]
        trils_f = cons[:, C_TRILS, :]
        triu_f = cons[:, C_TRIU, :]
        ident_b = cons_b[:, 0, :]
        ones_b = cons_b[:, 1, :]

        with contextlib.ExitStack() as ph:
            wbuf = [ph.enter_context(nc.sbuf_tensor(uname("adaw"), [128, KC, 1024], BF16)) for i in range(2)]
            wbuf_r = [Res(), Res()]
            mod_r = Res()
            pieces = []
            for li, l in enumerate(layers):
                for j in range(6):
                    pieces.append((ada_w_d[l, :, j * 1024:(j + 1) * 1024], li * 48 + j * 8))
            for j in range(2):
                pieces.append((fada_w_d[:, j * 1024:(j + 1) * 1024], 192 + j * 8))
            for pi, (src, col0) in enumerate(pieces):
                wb, wr = wbuf[pi % 2], wbuf_r[pi % 2]
                DMA("pool", wb[:], src.rearrange("(k p) n -> p k n", p=128), W=[wr])
                for oc in range(8):
                    bk = pi * 8 + oc
                    ps = banks[bk % 8][:, 0:NB]
                    pr = bank_r[bk % 8]
                    for k in range(KC):
                        MM(ps, wb[:, k, oc * 128:(oc + 1) * 128], cact_b[:, k * NB:(k + 1) * NB],
                           k == 0, k == KC - 1, [wr, r_const], [pr])
                    bcol = (layers[col0 // 48] * 48 + col0 % 48 + oc) if col0 < 192 else (192 + col0 - 192 + oc)
                    TS(mod[:, :, col0 + oc], ps, ada_bT[:, bcol:bcol + 1], None, ALU.add, None,
                       [pr, r_const], [mod_r])
            MEMSET(AB[:], 0.0, [mod_r])
            for li, l in enumerate(layers):
                for sub in range(2):
                    for b in range(NB):
                        sc = mod[:, b, li * 48 + (sub * 3 + 1) * 8: li * 48 + (sub * 3 + 2) * 8]
                        STT(AB[:, b, (li * 2 + sub) * 8:(li * 2 + sub + 1) * 8], sc, 1.0,
                            nw[:, l * 16 + sub * 8: l * 16 + sub * 8 + 8], ALU.add, ALU.mult, [mod_r, r_const], [mod_r])
            for b in range(NB):
                STT(AB[:, b, 64:72], mod[:, b, 200:208], 1.0, nw[:, 64:72], ALU.add, ALU.mult, [mod_r, r_const], [mod_r])
            TS(AB[:], AB[:], 32.0, None, ALU.mult, None, [mod_r], [mod_r])
            P.flush()

        def norm_mod(xt, xt_r, ncols, A, B, out_b, out_r, sq_ring, tmp_ring, rstd, rstd_r, ps, ps_r, out_f=None, out_f_r=None):
            for k in range(KC):
                sq, sq_r = sq_ring.get()
                ACT(sq[:, 0:ncols], xt[:, k, 0:ncols], AF.Square, [xt_r[k]], [sq_r])
                MM(ps[:, 0:ncols], ones_b, sq[:, 0:ncols], k == 0, k == KC - 1, [sq_r, r_const], [ps_r])
            ACT(rstd[:, 0:ncols], ps[:, 0:ncols], AF.Sqrt, [ps_r], [rstd_r], scale=1.0, bias=1024.0 * EPS)
            RECIP(rstd[:, 0:ncols], rstd[:, 0:ncols], [rstd_r], [rstd_r])
            for k in range(KC):
                tm, tm_r = tmp_ring.get()
                TT(tm[:, 0:ncols], xt[:, k, 0:ncols], rstd[:, 0:ncols], ALU.mult, [xt_r[k], rstd_r], [tm_r])
                if out_f is not None:
                    ACT(out_f[:, k, 0:ncols], tm[:, 0:ncols], AF.Identity, [tm_r], [out_f_r[k]],
                        scale=A[:, k:k + 1], bias=B[:, k:k + 1])
                ACT(out_b[:, k, 0:ncols], tm[:, 0:ncols], AF.Identity, [tm_r], [out_r[k]],
                    scale=A[:, k:k + 1], bias=B[:, k:k + 1])

        first = [True]

        def cur_src():
            return xT_in if first[0] else Xs

        def phase_A(li, l):
            TTK = 256
            NCH = TTK // 128
            src = cur_src()
            with contextlib.ExitStack() as ph:
                def pb(name, shape, dt=F32):
                    return ph.enter_context(nc.sbuf_tensor(uname(name), list(shape), dt))

                class _BankRing:
                    def __init__(self, ncol):
                        self.ncol = ncol

                    def get(self):
                        i = bring[0] % 8
                        bring[0] += 1
                        return (banks[i][:, 0:self.ncol], bank_r[i])
                bring = [0]
                wide = _BankRing(256)
                small = _BankRing(128)
                fullr = _BankRing(512)

                w_in_b = pb("w_in_b", [128, KC, DIN], BF16)
                w_out_b = pb("w_out_b", [128, KC, D], BF16)
                wsT_f = pb("wsT_f", [128, 512])
                wsT_b = pb("wsT_b", [128, 512], BF16)
                brow_b = pb("brow_b", [1, 512], BF16)
                lnw = pb("lnw", [128, 512])
                lnb = pb("lnb", [128, 512])
                r_w = Res()
                for k in range(KC):
                    DMA("pool", w_in_b[:, k, :], w_in_d[l, k * 128:(k + 1) * 128, :], W=[r_w])
                DMA("pool", w_out_b[:], w_out_d[l].rearrange("(k p) n -> p k n", p=128), W=[r_w])
                DMA("sp", wsT_f[:], wsT_d[l], W=[r_w])
                DMA("pool", brow_b[:], gsb_d[l:l + 1, :], W=[r_w])
                DMA("sp", lnw[:], lnw_d[l:l + 1, :].to_broadcast([128, 512]), W=[r_w])
                DMA("sp", lnb[:], lnb_d[l:l + 1, :].to_broadcast([128, 512]), W=[r_w])
                for g in range(4):
                    TT(wsT_b[:, g * 128:(g + 1) * 128], wsT_f[:, g * 128:(g + 1) * 128], triu_f, ALU.mult,
                       [r_w, r_const], [r_w])

                xt = pb("xt", [128, KC, TTK]); xt_r = [Res() for _ in range(KC)]
                hT = pb("hT", [128, KC, TTK], BF16); hT_r = [Res() for _ in range(KC)]
                sq_ring = Ring([(pb("sqa%d" % i, [128, TTK], BF16), Res()) for i in range(3)])
                tmp_ring = Ring([(pb("tma%d" % i, [128, TTK]), Res()) for i in range(3)])
                rstd = pb("rstd", [128, TTK]); rstd_r = Res()
                pre = pb("pre", [128, 12, TTK + 3]); pre_r = [Res() for _ in range(12)]
                qs = pb("qs", [128, 4, TTK]); ks = pb("ks", [128, 4, TTK]); vs_b = pb("vs_b", [128, 4, TTK], BF16)
                qkv_r = [Res() for _ in range(12)]
                qn_b = pb("qn_b", [128, 4, TTK], BF16); kn_b = pb("kn_b", [128, 4, TTK], BF16)
                qn_r = [Res() for _ in range(4)]; kn_r = [Res() for _ in range(4)]
                zs = pb("zs", [128, 4, TTK], BF16); zs_r = [Res() for _ in range(4)]
                gus = pb("gus", [128, 4, TTK], BF16); gus_r = [Res() for _ in range(4)]
                gvf_ring = Ring([(pb("gvf%d" % i, [128, 512]), Res()) for i in range(2)])
                gv_b = pb("gv_b", [128, NCH, 512], BF16); gv_r = [Res() for _ in range(NCH)]
                stat = pb("stat", [128, 8]); stat_r = Res()
                mixT = pb("mixT", [128, KC, TTK], BF16); mix_r = [Res() for _ in range(KC)]
                oT = pb("oT", [128, 4, TTK]); oT_r = [Res() for _ in range(4)]
                ba = pb("ba", [128, NCH * 8]); ba_r = Res()
                g_tm = pb("g_tm", [128, NCH * 4]); beta_tm = pb("beta_tm", [128, NCH * 4])
                G_tm = pb("G_tm", [128, NCH * 4]); e1 = pb("e1", [128, NCH * 4]); e2 = pb("e2", [128, NCH * 4])
                eGl = pb("eGl", [128, NCH * 4]); sm_r = Res()
                S_f = pb("S_f", [128, 4, 128]); S_b = pb("S_b", [128, 4, 128], BF16)
                S_r = [Res() for _ in range(4)]; Sb_r = [Res() for _ in range(4)]
                def hb(name, dt=F32):
                    return [(pb("%s%d" % (name, h), [128, 128], dt), Res()) for h in range(4)]
                gbc = hb("gbc"); eG = hb("eG"); dd = hb("dd"); dt_ = hb("dt_")
                Am = hb("Am", BF16); AM = [hb("AM%d_" % lv, BF16) for lv in range(2)]
                U0 = hb("U0", BF16); U1 = hb("U1", BF16); T0 = hb("T0", BF16); T1 = hb("T1", BF16)
                P1 = hb("P1", BF16); attnT = hb("attnT", BF16); qd = hb("qd", BF16)
                kbd = hb("kbd", BF16); kdec = hb("kdec", BF16); vb = hb("vb", BF16)
                u_f = hb("u_f"); wT = hb("wT", BF16); vnew = hb("vnew", BF16)

                A1 = AB[:, :, (li * 2) * 8:(li * 2 + 1) * 8]
                c4 = l * 4

                for seq in range(NB):
                    for h in range(4):
                        MEMSET(S_f[:, h, :], 0.0, [S_r[h]])
                        MEMSET(S_b[:, h, :], 0.0, [Sb_r[h]])
                    for j in range(12):
                        MEMSET(pre[:, j, 0:3], 0.0, [pre_r[j]])
                    for it in range(L // TTK):
                        tok0 = seq * L + it * TTK
                        dsl = src[:, tok0:tok0 + TTK].rearrange("(k p) t -> p k t", p=128)
                        DMA("sp", xt[:], dsl, W=xt_r)
                        ps, ps_r = wide.get()
                        norm_mod(xt, xt_r, TTK, A1[:, seq, :], mod[:, seq, li * 48: li * 48 + 8], hT, hT_r,
                                 sq_ring, tmp_ring, rstd, rstd_r, ps, ps_r)
                        def proj(col0):
                            ps, ps_r = wide.get()
                            for k in range(KC):
                                MM(ps, w_in_b[:, k, col0:col0 + 128], hT[:, k, :], k == 0, k == KC - 1,
                                   [r_w, hT_r[k]], [ps_r])
                            return ps, ps_r
                        for j in range(12):
                            ps, ps_r = proj(j * 128)
                            CP(pre[:, j, 3:3 + TTK], ps, [ps_r], [pre_r[j]], eng="act")
                            acc_t, acc_r = tmp_ring.get()
                            acc = acc_t[:]
                            cwj = cw[:, l * 48 + j * 4: l * 48 + j * 4 + 4]
                            TS(acc, pre[:, j, 0:TTK], cwj[:, 0:1], None, ALU.mult, None, [pre_r[j], r_const], [acc_r])
                            for tp in range(1, 4):
                                STT(acc, pre[:, j, tp:tp + TTK], cwj[:, tp:tp + 1], acc, ALU.mult, ALU.add,
                                    [pre_r[j], acc_r, r_const], [acc_r])
                            dst = (qs, ks, vs_b)[j // 4][:, j % 4, :]
                            ACT(dst, acc, AF.Silu, [acc_r], [qkv_r[j]])
                            CP(pre[:, j, 0:3], pre[:, j, TTK:TTK + 3], [pre_r[j]], [pre_r[j]], eng="pool")
                        for h in range(4):
                            ps, ps_r = proj(OFF_Z + h * 128)
                            ACT(zs[:, h, :], ps, AF.Silu, [ps_r], [zs_r[h]])
                        for g in range(4):
                            ps, ps_r = proj(OFF_GU + g * 128)
                            ACT(gus[:, g, :], ps, AF.Gelu_apprx_tanh, [ps_r], [gus_r[g]])
                        P.mute = CFG.get('upto', 99) < 2
                        psba, psba_r = small.get()
                        for c in range(NCH):
                            for k in range(KC):
                                MM(psba[:, c * 8:(c + 1) * 8], hT[:, k, c * 128:(c + 1) * 128],
                                   w_in_b[:, k, OFF_BA:OFF_BA + 8], k == 0, k == KC - 1, [r_w, hT_r[k]], [psba_r])
                        CP(ba[:], psba[:, 0:NCH * 8], [psba_r], [ba_r])
                        for c in range(NCH):
                            ACT(beta_tm[:, c * 4:c * 4 + 4], ba[:, c * 8:c * 8 + 4], AF.Sigmoid, [ba_r], [sm_r])
                            TT(g_tm[:, c * 4:c * 4 + 4], ba[:, c * 8 + 4:c * 8 + 8], dtb[:, c4:c4 + 4], ALU.add, [ba_r, r_const], [sm_r])
                        g2d = g_tm[:]
                        ACT(g2d, g2d, AF.Exp, [sm_r], [sm_r])
                        ACT(g2d, g2d, AF.Ln, [sm_r], [sm_r], scale=1.0, bias=1.0)
                        for c in range(NCH):
                            TT(g_tm[:, c * 4:c * 4 + 4], g_tm[:, c * 4:c * 4 + 4], nA[:, c4:c4 + 4], ALU.mult, [sm_r, r_const], [sm_r])
                        for c in range(NCH):
                            psg, psg_r = fullr.get()
                            for k in range(KC):
                                MM(psg[:], hT[:, k, c * 128:(c + 1) * 128], w_in_b[:, k, OFF_GV:OFF_GV + 512],
                                   k == 0, k == KC - 1, [r_w, hT_r[k]], [psg_r])
                            gvf, gvf_r = gvf_ring.get()
                            ACT(gvf[:], psg[:], AF.Gelu_apprx_tanh, [psg_r], [gvf_r])
                            P.op("dve", lambda e, gvf=gvf, c=c: e.reduce_sum(stat[:, c:c + 1], gvf[:], AX.X), [gvf_r], [stat_r])
                            TS(stat[:, c:c + 1], stat[:, c:c + 1], 1.0 / 512, None, ALU.mult, None, [stat_r], [stat_r])
                            TS(gvf[:], gvf[:], stat[:, c:c + 1], None, ALU.subtract, None, [gvf_r, stat_r], [gvf_r])
                            sq2, sq2_r = gvf_ring.get()
                            TT(sq2[:], gvf[:], gvf[:], ALU.mult, [gvf_r], [sq2_r])
                            P.op("dve", lambda e, sq2=sq2, c=c: e.reduce_sum(stat[:, 4 + c:5 + c], sq2[:], AX.X), [sq2_r], [stat_r])
                            ACT(stat[:, 4 + c:5 + c], stat[:, 4 + c:5 + c], AF.Sqrt, [stat_r], [stat_r], scale=1.0 / 512, bias=EPS)
                            RECIP(stat[:, 4 + c:5 + c], stat[:, 4 + c:5 + c], [stat_r], [stat_r])
                            STT(gvf[:], gvf[:], stat[:, 4 + c:5 + c], lnw[:], ALU.mult, ALU.mult, [gvf_r, stat_r, r_w], [gvf_r])
                            TT(gv_b[:, c, :], gvf[:], lnb[:], ALU.add, [gvf_r, r_w], [gv_r[c]])
                        P.mute = CFG.get('upto', 99) < 3
                        for g in range(4):
                            ps, ps_r = wide.get()
                            for c in range(NCH):
                                MM(ps[:, c * 128:(c + 1) * 128], gv_b[:, c, g * 128:(g + 1) * 128],
                                   wsT_b[:, g * 128:(g + 1) * 128], True, False, [gv_r[c], r_w], [ps_r])
                                MM(ps[:, c * 128:(c + 1) * 128], onesrow_b[0:1, :], brow_b[0:1, g * 128:(g + 1) * 128],
                                   False, True, [r_const, r_w], [ps_r])
                            TT(mixT[:, 4 + g, :], ps, gus[:, g, :], ALU.mult, [ps_r, gus_r[g]], [mix_r[4 + g]])
                        P.mute = CFG.get('upto', 99) < 4
                        for h in range(4):
                            for (srcb, r_i, dstb, dst_r, scl) in ((qs, h, qn_b, qn_r, 128.0 ** -0.5), (ks, 4 + h, kn_b, kn_r, 1.0)):
                                sq, sq_r = sq_ring.get()
                                ACT(sq[:], srcb[:, h, :], AF.Square, [qkv_r[r_i]], [sq_r])
                                ps, ps_r = wide.get()
                                MM(ps, ones_b, sq[:], True, True, [sq_r, r_const], [ps_r])
                                tm, tm_r = tmp_ring.get()
                                ACT(tm[:], ps, AF.Sqrt, [ps_r], [tm_r], scale=1.0, bias=EPS)
                                RECIP(tm[:], tm[:], [tm_r], [tm_r])
                                STT(dstb[:, h, :], srcb[:, h, :], scl, tm[:], ALU.mult, ALU.mult, [qkv_r[r_i], tm_r], [dst_r[h]])
                        psl, psl_r = small.get()
                        for c in range(NCH):
                            MM(psl[:, c * 4:(c + 1) * 4], ones_f, g_tm[:, c * 4:c * 4 + 4], True, True, [sm_r, r_const], [psl_r])
                            MM(psl[:, 16 + c * 4:16 + (c + 1) * 4], triu_f, g_tm[:, c * 4:c * 4 + 4], True, True, [sm_r, r_const], [psl_r])
                        sm2_r = Res()
                        CP(G_tm[:], psl[:, 16:16 + NCH * 4], [psl_r], [sm2_r])
                        ACT(eGl[:], psl[:, 0:NCH * 4], AF.Exp, [psl_r], [sm2_r])
                        TT(e2[:], psl[:, 0:NCH * 4], G_tm[:],
                           ALU.subtract, [psl_r, sm2_r], [sm2_r])
                        ACT(e2[:], e2[:], AF.Exp, [sm2_r], [sm2_r])
                        ACT(e1[:], G_tm[:], AF.Exp, [sm2_r], [sm2_r])
                        TT(e1[:], e1[:],
                           beta_tm[:], ALU.mult, [sm2_r, sm_r], [sm2_r])
                        P.mute = CFG.get('upto', 99) < 5
                        for c in range(NCH):
                            cs = slice(c * 128, (c + 1) * 128)
                            HS = range(4)
                            psG = {}
                            for h in HS:
                                TS(gbc[h][0][:], ones_f, g_tm[:, c * 4 + h:c * 4 + h + 1], None, ALU.mult, None, [sm_r, r_const], [gbc[h][1]])
                                psG[h] = small.get()
                                MM(psG[h][0], gbc[h][0][:], triu_f, True, True, [gbc[h][1], r_const], [psG[h][1]])
                            for h in HS:
                                pg, pg_r = psG[h]
                                Gc = G_tm[:, c * 4 + h:c * 4 + h + 1]
                                ACT(eG[h][0][:], pg, AF.Exp, [pg_r], [eG[h][1]])
                                TS(dd[h][0][:], pg, Gc, 0.0, ALU.subtract, ALU.max, [pg_r, sm2_r], [dd[h][1]])
                                TS(dt_[h][0][:], pg, Gc, 0.0, ALU.subtract, ALU.min, [pg_r, sm2_r], [dt_[h][1]])
                                ACT(dd[h][0][:], dd[h][0][:], AF.Exp, [dd[h][1]], [dd[h][1]], scale=-1.0)
                                ACT(dt_[h][0][:], dt_[h][0][:], AF.Exp, [dt_[h][1]], [dt_[h][1]])
                                TT(dd[h][0][:], dd[h][0][:], trils_f, ALU.mult, [dd[h][1], r_const], [dd[h][1]], eng="pool")
                                TT(dt_[h][0][:], dt_[h][0][:], triu_f, ALU.mult, [dt_[h][1], r_const], [dt_[h][1]], eng="pool")
                                TT(qd[h][0][:], qn_b[:, h, cs], eG[h][0][:], ALU.mult, [qn_r[h], eG[h][1]], [qd[h][1]])
                            for h in HS:
                                pk, pk_r = small.get()
                                MM(pk, kn_b[:, h, cs], kn_b[:, h, cs], True, True, [kn_r[h]], [pk_r])
                                STT(Am[h][0][:], pk, beta_tm[:, c * 4 + h:c * 4 + h + 1], dd[h][0][:], ALU.mult, ALU.mult,
                                    [pk_r, sm_r, dd[h][1]], [Am[h][1]])
                                pq, pq_r = small.get()
                                MM(pq, kn_b[:, h, cs], qn_b[:, h, cs], True, True, [kn_r[h], qn_r[h]], [pq_r])
                                TT(attnT[h][0][:], pq, dt_[h][0][:], ALU.mult, [pq_r, dt_[h][1]], [attnT[h][1]])
                                pt, pt_r = small.get()
                                MM(pt, kn_b[:, h, cs], ident_b, True, True, [kn_r[h], r_const], [pt_r])
                                TS(kbd[h][0][:], pt, e1[:, c * 4 + h:c * 4 + h + 1], None, ALU.mult, None, [pt_r, sm2_r], [kbd[h][1]])
                                ACT(kdec[h][0][:], pt, AF.Identity, [pt_r, sm2_r], [kdec[h][1]], scale=e2[:, c * 4 + h:c * 4 + h + 1])
                                pv, pv_r = small.get()
                                MM(pv, vs_b[:, h, cs], ident_b, True, True, [qkv_r[8 + h], r_const], [pv_r])
                                TS(vb[h][0][:], pv, beta_tm[:, c * 4 + h:c * 4 + h + 1], None, ALU.mult, None, [pv_r, sm_r], [vb[h][1]])
                            P.mute = CFG.get('upto', 99) < 6
                            Ucur = {}; Tcur = {}
                            for h in HS:
                                a0, a0_r = AM[0][h]
                                TT(a0[:], Am[h][0][:], cons[:, C_M0, :], ALU.mult, [Am[h][1], r_const], [a0_r], eng="pool")
                                pt, pt_r = small.get()
                                MM(pt, a0[:], ident_b, True, True, [a0_r, r_const], [pt_r])
                                TT(U0[h][0][:], ident_f, pt, ALU.subtract, [pt_r, r_const], [U0[h][1]])
                                TT(T0[h][0][:], ident_f, a0[:], ALU.subtract, [a0_r, r_const], [T0[h][1]], eng="pool")
                                Ucur[h] = U0[h]; Tcur[h] = T0[h]
                            for lv in range(1, 7):
                                pp = {}
                                for h in HS:
                                    al, al_r = AM[lv % 2][h]
                                    TT(al[:], Am[h][0][:], cons[:, C_M0 + lv, :], ALU.mult, [Am[h][1], r_const], [al_r], eng="pool")
                                    pp[h] = small.get()
                                    MM(pp[h][0], al[:], Ucur[h][0][:], True, True, [al_r, Ucur[h][1]], [pp[h][1]])
                                for h in HS:
                                    CP(P1[h][0][:], pp[h][0], [pp[h][1]], [P1[h][1]], eng="act")
                                px = {}
                                for h in HS:
                                    px[h] = small.get()
                                    MM(px[h][0], Tcur[h][0][:], P1[h][0][:], True, True, [Tcur[h][1], P1[h][1]], [px[h][1]])
                                for h in HS:
                                    Un = U1[h] if Ucur[h] is U0[h] else U0[h]
                                    TT(Un[0][:], Ucur[h][0][:], px[h][0], ALU.subtract, [Ucur[h][1], px[h][1]], [Un[1]])
                                    Ucur[h] = Un
                                if lv < 6:
                                    ptt = {}
                                    for h in HS:
                                        ptt[h] = small.get()
                                        MM(ptt[h][0], Ucur[h][0][:], ident_b, True, True, [Ucur[h][1], r_const], [ptt[h][1]])
                                    for h in HS:
                                        Tn = T1[h] if Tcur[h] is T0[h] else T0[h]
                                        CP(Tn[0][:], ptt[h][0], [ptt[h][1]], [Tn[1]], eng="act")
                                        Tcur[h] = Tn
                            P.mute = CFG.get('upto', 99) < 7
                            for h in HS:
                                pu, pu_r = small.get()
                                MM(pu, Ucur[h][0][:], vb[h][0][:], True, True, [Ucur[h][1], vb[h][1]], [pu_r])
                                CP(u_f[h][0][:], pu, [pu_r], [u_f[h][1]], eng="act")
                                pw, pw_r = small.get()
                                MM(pw, kbd[h][0][:], Ucur[h][0][:], True, True, [kbd[h][1], Ucur[h][1]], [pw_r])
                                CP(wT[h][0][:], pw, [pw_r], [wT[h][1]])
                            pws = {}
                            for h in HS:
                                pws[h] = small.get()
                                MM(pws[h][0], wT[h][0][:], S_b[:, h, :], True, True, [wT[h][1], Sb_r[h]], [pws[h][1]])
                            for h in HS:
                                TT(vnew[h][0][:], u_f[h][0][:], pws[h][0], ALU.subtract, [u_f[h][1], pws[h][1]], [vnew[h][1]])
                            for h in HS:
                                po, po_r = small.get()
                                MM(po, S_b[:, h, :], qd[h][0][:], True, False, [Sb_r[h], qd[h][1]], [po_r])
                                MM(po, vnew[h][0][:], attnT[h][0][:], False, True, [vnew[h][1], attnT[h][1]], [po_r])
                                CP(oT[:, h, cs], po, [po_r], [oT_r[h]], eng="act")
                                pS, pS_r = small.get()
                                MM(pS, kdec[h][0][:], vnew[h][0][:], True, True, [kdec[h][1], vnew[h][1]], [pS_r])
                                STT(S_f[:, h, :], S_f[:, h, :], eGl[:, c * 4 + h:c * 4 + h + 1], pS, ALU.mult, ALU.add,
                                    [S_r[h], sm2_r, pS_r], [S_r[h]])
                                CP(S_b[:, h, :], S_f[:, h, :], [S_r[h]], [Sb_r[h]], eng="act")
                        P.mute = CFG.get('upto', 99) < 8
                        for h in range(4):
                            sq, sq_r = sq_ring.get()
                            ACT(sq[:], oT[:, h, :], AF.Square, [oT_r[h]], [sq_r])
                            ps, ps_r = wide.get()
                            MM(ps, ones_b, sq[:], True, True, [sq_r, r_const], [ps_r])
                            tm, tm_r = tmp_ring.get()
                            ACT(tm[:], ps, AF.Sqrt, [ps_r], [tm_r], scale=1.0 / 128, bias=EPS)
                            RECIP(tm[:], tm[:], [tm_r], [tm_r])
                            TT(tm[:], tm[:], oT[:, h, :], ALU.mult, [tm_r, oT_r[h]], [tm_r])
                            STT(mixT[:, h, :], tm[:], dnw[:, l:l + 1], zs[:, h, :], ALU.mult, ALU.mult,
                                [tm_r, r_const, zs_r[h]], [mix_r[h]])
                        P.mute = CFG.get('upto', 99) < 0
                        for oc in range(KC):
                            ps, ps_r = wide.get()
                            for k in range(KC):
                                MM(ps, w_out_b[:, k, oc * 128:(oc + 1) * 128], mixT[:, k, :], k == 0, k == KC - 1,
                                   [r_w, mix_r[k]], [ps_r])
                            g1 = mod[:, seq, li * 48 + 16 + oc: li * 48 + 17 + oc]
                            STT(xt[:, oc, :], ps, g1, xt[:, oc, :], ALU.mult, ALU.add, [ps_r, xt_r[oc]], [xt_r[oc]])
                        DMA("sp", Xs[:, tok0:tok0 + TTK].rearrange("(k p) t -> p k t", p=128), xt[:], R=xt_r)
                first[0] = False
                P.flush()

        def phase_B(li, l):
            moe = (l % 2 == 1)
            j = l // 2
            FF = FF_MOE if moe else FF_DENSE
            nexp = NE if moe else 1
            TB = min(1024, L)
            NT = TB // 512
            src = cur_src()
            with contextlib.ExitStack() as ph:
                def pb(name, shape, dt=F32):
                    return ph.enter_context(nc.sbuf_tensor(uname(name), list(shape), dt))
                pbank = Ring([(banks[i], bank_r[i]) for i in range(8)])
                h2b = pb("h2b", [128, KC, TB], BF16); h2b_r = [[Res() for _ in range(KC)] for _ in range(NT)]
                yacc = pb("yacc", [128, KC, TB]); y_r = [[Res() for _ in range(KC)] for _ in range(NT)]
                xt2 = [pb("xtb%d" % i, [128, KC, 512]) for i in range(2)]
                xt2_r = [[Res() for _ in range(KC)] for _ in range(2)]
                sq_ring = Ring([(pb("sqb%d" % i, [128, 512], BF16), Res()) for i in range(3)])
                tmp_ring = Ring([(pb("tmb%d" % i, [128, 512]), Res()) for i in range(3)])
                sg_ring = Ring([(pb("sg%d" % i, [128, 512]), Res()) for i in range(3)])
                hid = [[(pb("hid%d_%d" % (i, f), [128, 512], BF16), Res()) for f in range(4)] for i in range(2)]
                rstd = pb("rstdb", [128, 512]); rstd_r = Res()
                wg = [pb("wg%d" % i, [128, KC, 512], BF16) for i in range(2)]
                wu = [pb("wu%d" % i, [128, KC, 512], BF16) for i in range(2)]
                wd = [pb("wd%d" % i, [128, 4, D], BF16) for i in range(2)]
                w_r = [Res(), Res()]
                if moe:
                    h2f = pb("h2f", [128, KC, 512]); h2f_r = [Res() for _ in range(KC)]
                    wr = pb("wr", [128, KC, NE]); wr_r = Res()
                    DMA("sp", wr[:], rt_d[j].rearrange("(k p) e -> p k e", p=128), W=[wr_r])
                    comb = pb("comb", [128, TB // 128, NE]); comb_r = Res()
                    combT = pb("combT", [8, TB]); combT_r = Res()
                    cbc = [pb("cbc%d" % i, [128, TB]) for i in range(2)]
                    cbc_r = [[Res() for _ in range(NT)] for _ in range(2)]
                    lg = pb("lg", [128, NE]); lg2 = pb("lg2", [128, NE]); mk1 = pb("mk1", [128, NE]); mk2 = pb("mk2", [128, NE])
                    m12 = pb("m12", [128, 4]); lg_r = Res()
                A2 = AB[:, :, (li * 2 + 1) * 8:(li * 2 + 2) * 8]
                pieces = []
                for e in range(nexp):
                    f0 = 0
                    while f0 < FF:
                        pw_ = min(512, FF - f0)
                        pieces.append((e, f0, pw_))
                        f0 += pw_

                def load_piece(pi):
                    e, f0, pw_ = pieces[pi]
                    bi = pi % 2
                    if moe:
                        g_src, u_src, d_src = mg_d[j, e], mu_d[j, e], md_d[j, e]
                    else:
                        g_src, u_src, d_src = fg_d[j], fu_d[j], fd_d[j]
                    DMA("pool", wg[bi][:, :, 0:pw_], g_src[:, f0:f0 + pw_].rearrange("(k p) n -> p k n", p=128), W=[w_r[bi]])
                    DMA("pool", wu[bi][:, :, 0:pw_], u_src[:, f0:f0 + pw_].rearrange("(k p) n -> p k n", p=128), W=[w_r[bi]])
                    DMA("pool", wd[bi][:, 0:pw_ // 128, :], d_src[f0:f0 + pw_, :].rearrange("(f p) n -> p f n", p=128), W=[w_r[bi]])

                xload = [0]
                for blk in range(T // TB):
                    seq = (blk * TB) // L
                    btok = blk * TB
                    load_piece(0)
                    for t in range(NT):
                        xb = xload[0] % 2; xload[0] += 1
                        xt, xt_r = xt2[xb], xt2_r[xb]
                        tok0 = btok + t * 512
                        DMA("sp", xt[:], src[:, tok0:tok0 + 512].rearrange("(k p) t -> p k t", p=128), W=xt_r)
                        ps, ps_r = pbank.get()
                        class _V:
                            def __getitem__(self, idx):
                                p_, k_, c_ = idx
                                return h2b[p_, k_, t * 512 + (c_.start or 0): t * 512 + (c_.stop or 512)]
                        norm_mod(xt, xt_r, 512, A2[:, seq, :], mod[:, seq, li * 48 + 24: li * 48 + 32], _V(), h2b_r[t],
                                 sq_ring, tmp_ring, rstd, rstd_r, ps, ps_r,
                                 out_f=(h2f if moe else None), out_f_r=(h2f_r if moe else None))
                        if moe:
                            P.mute = CFG.get('moe_upto', 99) < 1
                            for c in range(4):
                                gc = t * 4 + c
                                pl, pl_r = pbank.get()
                                for k in range(KC):
                                    MM(pl[:, 0:NE], h2f[:, k, c * 128:(c + 1) * 128], wr[:, k, :], k == 0, k == KC - 1,
                                       [h2f_r[k], wr_r], [pl_r])
                                CP(lg[:], pl[:, 0:NE], [pl_r], [lg_r])
                                P.op("dve", lambda e: e.reduce_max(m12[:, 0:1], lg[:], AX.X), [lg_r], [lg_r])
                                TS(mk1[:], lg[:], m12[:, 0:1], None, ALU.is_equal, None, [lg_r], [lg_r])
                                STT(lg2[:], mk1[:], -1e30, lg[:], ALU.mult, ALU.add, [lg_r], [lg_r])
                                P.op("dve", lambda e: e.reduce_max(m12[:, 1:2], lg2[:], AX.X), [lg_r], [lg_r])
                                TS(mk2[:], lg2[:], m12[:, 1:2], None, ALU.is_equal, None, [lg_r], [lg_r])
                                TT(m12[:, 2:3], m12[:, 1:2], m12[:, 0:1], ALU.subtract, [lg_r], [lg_r])
                                ACT(m12[:, 2:3], m12[:, 2:3], AF.Exp, [lg_r], [lg_r])
                                TS(m12[:, 2:3], m12[:, 2:3], 1.0, None, ALU.add, None, [lg_r], [lg_r])
                                RECIP(m12[:, 2:3], m12[:, 2:3], [lg_r], [lg_r])
                                TS(m12[:, 3:4], m12[:, 2:3], -1.0, 1.0, ALU.mult, ALU.add, [lg_r], [lg_r])
                                TS(mk1[:], mk1[:], m12[:, 2:3], None, ALU.mult, None, [lg_r], [lg_r])
                                STT(comb[:, gc, :], mk2[:], m12[:, 3:4], mk1[:], ALU.mult, ALU.add, [lg_r], [comb_r])
                                P.mute = CFG.get('moe_upto', 99) < 2
                                pc, pc_r = pbank.get()
                                MM(pc[0:8, 0:128], comb[:, gc, :], ident_f, True, True, [comb_r, r_const], [pc_r])
                                CP(combT[:, gc * 128:(gc + 1) * 128], pc[0:8, 0:128], [pc_r], [combT_r])
                    P.mute = False
                    first_acc = True
                    for pi, (e, f0, pw_) in enumerate(pieces):
                        if pi + 1 < len(pieces):
                            load_piece(pi + 1)
                        bi = pi % 2
                        nf = pw_ // 128
                        if moe and f0 == 0:
                            P.mute = CFG.get('moe_upto', 99) < 3
                            for t in range(NT):
                                pc, pc_r = pbank.get()
                                MM(pc[:], sel[0:8, e * 128:(e + 1) * 128], combT[:, t * 512:(t + 1) * 512], True, True,
                                   [r_const, combT_r], [pc_r])
                                CP(cbc[e % 2][:, t * 512:(t + 1) * 512], pc[:], [pc_r], [cbc_r[e % 2][t]], eng="act")
                        P.mute = False
                        for t in range(NT):
                            ts_ = slice(t * 512, (t + 1) * 512)
                            hb_ = hid[(pi * NT + t) % 2]
                            for f in range(nf):
                                pg, pg_r = pbank.get()
                                for k in range(KC):
                                    MM(pg[:], wg[bi][:, k, f * 128:(f + 1) * 128], h2b[:, k, ts_], k == 0, k == KC - 1,
                                       [w_r[bi], h2b_r[t][k]], [pg_r])
                                pu, pu_r = pbank.get()
                                for k in range(KC):
                                    MM(pu[:], wu[bi][:, k, f * 128:(f + 1) * 128], h2b[:, k, ts_], k == 0, k == KC - 1,
                                       [w_r[bi], h2b_r[t][k]], [pu_r])
                                sg, sg_r = sg_ring.get()
                                ACT(sg[:], pg[:], AF.Silu, [pg_r], [sg_r])
                                if moe:
                                    P.mute = CFG.get('moe_upto', 99) < 4
                                    TT(sg[:], sg[:], cbc[e % 2][:, ts_], ALU.mult, [sg_r, cbc_r[e % 2][t]], [sg_r])
                                P.mute = False
                                TT(hb_[f][0][:], pu[:], sg[:], ALU.mult, [pu_r, sg_r], [hb_[f][1]])
                            for oc in range(KC):
                                py, py_r = pbank.get()
                                for f in range(nf):
                                    MM(py[:], wd[bi][:, f, oc * 128:(oc + 1) * 128], hb_[f][0][:], f == 0, f == nf - 1,
                                       [w_r[bi], hb_[f][1]], [py_r])
                                if first_acc:
                                    CP(yacc[:, oc, ts_], py[:], [py_r], [y_r[t][oc]], eng="act")
                                else:
                                    TT(yacc[:, oc, ts_], yacc[:, oc, ts_], py[:], ALU.add, [y_r[t][oc], py_r], [y_r[t][oc]])
                        first_acc = False
                    for t in range(NT):
                        xb = xload[0] % 2; xload[0] += 1
                        xt, xt_r = xt2[xb], xt2_r[xb]
                        tok0 = btok + t * 512
                        DMA("sp", xt[:], src[:, tok0:tok0 + 512].rearrange("(k p) t -> p k t", p=128), W=xt_r)
                        for oc in range(KC):
                            g2 = mod[:, seq, li * 48 + 40 + oc: li * 48 + 41 + oc]
                            STT(xt[:, oc, :], yacc[:, oc, t * 512:(t + 1) * 512], g2, xt[:, oc, :], ALU.mult, ALU.add,
                                [y_r[t][oc], xt_r[oc]], [xt_r[oc]])
                        DMA("sp", Xs[:, tok0:tok0 + 512].rearrange("(k p) t -> p k t", p=128), xt[:], R=xt_r)
                first[0] = False
                P.flush()

        def phase_final():
            src = cur_src()
            with contextlib.ExitStack() as ph:
                def pb(name, shape, dt=F32):
                    return ph.enter_context(nc.sbuf_tensor(uname(name), list(shape), dt))
                pbank = Ring([(banks[i], bank_r[i]) for i in range(8)])
                xt2 = [pb("xtf%d" % i, [128, KC, 512]) for i in range(2)]
                xt2_r = [[Res() for _ in range(KC)] for _ in range(2)]
                ot2 = [pb("otf%d" % i, [128, KC, 512]) for i in range(2)]
                ot2_r = [[Res() for _ in range(KC)] for _ in range(2)]
                sq_ring = Ring([(pb("sqf%d" % i, [128, 512], BF16), Res()) for i in range(3)])
                tmp_ring = Ring([(pb("tmf%d" % i, [128, 512]), Res()) for i in range(3)])
                rstd = pb("rstdf", [128, 512]); rstd_r = Res()
                Af = AB[:, :, 64:72]
                for t in range(T // 512):
                    seq = (t * 512) // L
                    xt, xt_r = xt2[t % 2], xt2_r[t % 2]
                    ot, ot_r = ot2[t % 2], ot2_r[t % 2]
                    tok0 = t * 512
                    DMA("sp", xt[:], src[:, tok0:tok0 + 512].rearrange("(k p) t -> p k t", p=128), W=xt_r)
                    ps, ps_r = pbank.get()
                    for k in range(KC):
                        sq, sq_r = sq_ring.get()
                        ACT(sq[:], xt[:, k, :], AF.Square, [xt_r[k]], [sq_r])
                        MM(ps[:], ones_b, sq[:], k == 0, k == KC - 1, [sq_r, r_const], [ps_r])
                    ACT(rstd[:], ps[:], AF.Sqrt, [ps_r], [rstd_r], scale=1.0, bias=1024.0 * EPS)
                    RECIP(rstd[:], rstd[:], [rstd_r], [rstd_r])
                    for k in range(KC):
                        tm, tm_r = tmp_ring.get()
                        TT(tm[:], xt[:, k, :], rstd[:], ALU.mult, [xt_r[k], rstd_r], [tm_r])
                        ACT(ot[:, k, :], tm[:], AF.Identity, [tm_r], [ot_r[k]],
                            scale=Af[:, seq, k:k + 1], bias=mod[:, seq, 192 + k:193 + k])
                    DMA("sp", outT[:, tok0:tok0 + 512].rearrange("(k p) t -> p k t", p=128), ot[:], R=ot_r)
                P.flush()

        for li, l in enumerate(layers):
            phase_A(li, l)
            if CFG.get('upto', 99) >= 9:
                phase_B(li, l)
        phase_final()
    return nc


_PROG_CACHE = {}


def kernel(x, c, ada_w, ada_b, norm1_w, norm2_w, w_in, conv_w, a_log, dt_bias,
           dn_norm_w, gm_ln_w, gm_ln_b, gm_spatial_w, gm_spatial_b, w_out,
           ffn_w_gate, ffn_w_up, ffn_w_down, moe_router, moe_w_gate, moe_w_up,
           moe_w_down, final_ada_w, final_ada_b, final_norm_w):
    f = lambda a: np.ascontiguousarray(np.asarray(a, dtype=np.float32))
    x = f(x); c = f(c)
    B, L, _ = x.shape
    n_cores = CFG["n_cores"]
    NB = B // n_cores
    layers = list(CFG["layers"])
    key = (L, NB, tuple(layers))
    if key not in _PROG_CACHE:
        import time as _t, sys as _s
        _t0 = _t.time()
        _PROG_CACHE[key] = build(L, NB, layers)
        print("[kernel] build %.1fs" % (_t.time() - _t0), file=_s.stderr)
    nc = _PROG_CACHE[key]
    cons, sel = _consts()

    def pm(v, nchunk):
        v = f(v)
        lead = v.shape[:-1]
        return np.ascontiguousarray(np.moveaxis(v.reshape(*lead, nchunk, 128), -1, 0))

    ada_bT = pm(ada_b, 48).reshape(128, 4 * 48)
    fada_bT = pm(final_ada_b, 16).reshape(128, 16)
    nw = np.concatenate([np.stack([pm(norm1_w, 8), pm(norm2_w, 8)], axis=2).reshape(128, 64),
                         pm(final_norm_w, 8).reshape(128, 8)], axis=1)
    cwp = np.ascontiguousarray(np.transpose(pm(conv_w, 12), (0, 1, 3, 2))).reshape(128, 4 * 48)
    dnw = np.ascontiguousarray(f(dn_norm_w).T)
    alog_bc = np.ascontiguousarray(np.broadcast_to(f(a_log).reshape(1, 16), (128, 16)))
    dtb_bc = np.ascontiguousarray(np.broadcast_to(f(dt_bias).reshape(1, 16), (128, 16)))
    wsT = np.ascontiguousarray(np.transpose(f(gm_spatial_w), (0, 3, 1, 2))).reshape(4, 128, 512)
    gsb = f(gm_spatial_b).reshape(4, 512)
    shared = {
        "consts": cons.reshape(128, NCONST * 128), "sel": sel,
        "ada_w": f(ada_w), "ada_bT": ada_bT, "final_ada_w": f(final_ada_w), "fada_bT": fada_bT, "nw": nw,
        "w_in": f(w_in), "cw": cwp, "dnw": dnw, "alog_bc": alog_bc, "dtb_bc": dtb_bc,
        "gm_ln_w": f(gm_ln_w), "gm_ln_b": f(gm_ln_b), "wsT": wsT, "gsb": gsb, "w_out": f(w_out),
        "ffn_w_gate": f(ffn_w_gate), "ffn_w_up": f(ffn_w_up), "ffn_w_down": f(ffn_w_down),
        "moe_router": f(moe_router), "moe_w_gate": f(moe_w_gate), "moe_w_up": f(moe_w_up), "moe_w_down": f(moe_w_down),
    }
    if not any(l % 2 == 1 for l in layers):
        for k_ in ("moe_w_gate", "moe_w_up", "moe_w_down"):
            shared[k_] = np.zeros((1, 1, 1, 1), np.float32)
    in_maps = []
    for i in range(n_cores):
        xs = x[i * NB:(i + 1) * NB].reshape(NB * L, D)
        cs = c[i * NB:(i + 1) * NB]
        cTl = np.ascontiguousarray(np.transpose(cs.reshape(NB, KC, 128), (2, 1, 0))).reshape(128, KC * NB)
        m = dict(shared)
        m["xT"] = np.ascontiguousarray(xs.T)
        m["cT"] = cTl
        in_maps.append(m)
    import time as _t, sys as _s
    _t0 = _t.time()
    if CFG.get("sim"):
        return nc, in_maps
    res = run_bass_kernel_spmd(nc, in_maps, core_ids=list(range(n_cores)))
    print("[kernel] launch+transfer %.1fs" % (_t.time() - _t0), file=_s.stderr)
    out = np.empty((B, L, D), np.float32)
    for i in range(n_cores):
        out[i * NB:(i + 1) * NB] = res.results[i]["outT"].T.reshape(NB, L, D)
    return out
```

```python
import contextlib
import numpy as np
import concourse.bass as bass
import concourse.mybir as mybir
from concourse.bass_utils import run_bass_kernel_spmd

F32 = mybir.dt.float32
BF16 = mybir.dt.bfloat16
AF = mybir.ActivationFunctionType
ALU = mybir.AluOpType
AX = mybir.AxisListType

D = 1024
KC = 8
DIN = 3080
OFF_Z, OFF_BA, OFF_GU, OFF_GV = 1536, 2048, 2056, 2568
FF_DENSE = 2816
FF_MOE = 3584
NE = 8
EPS = 1e-6
N_CORES = 8

CFG = {"L": 4096, "NB": 2, "layers": [0, 1, 2, 3], "n_cores": 8}

ENGS = ["pe", "act", "dve", "pool", "sp"]
KEYS = ["pe", "act", "dve", "pool", "sp_dma", "pool_dma", "act_dma"]


class Res:
    __slots__ = ("w", "r", "x")

    def __init__(self, excl=False):
        self.w = None
        self.r = {}
        self.x = excl


class Prog:
    def __init__(self, nc, st):
        self.nc = nc
        self.q = {e: [] for e in ENGS}
        self.cnt = {k: 0 for k in KEYS}
        self.seen = {e: {} for e in ENGS}
        self.signaled = {k: set() for k in KEYS}
        self.rank_base = {k: 0 for k in KEYS}
        self.sems = {k: st.enter_context(nc.semaphore("s_" + k)) for k in KEYS}
        self.nops = 0
        self.mute = False

    def op(self, eng, fn, reads=(), writes=(), dma=False):
        if self.mute:
            return
        if any(r.x for r in reads):
            writes = list(writes) + [r for r in reads if r.x]
            reads = [r for r in reads if not r.x]
        key = eng + "_dma" if dma else eng
        idx = self.cnt[key] + 1
        self.cnt[key] = idx
        waits = {}
        for r in reads:
            if r.w is not None:
                k, v = r.w
                if not (k == "pe" and eng == "pe") and waits.get(k, 0) < v:
                    waits[k] = v
        for w in writes:
            if w.w is not None:
                k, v = w.w
                if not (k == "pe" and eng == "pe") and waits.get(k, 0) < v:
                    waits[k] = v
            for k, v in w.r.items():
                if k == eng:
                    continue
                if waits.get(k, 0) < v:
                    waits[k] = v
        wl = []
        seen = self.seen[eng]
        for k, v in waits.items():
            if seen.get(k, 0) >= v:
                continue
            seen[k] = v
            wl.append((k, v))
            self.signaled[k].add(v)
        for r in reads:
            if r.r.get(key, 0) < idx:
                r.r[key] = idx
        for w in writes:
            w.w = (key, idx)
            w.r = {}
        if dma:
            self.signaled[key].add(idx)
        self.q[eng].append((wl, fn, key, idx))
        self.nops += 1

    def barrier(self):
        tot = dict(self.cnt)
        for e in ENGS:
            wl = []
            for k, v in tot.items():
                if v == 0 or k == e:
                    continue
                if self.seen[e].get(k, 0) >= v:
                    continue
                self.seen[e][k] = v
                wl.append((k, v))
                self.signaled[k].add(v)
            if wl:
                self.q[e].append((wl, None, None, None))

    def flush(self):
        self.barrier()
        ranks = {}
        for k in KEYS:
            s = sorted(self.signaled[k])
            base = self.rank_base[k]
            ranks[k] = {v: base + i + 1 for i, v in enumerate(s)}
            self.rank_base[k] = base + len(s)
            self.signaled[k] = set()
        sems = self.sems
        q = self.q
        self.q = {e: [] for e in ENGS}

        def run(e):
            def body(engine):
                for wl, fn, key, idx in q[e]:
                    for k, v in wl:
                        engine.wait_ge(sems[k], ranks[k][v] * (16 if k.endswith("_dma") else 1))
                    if fn is None:
                        continue
                    ins = fn(engine)
                    if idx in ranks[key]:
                        ins.then_inc(sems[key], 16 if key.endswith("_dma") else 1)
            return body

        with self.nc.Block() as block:
            block.tensor(run("pe"))
            block.scalar(run("act"))
            block.vector(run("dve"))
            block.gpsimd(run("pool"))
            block.sync(run("sp"))


class Ring:
    def __init__(self, items):
        self.items = items
        self.i = 0

    def get(self):
        it = self.items[self.i % len(self.items)]
        self.i += 1
        return it


C_ID, C_ONES, C_TRIL, C_TRILS, C_TRIU, C_M0 = 0, 1, 2, 3, 4, 5
NCONST = 12


def _consts():
    c = np.zeros((NCONST, 128, 128), np.float32)
    i = np.arange(128)[:, None]
    j = np.arange(128)[None, :]
    c[C_ID] = (i == j)
    c[C_ONES] = 1.0
    c[C_TRIL] = (i >= j)
    c[C_TRILS] = (i > j)
    c[C_TRIU] = (i <= j)
    for l in range(7):
        b = 1 << l
        c[C_M0 + l] = ((i // (2 * b)) == (j // (2 * b))) & ((i % (2 * b)) >= b) & ((j % (2 * b)) < b)
    cons = np.ascontiguousarray(c.transpose(1, 0, 2))
    sel = np.zeros((8, 8, 128), np.float32)
    for e in range(8):
        sel[e, e, :] = 1.0
    return cons, sel.reshape(8, 1024)


def build(L, NB, layers):
    T = NB * L
    nl = len(layers)
    nc = bass.Bass("TRN2", target_bir_lowering=False)

    def din(name, shape, dt=F32):
        return nc.dram_tensor(name, list(shape), dt, kind="ExternalInput").ap()

    xT_in = din("xT", [D, T])
    outT = nc.dram_tensor("outT", [D, T], F32, kind="ExternalOutput").ap()
    Xs = nc.dram_tensor("Xs", [D, T], F32, kind="Internal").ap()
    cT_d = din("cT", [128, KC * NB])
    consts_d = din("consts", [128, NCONST * 128])
    sel_d = din("sel", [8, 1024])
    ada_w_d = din("ada_w", [4, D, 6 * D])
    ada_bT_d = din("ada_bT", [128, 4 * 48])
    fada_w_d = din("final_ada_w", [D, 2 * D])
    fada_bT_d = din("fada_bT", [128, 16])
    nw_d = din("nw", [128, 4 * 16 + 8])
    w_in_d = din("w_in", [4, D, DIN])
    cw_d = din("cw", [128, 4 * 48])
    dnw_d = din("dnw", [128, 4])
    alog_d = din("alog_bc", [128, 16])
    dtb_d = din("dtb_bc", [128, 16])
    lnw_d = din("gm_ln_w", [4, 512])
    lnb_d = din("gm_ln_b", [4, 512])
    wsT_d = din("wsT", [4, 128, 512])
    gsb_d = din("gsb", [4, 512])
    w_out_d = din("w_out", [4, D, D])
    fg_d = din("ffn_w_gate", [2, D, FF_DENSE])
    fu_d = din("ffn_w_up", [2, D, FF_DENSE])
    fd_d = din("ffn_w_down", [2, FF_DENSE, D])
    rt_d = din("moe_router", [2, D, NE])
    has_moe = any(l % 2 == 1 for l in layers)
    mg_d = din("moe_w_gate", [2, NE, D, FF_MOE] if has_moe else [1, 1, 1, 1])
    mu_d = din("moe_w_up", [2, NE, D, FF_MOE] if has_moe else [1, 1, 1, 1])
    md_d = din("moe_w_down", [2, NE, FF_MOE, D] if has_moe else [1, 1, 1, 1])

    with contextlib.ExitStack() as st:
        uid = [0]

        def uname(name):
            uid[0] += 1
            return "sb%d_%s" % (uid[0], name)

        def sb(name, shape, dt=F32):
            return st.enter_context(nc.sbuf_tensor(uname(name), list(shape), dt))

        P = Prog(nc, st)
        banks = [st.enter_context(nc.psum_tensor("bank%d" % i, [128, 512], F32)) for i in range(8)]
        bank_r = [Res(True) for _ in range(8)]

        cons = sb("cons", [128, NCONST, 128])
        cons_b = sb("cons_b", [128, 2, 128], BF16)
        onesrow_b = sb("onesrow_b", [1, 128], BF16)
        cT = sb("cT", [128, KC * NB])
        cact_b = sb("cact_b", [128, KC * NB], BF16)
        mod = sb("mod", [128, NB, 4 * 48 + 16])
        ada_bT = sb("ada_bT", [128, 4 * 48 + 16])
        nw = sb("nw", [128, 4 * 16 + 8])
        AB = sb("AB", [128, NB, (4 * 2 + 1) * 8])
        cw = sb("cw", [128, 4 * 48])
        dnw = sb("dnw", [128, 4])
        nA = sb("nA", [128, 16])
        dtb = sb("dtb", [128, 16])
        r_const = Res()

        def DMA(eng, out, in_, R=(), W=()):
            P.op(eng, lambda e: e.dma_start(out=out, in_=in_), R, W, dma=True)

        def MM(out, lhsT, rhs, start, stop, R, W):
            P.op("pe", lambda e: e.matmul(out, lhsT, rhs, start=start, stop=stop), R, W)

        def ACT(out, in_, func, R, W, scale=1.0, bias=0.0):
            P.op("act", lambda e: e.activation(out, in_, func, bias=bias, scale=scale), R, W)

        def TT(out, a, b, op, R, W, eng="dve"):
            P.op(eng, lambda e: e.tensor_tensor(out, a, b, op), R, W)

        def TS(out, a, s1, s2, op0, op1, R, W, eng="dve"):
            if s2 is None:
                P.op(eng, lambda e: e.tensor_scalar(out, a, s1, None, op0), R, W)
            else:
                P.op(eng, lambda e: e.tensor_scalar(out, a, s1, s2, op0, op1), R, W)

        def STT(out, a, s, b, op0, op1, R, W, eng="dve"):
            P.op(eng, lambda e: e.scalar_tensor_tensor(out, a, s, b, op0, op1), R, W)

        def CP(out, in_, R, W, eng="dve"):
            if eng == "act":
                P.op("act", lambda e: e.copy(out, in_), R, W)
            else:
                P.op(eng, lambda e: e.tensor_copy(out, in_), R, W)

        def RECIP(out, in_, R, W):
            P.op("dve", lambda e: e.reciprocal(out, in_), R, W)

        def MEMSET(ap, val, W, eng="pool"):
            P.op(eng, lambda e: e.memset(ap, val), (), W)

        DMA("sp", cons[:], consts_d.rearrange("p (a b) -> p a b", a=NCONST), W=[r_const])
        DMA("pool", cons_b[:], consts_d[:, 0:256].rearrange("p (a b) -> p a b", a=2), W=[r_const])
        DMA("sp", cT[:], cT_d, W=[r_const])
        DMA("sp", ada_bT[:, 0:192], ada_bT_d, W=[r_const])
        DMA("sp", ada_bT[:, 192:208], fada_bT_d, W=[r_const])
        DMA("sp", nw[:], nw_d, W=[r_const])
        DMA("sp", cw[:], cw_d, W=[r_const])
        DMA("sp", dnw[:], dnw_d, W=[r_const])
        DMA("sp", nA[:], alog_d, W=[r_const])
        DMA("sp", dtb[:], dtb_d, W=[r_const])
        MEMSET(onesrow_b[:], 1.0, [r_const])
        ACT(nA[:], nA[:], AF.Exp, [r_const], [r_const])
        TS(nA[:], nA[:], -1.0, None, ALU.mult, None, [r_const], [r_const])
        ACT(cact_b[:], cT[:], AF.Silu, [r_const], [r_const])

        ident_f = cons[:, C_ID, :]
        ones_f = cons[:, C_ONES, :]
        trils_f = cons[:, C_TRILS, :]
        triu_f = cons[:, C_TRIU, :]
        ident_b = cons_b[:, 0, :]
        ones_b = cons_b[:, 1, :]

        with contextlib.ExitStack() as ph:
            wbuf = [ph.enter_context(nc.sbuf_tensor(uname("adaw"), [128, KC, 1024], BF16)) for i in range(2)]
            wbuf_r = [Res(), Res()]
            mod_r = Res()
            pieces = []
            for li, l in enumerate(layers):
                for j in range(6):
                    pieces.append((ada_w_d[l, :, j * 1024:(j + 1) * 1024], li * 48 + j * 8))
            for j in range(2):
                pieces.append((fada_w_d[:, j * 1024:(j + 1) * 1024], 192 + j * 8))
            for pi, (src, col0) in enumerate(pieces):
                wb, wr = wbuf[pi % 2], wbuf_r[pi % 2]
                DMA("pool", wb[:], src.rearrange("(k p) n -> p k n", p=128), W=[wr])
                for oc in range(8):
                    bk = pi * 8 + oc
                    ps = banks[bk % 8][:, 0:NB]
                    pr = bank_r[bk % 8]
                    for k in range(KC):
                        MM(ps, wb[:, k, oc * 128:(oc + 1) * 128], cact_b[:, k * NB:(k + 1) * NB],
                           k == 0, k == KC - 1, [wr, r_const], [pr])
                    bcol = (layers[col0 // 48] * 48 + col0 % 48 + oc) if col0 < 192 else (192 + col0 - 192 + oc)
                    TS(mod[:, :, col0 + oc], ps, ada_bT[:, bcol:bcol + 1], None, ALU.add, None,
                       [pr, r_const], [mod_r])
            MEMSET(AB[:], 0.0, [mod_r])
            for li, l in enumerate(layers):
                for sub in range(2):
                    for b in range(NB):
                        sc = mod[:, b, li * 48 + (sub * 3 + 1) * 8: li * 48 + (sub * 3 + 2) * 8]
                        STT(AB[:, b, (li * 2 + sub) * 8:(li * 2 + sub + 1) * 8], sc, 1.0,
                            nw[:, l * 16 + sub * 8: l * 16 + sub * 8 + 8], ALU.add, ALU.mult, [mod_r, r_const], [mod_r])
            for b in range(NB):
                STT(AB[:, b, 64:72], mod[:, b, 200:208], 1.0, nw[:, 64:72], ALU.add, ALU.mult, [mod_r, r_const], [mod_r])
            TS(AB[:], AB[:], 32.0, None, ALU.mult, None, [mod_r], [mod_r])
            P.flush()

        def norm_mod(xt, xt_r, ncols, A, B, out_b, out_r, sq_ring, tmp_ring, rstd, rstd_r, ps, ps_r, out_f=None, out_f_r=None):
            for k in range(KC):
                sq, sq_r = sq_ring.get()
                ACT(sq[:, 0:ncols], xt[:, k, 0:ncols], AF.Square, [xt_r[k]], [sq_r])
                MM(ps[:, 0:ncols], ones_b, sq[:, 0:ncols], k == 0, k == KC - 1, [sq_r, r_const], [ps_r])
            ACT(rstd[:, 0:ncols], ps[:, 0:ncols], AF.Ln, [ps_r], [rstd_r], scale=1.0, bias=1024.0 * EPS)
            ACT(rstd[:, 0:ncols], rstd[:, 0:ncols], AF.Exp, [rstd_r], [rstd_r], scale=-0.5)
            for k in range(KC):
                tm, tm_r = tmp_ring.get()
                TT(tm[:, 0:ncols], xt[:, k, 0:ncols], rstd[:, 0:ncols], ALU.mult, [xt_r[k], rstd_r], [tm_r])
                if out_f is not None:
                    ACT(out_f[:, k, 0:ncols], tm[:, 0:ncols], AF.Identity, [tm_r], [out_f_r[k]],
                        scale=A[:, k:k + 1], bias=B[:, k:k + 1])
                ACT(out_b[:, k, 0:ncols], tm[:, 0:ncols], AF.Identity, [tm_r], [out_r[k]],
                    scale=A[:, k:k + 1], bias=B[:, k:k + 1])

        first = [True]

        def cur_src():
            return xT_in if first[0] else Xs

        def phase_A(li, l):
            TTK = 256
            NCH = TTK // 128
            src = cur_src()
            with contextlib.ExitStack() as ph:
                def pb(name, shape, dt=F32):
                    return ph.enter_context(nc.sbuf_tensor(uname(name), list(shape), dt))

                class _BankRing:
                    def __init__(self, ncol):
                        self.ncol = ncol

                    def get(self):
                        i = bring[0] % 8
                        bring[0] += 1
                        return (banks[i][:, 0:self.ncol], bank_r[i])
                bring = [0]
                wide = _BankRing(256)
                small = _BankRing(128)
                fullr = _BankRing(512)

                w_in_b = pb("w_in_b", [128, KC, DIN], BF16)
                w_out_b = pb("w_out_b", [128, KC, D], BF16)
                wsT_f = pb("wsT_f", [128, 512])
                wsT_b = pb("wsT_b", [128, 512], BF16)
                brow_b = pb("brow_b", [1, 512], BF16)
                lnw = pb("lnw", [128, 512])
                lnb = pb("lnb", [128, 512])
                r_w = Res()
                for k in range(KC):
                    DMA("pool", w_in_b[:, k, :], w_in_d[l, k * 128:(k + 1) * 128, :], W=[r_w])
                DMA("pool", w_out_b[:], w_out_d[l].rearrange("(k p) n -> p k n", p=128), W=[r_w])
                DMA("sp", wsT_f[:], wsT_d[l], W=[r_w])
                DMA("pool", brow_b[:], gsb_d[l:l + 1, :], W=[r_w])
                DMA("sp", lnw[:], lnw_d[l:l + 1, :].to_broadcast([128, 512]), W=[r_w])
                DMA("sp", lnb[:], lnb_d[l:l + 1, :].to_broadcast([128, 512]), W=[r_w])
                for g in range(4):
                    TT(wsT_b[:, g * 128:(g + 1) * 128], wsT_f[:, g * 128:(g + 1) * 128], triu_f, ALU.mult,
                       [r_w, r_const], [r_w])

                xt1 = (pb("xt", [128, KC, TTK]), [Res() for _ in range(KC)])
                xtL = [xt1, xt1]
                xo_ring = Ring([(pb("xo%d" % i, [128, TTK]), Res()) for i in range(3)])
                hT = pb("hT", [128, KC, TTK], BF16); hT_r = [Res() for _ in range(KC)]
                sq_ring = Ring([(pb("sqa%d" % i, [128, TTK], BF16), Res()) for i in range(3)])
                tmp_ring = Ring([(pb("tma%d" % i, [128, TTK]), Res()) for i in range(3)])
                rstd = pb("rstd", [128, TTK]); rstd_r = Res()
                pre = pb("pre", [128, 12, TTK + 3]); pre_r = [Res() for _ in range(12)]
                qs = pb("qs", [128, 4, TTK]); ks = pb("ks", [128, 4, TTK])
                vsL = [pb("vs_b%d" % i, [128, 4, TTK], BF16) for i in range(2)]
                qkvL = [[Res() for _ in range(12)] for _ in range(2)]
                qnL = [(pb("qn_b%d" % i, [128, 4, TTK], BF16), [Res() for _ in range(4)]) for i in range(2)]
                knL = [(pb("kn_b%d" % i, [128, 4, TTK], BF16), [Res() for _ in range(4)]) for i in range(2)]
                zsL = [(pb("zs%d" % i, [128, 4, TTK], BF16), [Res() for _ in range(4)]) for i in range(2)]
                gus = pb("gus", [128, 4, TTK], BF16); gus_r = [Res() for _ in range(4)]
                gvf_ring = Ring([(pb("gvf%d" % i, [128, 512]), Res()) for i in range(2)])
                gv_b = pb("gv_b", [128, NCH, 512], BF16); gv_r = [Res() for _ in range(NCH)]
                stat = pb("stat", [128, 8]); stat_r = Res()
                mixL = [(pb("mixT%d" % i, [128, KC, TTK], BF16), [Res() for _ in range(KC)]) for i in range(2)]
                oT = pb("oT", [128, 4, TTK]); oT_r = [Res() for _ in range(4)]
                ba = pb("ba", [128, NCH * 8]); ba_r = Res()
                smL = [tuple([pb("%s%d" % (nm_, i), [128, NCH * 4]) for nm_ in ("g_tm", "beta_tm", "G_tm", "e1", "e2", "eGl")]
                             + [Res(), Res()]) for i in range(2)]
                S_f = pb("S_f", [128, 4, 128]); S_b = pb("S_b", [128, 4, 128], BF16)
                S_r = [Res() for _ in range(4)]; Sb_r = [Res() for _ in range(4)]
                def hb(name, dt=F32):
                    return [(pb("%s%d" % (name, h), [128, 128], dt), Res()) for h in range(4 * NCH)]
                gbc = hb("gbc"); eG = hb("eG", BF16); dd = hb("dd", BF16); dt_ = hb("dt_", BF16)
                Am = hb("Am", BF16); AM = [hb("AM%d_" % lv, BF16) for lv in range(2)]
                U0 = hb("U0", BF16); U1 = hb("U1", BF16); T0 = hb("T0", BF16); T1 = hb("T1", BF16)
                P1 = hb("P1", BF16); attnT = hb("attnT", BF16); qd = hb("qd", BF16)
                kbd = hb("kbd", BF16); kdec = hb("kdec", BF16); vb = hb("vb", BF16)
                u_f = hb("u_f"); wT = hb("wT", BF16); vnew = hb("vnew", BF16)

                A1 = AB[:, :, (li * 2) * 8:(li * 2 + 1) * 8]
                c4 = l * 4

                def S1(seq, it, bi):
                    xt, xt_r = xtL[bi]; vs_b = vsL[bi]; qn_b, qn_r = qnL[bi]; kn_b, kn_r = knL[bi]
                    zs, zs_r = zsL[bi]; mixT, mix_r = mixL[bi]; qkv_r = qkvL[bi]
                    g_tm, beta_tm, G_tm, e1, e2, eGl, sm_r, sm2_r = smL[bi]
                    if it == 0:
                        for j in range(12):
                            MEMSET(pre[:, j, 0:3], 0.0, [pre_r[j]])
                    if True:
                        tok0 = seq * L + it * TTK
                        dsl = src[:, tok0:tok0 + TTK].rearrange("(k p) t -> p k t", p=128)
                        DMA("sp", xt[:], dsl, W=xt_r)
                        ps, ps_r = wide.get()
                        norm_mod(xt, xt_r, TTK, A1[:, seq, :], mod[:, seq, li * 48: li * 48 + 8], hT, hT_r,
                                 sq_ring, tmp_ring, rstd, rstd_r, ps, ps_r)
                        yield
                        def proj(col0):
                            ps, ps_r = wide.get()
                            for k in range(KC):
                                MM(ps, w_in_b[:, k, col0:col0 + 128], hT[:, k, :], k == 0, k == KC - 1,
                                   [r_w, hT_r[k]], [ps_r])
                            return ps, ps_r
                        for j in range(12):
                            ps, ps_r = proj(j * 128)
                            CP(pre[:, j, 3:3 + TTK], ps, [ps_r], [pre_r[j]], eng="act")
                            acc_t, acc_r = tmp_ring.get()
                            acc = acc_t[:]
                            cwj = cw[:, l * 48 + j * 4: l * 48 + j * 4 + 4]
                            TS(acc, pre[:, j, 0:TTK], cwj[:, 0:1], None, ALU.mult, None, [pre_r[j], r_const], [acc_r])
                            for tp in range(1, 4):
                                STT(acc, pre[:, j, tp:tp + TTK], cwj[:, tp:tp + 1], acc, ALU.mult, ALU.add,
                                    [pre_r[j], acc_r, r_const], [acc_r])
                            dst = (qs, ks, vs_b)[j // 4][:, j % 4, :]
                            ACT(dst, acc, AF.Silu, [acc_r], [qkv_r[j]])
                            CP(pre[:, j, 0:3], pre[:, j, TTK:TTK + 3], [pre_r[j]], [pre_r[j]], eng="pool")
                            yield
                        for h in range(4):
                            ps, ps_r = proj(OFF_Z + h * 128)
                            ACT(zs[:, h, :], ps, AF.Silu, [ps_r], [zs_r[h]])
                            yield
                        for g in range(4):
                            ps, ps_r = proj(OFF_GU + g * 128)
                            ACT(gus[:, g, :], ps, AF.Gelu_apprx_tanh, [ps_r], [gus_r[g]])
                            yield
                        psba, psba_r = small.get()
                        for c in range(NCH):
                            for k in range(KC):
                                MM(psba[:, c * 8:(c + 1) * 8], hT[:, k, c * 128:(c + 1) * 128],
                                   w_in_b[:, k, OFF_BA:OFF_BA + 8], k == 0, k == KC - 1, [r_w, hT_r[k]], [psba_r])
                        CP(ba[:], psba[:, 0:NCH * 8], [psba_r], [ba_r])
                        for c in range(NCH):
                            ACT(beta_tm[:, c * 4:c * 4 + 4], ba[:, c * 8:c * 8 + 4], AF.Sigmoid, [ba_r], [sm_r])
                            TT(g_tm[:, c * 4:c * 4 + 4], ba[:, c * 8 + 4:c * 8 + 8], dtb[:, c4:c4 + 4], ALU.add, [ba_r, r_const], [sm_r])
                        g2d = g_tm[:]
                        ACT(g2d, g2d, AF.Exp, [sm_r], [sm_r])
                        ACT(g2d, g2d, AF.Ln, [sm_r], [sm_r], scale=1.0, bias=1.0)
                        for c in range(NCH):
                            TT(g_tm[:, c * 4:c * 4 + 4], g_tm[:, c * 4:c * 4 + 4], nA[:, c4:c4 + 4], ALU.mult, [sm_r, r_const], [sm_r])
                        for c in range(NCH):
                            psg, psg_r = fullr.get()
                            for k in range(KC):
                                MM(psg[:], hT[:, k, c * 128:(c + 1) * 128], w_in_b[:, k, OFF_GV:OFF_GV + 512],
                                   k == 0, k == KC - 1, [r_w, hT_r[k]], [psg_r])
                            gvf, gvf_r = gvf_ring.get()
                            ACT(gvf[:], psg[:], AF.Gelu_apprx_tanh, [psg_r], [gvf_r])
                            P.op("dve", lambda e, gvf=gvf, c=c: e.reduce_sum(stat[:, c:c + 1], gvf[:], AX.X), [gvf_r], [stat_r])
                            TS(stat[:, c:c + 1], stat[:, c:c + 1], 1.0 / 512, None, ALU.mult, None, [stat_r], [stat_r])
                            TS(gvf[:], gvf[:], stat[:, c:c + 1], None, ALU.subtract, None, [gvf_r, stat_r], [gvf_r])
                            sq2, sq2_r = gvf_ring.get()
                            TT(sq2[:], gvf[:], gvf[:], ALU.mult, [gvf_r], [sq2_r])
                            P.op("dve", lambda e, sq2=sq2, c=c: e.reduce_sum(stat[:, 4 + c:5 + c], sq2[:], AX.X), [sq2_r], [stat_r])
                            ACT(stat[:, 4 + c:5 + c], stat[:, 4 + c:5 + c], AF.Ln, [stat_r], [stat_r], scale=1.0 / 512, bias=EPS)
                            ACT(stat[:, 4 + c:5 + c], stat[:, 4 + c:5 + c], AF.Exp, [stat_r], [stat_r], scale=-0.5)
                            STT(gvf[:], gvf[:], stat[:, 4 + c:5 + c], lnw[:], ALU.mult, ALU.mult, [gvf_r, stat_r, r_w], [gvf_r])
                            TT(gv_b[:, c, :], gvf[:], lnb[:], ALU.add, [gvf_r, r_w], [gv_r[c]])
                            yield
                        for g in range(4):
                            ps, ps_r = wide.get()
                            for c in range(NCH):
                                MM(ps[:, c * 128:(c + 1) * 128], gv_b[:, c, g * 128:(g + 1) * 128],
                                   wsT_b[:, g * 128:(g + 1) * 128], True, False, [gv_r[c], r_w], [ps_r])
                                MM(ps[:, c * 128:(c + 1) * 128], onesrow_b[0:1, :], brow_b[0:1, g * 128:(g + 1) * 128],
                                   False, True, [r_const, r_w], [ps_r])
                            TT(mixT[:, 4 + g, :], ps, gus[:, g, :], ALU.mult, [ps_r, gus_r[g]], [mix_r[4 + g]])
                            yield
                        for h in range(4):
                            for (srcb, r_i, dstb, dst_r, scl) in ((qs, h, qn_b, qn_r, 128.0 ** -0.5), (ks, 4 + h, kn_b, kn_r, 1.0)):
                                sq, sq_r = sq_ring.get()
                                ACT(sq[:], srcb[:, h, :], AF.Square, [qkv_r[r_i]], [sq_r])
                                ps, ps_r = wide.get()
                                MM(ps, ones_b, sq[:], True, True, [sq_r, r_const], [ps_r])
                                tm, tm_r = tmp_ring.get()
                                ACT(tm[:], ps, AF.Ln, [ps_r], [tm_r], scale=1.0, bias=EPS)
                                ACT(tm[:], tm[:], AF.Exp, [tm_r], [tm_r], scale=-0.5)
                                STT(dstb[:, h, :], srcb[:, h, :], scl, tm[:], ALU.mult, ALU.mult, [qkv_r[r_i], tm_r], [dst_r[h]])
                            yield
                        psl, psl_r = small.get()
                        for c in range(NCH):
                            MM(psl[:, c * 4:(c + 1) * 4], ones_f, g_tm[:, c * 4:c * 4 + 4], True, True, [sm_r, r_const], [psl_r])
                            MM(psl[:, 16 + c * 4:16 + (c + 1) * 4], triu_f, g_tm[:, c * 4:c * 4 + 4], True, True, [sm_r, r_const], [psl_r])
                        CP(G_tm[:], psl[:, 16:16 + NCH * 4], [psl_r], [sm2_r])
                        ACT(eGl[:], psl[:, 0:NCH * 4], AF.Exp, [psl_r], [sm2_r])
                        TT(e2[:], psl[:, 0:NCH * 4], G_tm[:],
                           ALU.subtract, [psl_r, sm2_r], [sm2_r])
                        ACT(e2[:], e2[:], AF.Exp, [sm2_r], [sm2_r])
                        ACT(e1[:], G_tm[:], AF.Exp, [sm2_r], [sm2_r])
                        TT(e1[:], e1[:],
                           beta_tm[:], ALU.mult, [sm2_r, sm_r], [sm2_r])
                        yield

                def S2(seq, it, bi):
                    xt, xt_r = xtL[bi]; vs_b = vsL[bi]; qn_b, qn_r = qnL[bi]; kn_b, kn_r = knL[bi]
                    zs, zs_r = zsL[bi]; mixT, mix_r = mixL[bi]; qkv_r = qkvL[bi]
                    g_tm, beta_tm, G_tm, e1, e2, eGl, sm_r, sm2_r = smL[bi]
                    if it == 0:
                        for h in range(4):
                            MEMSET(S_f[:, h, :], 0.0, [S_r[h]])
                            MEMSET(S_b[:, h, :], 0.0, [Sb_r[h]])
                    if True:
                        tok0 = seq * L + it * TTK
                        CHN = [(c, h) for c in range(NCH) for h in range(4)]
                        def csl(c):
                            return slice(c * 128, (c + 1) * 128)
                        def col(c, h):
                            return slice(c * 4 + h, c * 4 + h + 1)
                        psG = {}
                        for (c, h) in CHN:
                            i = c * 4 + h
                            TS(gbc[i][0][:], ones_f, g_tm[:, col(c, h)], None, ALU.mult, None, [sm_r, r_const], [gbc[i][1]])
                            psG[i] = small.get()
                            MM(psG[i][0], gbc[i][0][:], triu_f, True, True, [gbc[i][1], r_const], [psG[i][1]])
                        for (c, h) in CHN:
                            i = c * 4 + h
                            pg, pg_r = psG[i]
                            Gc = G_tm[:, col(c, h)]
                            ACT(eG[i][0][:], pg, AF.Exp, [pg_r], [eG[i][1]])
                            TS(dd[i][0][:], pg, Gc, 0.0, ALU.subtract, ALU.max, [pg_r, sm2_r], [dd[i][1]])
                            TS(dt_[i][0][:], pg, Gc, 0.0, ALU.subtract, ALU.min, [pg_r, sm2_r], [dt_[i][1]])
                            ACT(dd[i][0][:], dd[i][0][:], AF.Exp, [dd[i][1]], [dd[i][1]], scale=-1.0)
                            ACT(dt_[i][0][:], dt_[i][0][:], AF.Exp, [dt_[i][1]], [dt_[i][1]])
                            TT(dd[i][0][:], dd[i][0][:], trils_f, ALU.mult, [dd[i][1], r_const], [dd[i][1]], eng="pool")
                            TT(dt_[i][0][:], dt_[i][0][:], triu_f, ALU.mult, [dt_[i][1], r_const], [dt_[i][1]], eng="pool")
                            TT(qd[i][0][:], qn_b[:, h, csl(c)], eG[i][0][:], ALU.mult, [qn_r[h], eG[i][1]], [qd[i][1]])
                            yield
                        for (c, h) in CHN:
                            i = c * 4 + h
                            cs = csl(c)
                            pk, pk_r = small.get()
                            MM(pk, kn_b[:, h, cs], kn_b[:, h, cs], True, True, [kn_r[h]], [pk_r])
                            STT(Am[i][0][:], pk, beta_tm[:, col(c, h)], dd[i][0][:], ALU.mult, ALU.mult,
                                [pk_r, sm_r, dd[i][1]], [Am[i][1]])
                            pq, pq_r = small.get()
                            MM(pq, kn_b[:, h, cs], qn_b[:, h, cs], True, True, [kn_r[h], qn_r[h]], [pq_r])
                            TT(attnT[i][0][:], pq, dt_[i][0][:], ALU.mult, [pq_r, dt_[i][1]], [attnT[i][1]])
                            pt, pt_r = small.get()
                            MM(pt, kn_b[:, h, cs], ident_b, True, True, [kn_r[h], r_const], [pt_r])
                            TS(kbd[i][0][:], pt, e1[:, col(c, h)], None, ALU.mult, None, [pt_r, sm2_r], [kbd[i][1]])
                            ACT(kdec[i][0][:], pt, AF.Identity, [pt_r, sm2_r], [kdec[i][1]], scale=e2[:, col(c, h)])
                            pv, pv_r = small.get()
                            MM(pv, vs_b[:, h, cs], ident_b, True, True, [qkv_r[8 + h], r_const], [pv_r])
                            TS(vb[i][0][:], pv, beta_tm[:, col(c, h)], None, ALU.mult, None, [pv_r, sm_r], [vb[i][1]])
                            yield
                        Ucur = {}; Tcur = {}
                        for (c, h) in CHN:
                            i = c * 4 + h
                            a0, a0_r = AM[0][i]
                            TT(a0[:], Am[i][0][:], cons[:, C_M0, :], ALU.mult, [Am[i][1], r_const], [a0_r], eng="pool")
                            pt, pt_r = small.get()
                            MM(pt, a0[:], ident_b, True, True, [a0_r, r_const], [pt_r])
                            TT(U0[i][0][:], ident_f, pt, ALU.subtract, [pt_r, r_const], [U0[i][1]])
                            TT(T0[i][0][:], ident_f, a0[:], ALU.subtract, [a0_r, r_const], [T0[i][1]], eng="pool")
                            Ucur[i] = U0[i]; Tcur[i] = T0[i]
                        yield
                        for lv in range(1, 7):
                            pp = {}
                            for i in range(NCH * 4):
                                al, al_r = AM[lv % 2][i]
                                TT(al[:], Am[i][0][:], cons[:, C_M0 + lv, :], ALU.mult, [Am[i][1], r_const], [al_r], eng="pool")
                                pp[i] = small.get()
                                MM(pp[i][0], al[:], Ucur[i][0][:], True, True, [al_r, Ucur[i][1]], [pp[i][1]])
                            for i in range(NCH * 4):
                                CP(P1[i][0][:], pp[i][0], [pp[i][1]], [P1[i][1]], eng="act")
                            yield
                            px = {}
                            for i in range(NCH * 4):
                                px[i] = small.get()
                                MM(px[i][0], Tcur[i][0][:], P1[i][0][:], True, True, [Tcur[i][1], P1[i][1]], [px[i][1]])
                            for i in range(NCH * 4):
                                Un = U1[i] if Ucur[i] is U0[i] else U0[i]
                                TT(Un[0][:], Ucur[i][0][:], px[i][0], ALU.subtract, [Ucur[i][1], px[i][1]], [Un[1]])
                                Ucur[i] = Un
                            yield
                            if lv < 6:
                                ptt = {}
                                for i in range(NCH * 4):
                                    ptt[i] = small.get()
                                    MM(ptt[i][0], Ucur[i][0][:], ident_b, True, True, [Ucur[i][1], r_const], [ptt[i][1]])
                                for i in range(NCH * 4):
                                    Tn = T1[i] if Tcur[i] is T0[i] else T0[i]
                                    CP(Tn[0][:], ptt[i][0], [ptt[i][1]], [Tn[1]], eng="act")
                                    Tcur[i] = Tn
                                yield
                        for i in range(NCH * 4):
                            pu, pu_r = small.get()
                            MM(pu, Ucur[i][0][:], vb[i][0][:], True, True, [Ucur[i][1], vb[i][1]], [pu_r])
                            CP(u_f[i][0][:], pu, [pu_r], [u_f[i][1]], eng="act")
                            pw, pw_r = small.get()
                            MM(pw, kbd[i][0][:], Ucur[i][0][:], True, True, [kbd[i][1], Ucur[i][1]], [pw_r])
                            CP(wT[i][0][:], pw, [pw_r], [wT[i][1]])
                            yield
                        for c in range(NCH):
                            cs = csl(c)
                            pws = {}
                            for h in range(4):
                                i = c * 4 + h
                                pws[h] = small.get()
                                MM(pws[h][0], wT[i][0][:], S_b[:, h, :], True, True, [wT[i][1], Sb_r[h]], [pws[h][1]])
                            for h in range(4):
                                i = c * 4 + h
                                TT(vnew[i][0][:], u_f[i][0][:], pws[h][0], ALU.subtract, [u_f[i][1], pws[h][1]], [vnew[i][1]])
                            yield
                            for h in range(4):
                                i = c * 4 + h
                                po, po_r = small.get()
                                MM(po, S_b[:, h, :], qd[i][0][:], True, False, [Sb_r[h], qd[i][1]], [po_r])
                                MM(po, vnew[i][0][:], attnT[i][0][:], False, True, [vnew[i][1], attnT[i][1]], [po_r])
                                CP(oT[:, h, cs], po, [po_r], [oT_r[h]], eng="act")
                                pS, pS_r = small.get()
                                MM(pS, kdec[i][0][:], vnew[i][0][:], True, True, [kdec[i][1], vnew[i][1]], [pS_r])
                                STT(S_f[:, h, :], S_f[:, h, :], eGl[:, col(c, h)], pS, ALU.mult, ALU.add,
                                    [S_r[h], sm2_r, pS_r], [S_r[h]])
                                CP(S_b[:, h, :], S_f[:, h, :], [S_r[h]], [Sb_r[h]], eng="act")
                                yield
                        for h in range(4):
                            sq, sq_r = sq_ring.get()
                            ACT(sq[:], oT[:, h, :], AF.Square, [oT_r[h]], [sq_r])
                            ps, ps_r = wide.get()
                            MM(ps, ones_b, sq[:], True, True, [sq_r, r_const], [ps_r])
                            tm, tm_r = tmp_ring.get()
                            ACT(tm[:], ps, AF.Ln, [ps_r], [tm_r], scale=1.0 / 128, bias=EPS)
                            ACT(tm[:], tm[:], AF.Exp, [tm_r], [tm_r], scale=-0.5)
                            TT(tm[:], tm[:], oT[:, h, :], ALU.mult, [tm_r, oT_r[h]], [tm_r])
                            STT(mixT[:, h, :], tm[:], dnw[:, l:l + 1], zs[:, h, :], ALU.mult, ALU.mult,
                                [tm_r, r_const, zs_r[h]], [mix_r[h]])
                            yield
                        for oc in range(KC):
                            ps, ps_r = wide.get()
                            for k in range(KC):
                                MM(ps, w_out_b[:, k, oc * 128:(oc + 1) * 128], mixT[:, k, :], k == 0, k == KC - 1,
                                   [r_w, mix_r[k]], [ps_r])
                            g1 = mod[:, seq, li * 48 + 16 + oc: li * 48 + 17 + oc]
                            xo, xo_r = xo_ring.get()
                            DMA("sp", xo[:], src[oc * 128:(oc + 1) * 128, tok0:tok0 + TTK], W=[xo_r])
                            STT(xo[:], ps, g1, xo[:], ALU.mult, ALU.add, [ps_r, xo_r], [xo_r])
                            DMA("sp", Xs[oc * 128:(oc + 1) * 128, tok0:tok0 + TTK], xo[:], R=[xo_r])
                            yield
                        yield

                def interleave(ga, gb, ra=1, rb=1):
                    gens = [(ga, ra), (gb, rb)]
                    alive = [g is not None for g, _ in gens]
                    while any(alive):
                        for gi, (g, r_) in enumerate(gens):
                            if not alive[gi]:
                                continue
                            for _ in range(r_):
                                try:
                                    next(g)
                                except StopIteration:
                                    alive[gi] = False
                                    break

                tiles = [(sq_, it_) for sq_ in range(NB) for it_ in range(L // TTK)]
                for _ in S1(tiles[0][0], tiles[0][1], 0):
                    pass
                for n_, (sq_, it_) in enumerate(tiles):
                    g2 = S2(sq_, it_, n_ % 2)
                    g1 = S1(tiles[n_ + 1][0], tiles[n_ + 1][1], (n_ + 1) % 2) if n_ + 1 < len(tiles) else None
                    interleave(g2, g1, 2, 1)
                first[0] = False
                P.flush()

        def phase_B(li, l):
            moe = (l % 2 == 1)
            j = l // 2
            FF = FF_MOE if moe else FF_DENSE
            nexp = NE if moe else 1
            TB = min(1024, L)
            NT = TB // 512
            src = cur_src()
            with contextlib.ExitStack() as ph:
                def pb(name, shape, dt=F32):
                    return ph.enter_context(nc.sbuf_tensor(uname(name), list(shape), dt))
                pbank = Ring([(banks[i], bank_r[i]) for i in range(8)])
                h2b = pb("h2b", [128, KC, TB], BF16); h2b_r = [[Res() for _ in range(KC)] for _ in range(NT)]
                yacc = pb("yacc", [128, KC, TB]); y_r = [[Res() for _ in range(KC)] for _ in range(NT)]
                xt2 = [pb("xtb%d" % i, [128, KC, 512]) for i in range(2)]
                xt2_r = [[Res() for _ in range(KC)] for _ in range(2)]
                sq_ring = Ring([(pb("sqb%d" % i, [128, 512], BF16), Res()) for i in range(3)])
                tmp_ring = Ring([(pb("tmb%d" % i, [128, 512]), Res()) for i in range(3)])
                sg_ring = Ring([(pb("sg%d" % i, [128, 512]), Res()) for i in range(3)])
                hid = [[(pb("hid%d_%d" % (i, f), [128, 512], BF16), Res()) for f in range(4)] for i in range(2)]
                rstd = pb("rstdb", [128, 512]); rstd_r = Res()
                wg = [pb("wg%d" % i, [128, KC, 512], BF16) for i in range(2)]
                wu = [pb("wu%d" % i, [128, KC, 512], BF16) for i in range(2)]
                wd = [pb("wd%d" % i, [128, 4, D], BF16) for i in range(2)]
                w_r = [Res(), Res()]
                if moe:
                    h2f = pb("h2f", [128, KC, 512]); h2f_r = [Res() for _ in range(KC)]
                    wr = pb("wr", [128, KC, NE]); wr_r = Res()
                    DMA("sp", wr[:], rt_d[j].rearrange("(k p) e -> p k e", p=128), W=[wr_r])
                    sel = pb("sel", [8, 1024]); sel_r = Res()
                    DMA("sp", sel[:], sel_d, W=[sel_r])
                    comb = pb("comb", [128, TB // 128, NE]); comb_r = Res()
                    combT = pb("combT", [8, TB]); combT_r = Res()
                    cbc = [pb("cbc%d" % i, [128, TB]) for i in range(2)]
                    cbc_r = [[Res() for _ in range(NT)] for _ in range(2)]
                    lg = pb("lg", [128, NE]); lg2 = pb("lg2", [128, NE]); mk1 = pb("mk1", [128, NE]); mk2 = pb("mk2", [128, NE])
                    m12 = pb("m12", [128, 4]); lg_r = Res()
                A2 = AB[:, :, (li * 2 + 1) * 8:(li * 2 + 2) * 8]
                pieces = []
                for e in range(nexp):
                    f0 = 0
                    while f0 < FF:
                        pw_ = min(512, FF - f0)
                        pieces.append((e, f0, pw_))
                        f0 += pw_

                def load_piece(pi):
                    e, f0, pw_ = pieces[pi]
                    bi = pi % 2
                    if moe:
                        g_src, u_src, d_src = mg_d[j, e], mu_d[j, e], md_d[j, e]
                    else:
                        g_src, u_src, d_src = fg_d[j], fu_d[j], fd_d[j]
                    DMA("pool", wg[bi][:, :, 0:pw_], g_src[:, f0:f0 + pw_].rearrange("(k p) n -> p k n", p=128), W=[w_r[bi]])
                    DMA("pool", wu[bi][:, :, 0:pw_], u_src[:, f0:f0 + pw_].rearrange("(k p) n -> p k n", p=128), W=[w_r[bi]])
                    DMA("pool", wd[bi][:, 0:pw_ // 128, :], d_src[f0:f0 + pw_, :].rearrange("(f p) n -> p f n", p=128), W=[w_r[bi]])

                xload = [0]
                for blk in range(T // TB):
                    seq = (blk * TB) // L
                    btok = blk * TB
                    load_piece(0)
                    for t in range(NT):
                        xb = xload[0] % 2; xload[0] += 1
                        xt, xt_r = xt2[xb], xt2_r[xb]
                        tok0 = btok + t * 512
                        DMA("sp", xt[:], src[:, tok0:tok0 + 512].rearrange("(k p) t -> p k t", p=128), W=xt_r)
                        ps, ps_r = pbank.get()
                        class _V:
                            def __getitem__(self, idx):
                                p_, k_, c_ = idx
                                return h2b[p_, k_, t * 512 + (c_.start or 0): t * 512 + (c_.stop or 512)]
                        norm_mod(xt, xt_r, 512, A2[:, seq, :], mod[:, seq, li * 48 + 24: li * 48 + 32], _V(), h2b_r[t],
                                 sq_ring, tmp_ring, rstd, rstd_r, ps, ps_r,
                                 out_f=(h2f if moe else None), out_f_r=(h2f_r if moe else None))
                        if moe:
                            P.mute = CFG.get('moe_upto', 99) < 1
                            for c in range(4):
                                gc = t * 4 + c
                                pl, pl_r = pbank.get()
                                for k in range(KC):
                                    MM(pl[:, 0:NE], h2f[:, k, c * 128:(c + 1) * 128], wr[:, k, :], k == 0, k == KC - 1,
                                       [h2f_r[k], wr_r], [pl_r])
                                CP(lg[:], pl[:, 0:NE], [pl_r], [lg_r])
                                P.op("dve", lambda e: e.reduce_max(m12[:, 0:1], lg[:], AX.X), [lg_r], [lg_r])
                                TS(mk1[:], lg[:], m12[:, 0:1], None, ALU.is_equal, None, [lg_r], [lg_r])
                                STT(lg2[:], mk1[:], -1e30, lg[:], ALU.mult, ALU.add, [lg_r], [lg_r])
                                P.op("dve", lambda e: e.reduce_max(m12[:, 1:2], lg2[:], AX.X), [lg_r], [lg_r])
                                TS(mk2[:], lg2[:], m12[:, 1:2], None, ALU.is_equal, None, [lg_r], [lg_r])
                                TT(m12[:, 2:3], m12[:, 1:2], m12[:, 0:1], ALU.subtract, [lg_r], [lg_r])
                                ACT(m12[:, 2:3], m12[:, 2:3], AF.Exp, [lg_r], [lg_r])
                                TS(m12[:, 2:3], m12[:, 2:3], 1.0, None, ALU.add, None, [lg_r], [lg_r])
                                RECIP(m12[:, 2:3], m12[:, 2:3], [lg_r], [lg_r])
                                TS(m12[:, 3:4], m12[:, 2:3], -1.0, 1.0, ALU.mult, ALU.add, [lg_r], [lg_r])
                                TS(mk1[:], mk1[:], m12[:, 2:3], None, ALU.mult, None, [lg_r], [lg_r])
                                STT(comb[:, gc, :], mk2[:], m12[:, 3:4], mk1[:], ALU.mult, ALU.add, [lg_r], [comb_r])
                                P.mute = CFG.get('moe_upto', 99) < 2
                                pc, pc_r = pbank.get()
                                MM(pc[0:8, 0:128], comb[:, gc, :], ident_f, True, True, [comb_r, r_const], [pc_r])
                                CP(combT[:, gc * 128:(gc + 1) * 128], pc[0:8, 0:128], [pc_r], [combT_r])
                    P.mute = False
                    first_acc = True
                    for pi, (e, f0, pw_) in enumerate(pieces):
                        if pi + 1 < len(pieces):
                            load_piece(pi + 1)
                        bi = pi % 2
                        nf = pw_ // 128
                        if moe and f0 == 0:
                            P.mute = CFG.get('moe_upto', 99) < 3
                            for t in range(NT):
                                pc, pc_r = pbank.get()
                                MM(pc[:], sel[0:8, e * 128:(e + 1) * 128], combT[:, t * 512:(t + 1) * 512], True, True,
                                   [sel_r, combT_r], [pc_r])
                                CP(cbc[e % 2][:, t * 512:(t + 1) * 512], pc[:], [pc_r], [cbc_r[e % 2][t]], eng="act")
                        P.mute = False
                        for t in range(NT):
                            ts_ = slice(t * 512, (t + 1) * 512)
                            hb_ = hid[(pi * NT + t) % 2]
                            for f in range(nf):
                                pg, pg_r = pbank.get()
                                for k in range(KC):
                                    MM(pg[:], wg[bi][:, k, f * 128:(f + 1) * 128], h2b[:, k, ts_], k == 0, k == KC - 1,
                                       [w_r[bi], h2b_r[t][k]], [pg_r])
                                pu, pu_r = pbank.get()
                                for k in range(KC):
                                    MM(pu[:], wu[bi][:, k, f * 128:(f + 1) * 128], h2b[:, k, ts_], k == 0, k == KC - 1,
                                       [w_r[bi], h2b_r[t][k]], [pu_r])
                                sg, sg_r = sg_ring.get()
                                ACT(sg[:], pg[:], AF.Silu, [pg_r], [sg_r])
                                if moe:
                                    P.mute = CFG.get('moe_upto', 99) < 4
                                    TT(sg[:], sg[:], cbc[e % 2][:, ts_], ALU.mult, [sg_r, cbc_r[e % 2][t]], [sg_r])
                                P.mute = False
                                TT(hb_[f][0][:], pu[:], sg[:], ALU.mult, [pu_r, sg_r], [hb_[f][1]])
                            for oc in range(KC):
                                py, py_r = pbank.get()
                                for f in range(nf):
                                    MM(py[:], wd[bi][:, f, oc * 128:(oc + 1) * 128], hb_[f][0][:], f == 0, f == nf - 1,
                                       [w_r[bi], hb_[f][1]], [py_r])
                                if first_acc:
                                    CP(yacc[:, oc, ts_], py[:], [py_r], [y_r[t][oc]], eng="act")
                                else:
                                    TT(yacc[:, oc, ts_], yacc[:, oc, ts_], py[:], ALU.add, [y_r[t][oc], py_r], [y_r[t][oc]])
                        first_acc = False
                    for t in range(NT):
                        xb = xload[0] % 2; xload[0] += 1
                        xt, xt_r = xt2[xb], xt2_r[xb]
                        tok0 = btok + t * 512
                        DMA("sp", xt[:], src[:, tok0:tok0 + 512].rearrange("(k p) t -> p k t", p=128), W=xt_r)
                        for oc in range(KC):
                            g2 = mod[:, seq, li * 48 + 40 + oc: li * 48 + 41 + oc]
                            STT(xt[:, oc, :], yacc[:, oc, t * 512:(t + 1) * 512], g2, xt[:, oc, :], ALU.mult, ALU.add,
                                [y_r[t][oc], xt_r[oc]], [xt_r[oc]])
                        DMA("sp", Xs[:, tok0:tok0 + 512].rearrange("(k p) t -> p k t", p=128), xt[:], R=xt_r)
                first[0] = False
                P.flush()

        def phase_final():
            src = cur_src()
            with contextlib.ExitStack() as ph:
                def pb(name, shape, dt=F32):
                    return ph.enter_context(nc.sbuf_tensor(uname(name), list(shape), dt))
                pbank = Ring([(banks[i], bank_r[i]) for i in range(8)])
                xt2 = [pb("xtf%d" % i, [128, KC, 512]) for i in range(2)]
                xt2_r = [[Res() for _ in range(KC)] for _ in range(2)]
                ot2 = [pb("otf%d" % i, [128, KC, 512]) for i in range(2)]
                ot2_r = [[Res() for _ in range(KC)] for _ in range(2)]
                sq_ring = Ring([(pb("sqf%d" % i, [128, 512], BF16), Res()) for i in range(3)])
                tmp_ring = Ring([(pb("tmf%d" % i, [128, 512]), Res()) for i in range(3)])
                rstd = pb("rstdf", [128, 512]); rstd_r = Res()
                Af = AB[:, :, 64:72]
                for t in range(T // 512):
                    seq = (t * 512) // L
                    xt, xt_r = xt2[t % 2], xt2_r[t % 2]
                    ot, ot_r = ot2[t % 2], ot2_r[t % 2]
                    tok0 = t * 512
                    DMA("sp", xt[:], src[:, tok0:tok0 + 512].rearrange("(k p) t -> p k t", p=128), W=xt_r)
                    ps, ps_r = pbank.get()
                    for k in range(KC):
                        sq, sq_r = sq_ring.get()
                        ACT(sq[:], xt[:, k, :], AF.Square, [xt_r[k]], [sq_r])
                        MM(ps[:], ones_b, sq[:], k == 0, k == KC - 1, [sq_r, r_const], [ps_r])
                    ACT(rstd[:], ps[:], AF.Ln, [ps_r], [rstd_r], scale=1.0, bias=1024.0 * EPS)
                    ACT(rstd[:], rstd[:], AF.Exp, [rstd_r], [rstd_r], scale=-0.5)
                    for k in range(KC):
                        tm, tm_r = tmp_ring.get()
                        TT(tm[:], xt[:, k, :], rstd[:], ALU.mult, [xt_r[k], rstd_r], [tm_r])
                        ACT(ot[:, k, :], tm[:], AF.Identity, [tm_r], [ot_r[k]],
                            scale=Af[:, seq, k:k + 1], bias=mod[:, seq, 192 + k:193 + k])
                    DMA("sp", outT[:, tok0:tok0 + 512].rearrange("(k p) t -> p k t", p=128), ot[:], R=ot_r)
                P.flush()

        for li, l in enumerate(layers):
            phase_A(li, l)
            if CFG.get('upto', 99) >= 9:
                phase_B(li, l)
        phase_final()
    return nc


_PROG_CACHE = {}


def kernel(x, c, ada_w, ada_b, norm1_w, norm2_w, w_in, conv_w, a_log, dt_bias,
           dn_norm_w, gm_ln_w, gm_ln_b, gm_spatial_w, gm_spatial_b, w_out,
           ffn_w_gate, ffn_w_up, ffn_w_down, moe_router, moe_w_gate, moe_w_up,
           moe_w_down, final_ada_w, final_ada_b, final_norm_w):
    f = lambda a: np.ascontiguousarray(np.asarray(a, dtype=np.float32))
    x = f(x); c = f(c)
    B, L, _ = x.shape
    n_cores = CFG["n_cores"]
    NB = B // n_cores
    layers = list(CFG["layers"])
    key = (L, NB, tuple(layers))
    if key not in _PROG_CACHE:
        import time as _t, sys as _s
        _t0 = _t.time()
        _PROG_CACHE[key] = build(L, NB, layers)
        print("[kernel] build %.1fs" % (_t.time() - _t0), file=_s.stderr)
    nc = _PROG_CACHE[key]
    cons, sel = _consts()

    def pm(v, nchunk):
        v = f(v)
        lead = v.shape[:-1]
        return np.ascontiguousarray(np.moveaxis(v.reshape(*lead, nchunk, 128), -1, 0))

    ada_bT = pm(ada_b, 48).reshape(128, 4 * 48)
    fada_bT = pm(final_ada_b, 16).reshape(128, 16)
    nw = np.concatenate([np.stack([pm(norm1_w, 8), pm(norm2_w, 8)], axis=2).reshape(128, 64),
                         pm(final_norm_w, 8).reshape(128, 8)], axis=1)
    cwp = np.ascontiguousarray(np.transpose(pm(conv_w, 12), (0, 1, 3, 2))).reshape(128, 4 * 48)
    dnw = np.ascontiguousarray(f(dn_norm_w).T)
    alog_bc = np.ascontiguousarray(np.broadcast_to(f(a_log).reshape(1, 16), (128, 16)))
    dtb_bc = np.ascontiguousarray(np.broadcast_to(f(dt_bias).reshape(1, 16), (128, 16)))
    wsT = np.ascontiguousarray(np.transpose(f(gm_spatial_w), (0, 3, 1, 2))).reshape(4, 128, 512)
    gsb = f(gm_spatial_b).reshape(4, 512)
    shared = {
        "consts": cons.reshape(128, NCONST * 128), "sel": sel,
        "ada_w": f(ada_w), "ada_bT": ada_bT, "final_ada_w": f(final_ada_w), "fada_bT": fada_bT, "nw": nw,
        "w_in": f(w_in), "cw": cwp, "dnw": dnw, "alog_bc": alog_bc, "dtb_bc": dtb_bc,
        "gm_ln_w": f(gm_ln_w), "gm_ln_b": f(gm_ln_b), "wsT": wsT, "gsb": gsb, "w_out": f(w_out),
        "ffn_w_gate": f(ffn_w_gate), "ffn_w_up": f(ffn_w_up), "ffn_w_down": f(ffn_w_down),
        "moe_router": f(moe_router), "moe_w_gate": f(moe_w_gate), "moe_w_up": f(moe_w_up), "moe_w_down": f(moe_w_down),
    }
    if not any(l % 2 == 1 for l in layers):
        for k_ in ("moe_w_gate", "moe_w_up", "moe_w_down"):
            shared[k_] = np.zeros((1, 1, 1, 1), np.float32)
    in_maps = []
    for i in range(n_cores):
        xs = x[i * NB:(i + 1) * NB].reshape(NB * L, D)
        cs = c[i * NB:(i + 1) * NB]
        cTl = np.ascontiguousarray(np.transpose(cs.reshape(NB, KC, 128), (2, 1, 0))).reshape(128, KC * NB)
        m = dict(shared)
        m["xT"] = np.ascontiguousarray(xs.T)
        m["cT"] = cTl
        in_maps.append(m)
    import time as _t, sys as _s
    _t0 = _t.time()
    if CFG.get("sim"):
        return nc, in_maps
    res = run_bass_kernel_spmd(nc, in_maps, core_ids=list(range(n_cores)))
    print("[kernel] launch+transfer %.1fs" % (_t.time() - _t0), file=_s.stderr)
    out = np.empty((B, L, D), np.float32)
    for i in range(n_cores):
        out[i * NB:(i + 1) * NB] = res.results[i]["outT"].T.reshape(NB, L, D)
    return out
```

```python
import contextlib
import numpy as np
import concourse.bass as bass
import concourse.mybir as mybir
from concourse.bass_utils import run_bass_kernel_spmd

F32 = mybir.dt.float32
BF16 = mybir.dt.bfloat16
AF = mybir.ActivationFunctionType
ALU = mybir.AluOpType
AX = mybir.AxisListType

D = 1024
KC = 8
DIN = 3080
OFF_Z, OFF_BA, OFF_GU, OFF_GV = 1536, 2048, 2056, 2568
FF_DENSE = 2816
FF_MOE = 3584
NE = 8
EPS = 1e-6
N_CORES = 8

CFG = {"L": 4096, "NB": 2, "layers": [0, 1, 2, 3], "n_cores": 8}

ENGS = ["pe", "act", "dve", "pool", "sp"]
KEYS = ["pe", "act", "dve", "pool", "sp_dma", "pool_dma", "act_dma"]


class Res:
    __slots__ = ("w", "r", "x")

    def __init__(self, excl=False):
        self.w = None
        self.r = {}
        self.x = excl


class Prog:
    def __init__(self, nc, st):
        self.nc = nc
        self.q = {e: [] for e in ENGS}
        self.cnt = {k: 0 for k in KEYS}
        self.seen = {e: {} for e in ENGS}
        self.signaled = {k: set() for k in KEYS}
        self.rank_base = {k: 0 for k in KEYS}
        self.sems = {k: st.enter_context(nc.semaphore("s_" + k)) for k in KEYS}
        self.nops = 0
        self.mute = False

    def op(self, eng, fn, reads=(), writes=(), dma=False):
        if self.mute:
            return
        if any(r.x for r in reads):
            writes = list(writes) + [r for r in reads if r.x]
            reads = [r for r in reads if not r.x]
        key = eng + "_dma" if dma else eng
        idx = self.cnt[key] + 1
        self.cnt[key] = idx
        waits = {}
        for r in reads:
            if r.w is not None:
                k, v = r.w
                if not (k == "pe" and eng == "pe") and waits.get(k, 0) < v:
                    waits[k] = v
        for w in writes:
            if w.w is not None:
                k, v = w.w
                if not (k == "pe" and eng == "pe") and waits.get(k, 0) < v:
                    waits[k] = v
            for k, v in w.r.items():
                if k == eng:
                    continue
                if waits.get(k, 0) < v:
                    waits[k] = v
        wl = []
        seen = self.seen[eng]
        for k, v in waits.items():
            if seen.get(k, 0) >= v:
                continue
            seen[k] = v
            wl.append((k, v))
            self.signaled[k].add(v)
        for r in reads:
            if r.r.get(key, 0) < idx:
                r.r[key] = idx
        for w in writes:
            w.w = (key, idx)
            w.r = {}
        if dma:
            self.signaled[key].add(idx)
        self.q[eng].append((wl, fn, key, idx))
        self.nops += 1

    def barrier(self):
        tot = dict(self.cnt)
        for e in ENGS:
            wl = []
            for k, v in tot.items():
                if v == 0 or k == e:
                    continue
                if self.seen[e].get(k, 0) >= v:
                    continue
                self.seen[e][k] = v
                wl.append((k, v))
                self.signaled[k].add(v)
            if wl:
                self.q[e].append((wl, None, None, None))

    def flush(self):
        self.barrier()
        ranks = {}
        for k in KEYS:
            s = sorted(self.signaled[k])
            base = self.rank_base[k]
            ranks[k] = {v: base + i + 1 for i, v in enumerate(s)}
            self.rank_base[k] = base + len(s)
            self.signaled[k] = set()
        sems = self.sems
        q = self.q
        self.q = {e: [] for e in ENGS}

        def run(e):
            def body(engine):
                for wl, fn, key, idx in q[e]:
                    for k, v in wl:
                        engine.wait_ge(sems[k], ranks[k][v] * (16 if k.endswith("_dma") else 1))
                    if fn is None:
                        continue
                    ins = fn(engine)
                    if idx in ranks[key]:
                        ins.then_inc(sems[key], 16 if key.endswith("_dma") else 1)
            return body

        with self.nc.Block() as block:
            block.tensor(run("pe"))
            block.scalar(run("act"))
            block.vector(run("dve"))
            block.gpsimd(run("pool"))
            block.sync(run("sp"))


class Ring:
    def __init__(self, items):
        self.items = items
        self.i = 0

    def get(self):
        it = self.items[self.i % len(self.items)]
        self.i += 1
        return it


C_ID, C_ONES, C_TRIL, C_TRILS, C_TRIU, C_M0 = 0, 1, 2, 3, 4, 5
NCONST = 12


def _consts():
    c = np.zeros((NCONST, 128, 128), np.float32)
    i = np.arange(128)[:, None]
    j = np.arange(128)[None, :]
    c[C_ID] = (i == j)
    c[C_ONES] = 1.0
    c[C_TRIL] = (i >= j)
    c[C_TRILS] = (i > j)
    c[C_TRIU] = (i <= j)
    for l in range(7):
        b = 1 << l
        c[C_M0 + l] = ((i // (2 * b)) == (j // (2 * b))) & ((i % (2 * b)) >= b) & ((j % (2 * b)) < b)
    cons = np.ascontiguousarray(c.transpose(1, 0, 2))
    sel = np.zeros((8, 8, 128), np.float32)
    for e in range(8):
        sel[e, e, :] = 1.0
    return cons, sel.reshape(8, 1024)


def build(L, NB, layers):
    T = NB * L
    nl = len(layers)
    nc = bass.Bass("TRN2", target_bir_lowering=False)

    def din(name, shape, dt=F32):
        return nc.dram_tensor(name, list(shape), dt, kind="ExternalInput").ap()

    xT_in = din("xT", [D, T])
    outT = nc.dram_tensor("outT", [D, T], F32, kind="ExternalOutput").ap()
    Xs = nc.dram_tensor("Xs", [D, T], F32, kind="Internal").ap()
    cT_d = din("cT", [128, KC * NB])
    consts_d = din("consts", [128, NCONST * 128])
    sel_d = din("sel", [8, 1024])
    ada_w_d = din("ada_w", [4, D, 6 * D])
    ada_bT_d = din("ada_bT", [128, 4 * 48])
    fada_w_d = din("final_ada_w", [D, 2 * D])
    fada_bT_d = din("fada_bT", [128, 16])
    nw_d = din("nw", [128, 4 * 16 + 8])
    w_in_d = din("w_in", [4, D, DIN])
    cw_d = din("cw", [128, 4 * 48])
    dnw_d = din("dnw", [128, 4])
    alog_d = din("alog_bc", [128, 16])
    dtb_d = din("dtb_bc", [128, 16])
    lnw_d = din("gm_ln_w", [4, 512])
    lnb_d = din("gm_ln_b", [4, 512])
    wsT_d = din("wsT", [4, 128, 512])
    gsb_d = din("gsb", [4, 512])
    w_out_d = din("w_out", [4, D, D])
    fg_d = din("ffn_w_gate", [2, D, FF_DENSE])
    fu_d = din("ffn_w_up", [2, D, FF_DENSE])
    fd_d = din("ffn_w_down", [2, FF_DENSE, D])
    rt_d = din("moe_router", [2, D, NE])
    has_moe = any(l % 2 == 1 for l in layers)
    mg_d = din("moe_w_gate", [2, NE, D, FF_MOE] if has_moe else [1, 1, 1, 1])
    mu_d = din("moe_w_up", [2, NE, D, FF_MOE] if has_moe else [1, 1, 1, 1])
    md_d = din("moe_w_down", [2, NE, FF_MOE, D] if has_moe else [1, 1, 1, 1])

    with contextlib.ExitStack() as st:
        uid = [0]

        def uname(name):
            uid[0] += 1
            return "sb%d_%s" % (uid[0], name)

        def sb(name, shape, dt=F32):
            return st.enter_context(nc.sbuf_tensor(uname(name), list(shape), dt))

        P = Prog(nc, st)
        banks = [st.enter_context(nc.psum_tensor("bank%d" % i, [128, 512], F32)) for i in range(8)]
        bank_r = [Res(True) for _ in range(8)]

        cons = sb("cons", [128, NCONST, 128])
        cons_b = sb("cons_b", [128, 2, 128], BF16)
        sel = sb("sel", [8, 1024])
        onesrow_b = sb("onesrow_b", [1, 128], BF16)
        cT = sb("cT", [128, KC * NB])
        cact_b = sb("cact_b", [128, KC * NB], BF16)
        mod = sb("mod", [128, NB, 4 * 48 + 16])
        ada_bT = sb("ada_bT", [128, 4 * 48 + 16])
        nw = sb("nw", [128, 4 * 16 + 8])
        AB = sb("AB", [128, NB, (4 * 2 + 1) * 8])
        cw = sb("cw", [128, 4 * 48])
        dnw = sb("dnw", [128, 4])
        nA = sb("nA", [128, 16])
        dtb = sb("dtb", [128, 16])
        r_const = Res()

        def DMA(eng, out, in_, R=(), W=()):
            P.op(eng, lambda e: e.dma_start(out=out, in_=in_), R, W, dma=True)

        def MM(out, lhsT, rhs, start, stop, R, W):
            P.op("pe", lambda e: e.matmul(out, lhsT, rhs, start=start, stop=stop), R, W)

        def ACT(out, in_, func, R, W, scale=1.0, bias=0.0):
            P.op("act", lambda e: e.activation(out, in_, func, bias=bias, scale=scale), R, W)

        def TT(out, a, b, op, R, W, eng="dve"):
            P.op(eng, lambda e: e.tensor_tensor(out, a, b, op), R, W)

        def TS(out, a, s1, s2, op0, op1, R, W, eng="dve"):
            if s2 is None:
                P.op(eng, lambda e: e.tensor_scalar(out, a, s1, None, op0), R, W)
            else:
                P.op(eng, lambda e: e.tensor_scalar(out, a, s1, s2, op0, op1), R, W)

        def STT(out, a, s, b, op0, op1, R, W, eng="dve"):
            P.op(eng, lambda e: e.scalar_tensor_tensor(out, a, s, b, op0, op1), R, W)

        def CP(out, in_, R, W, eng="dve"):
            if eng == "act":
                P.op("act", lambda e: e.copy(out, in_), R, W)
            else:
                P.op(eng, lambda e: e.tensor_copy(out, in_), R, W)

        def RECIP(out, in_, R, W):
            P.op("dve", lambda e: e.reciprocal(out, in_), R, W)

        def MEMSET(ap, val, W, eng="pool"):
            P.op(eng, lambda e: e.memset(ap, val), (), W)

        DMA("sp", cons[:], consts_d.rearrange("p (a b) -> p a b", a=NCONST), W=[r_const])
        DMA("pool", cons_b[:], consts_d[:, 0:256].rearrange("p (a b) -> p a b", a=2), W=[r_const])
        DMA("sp", sel[:], sel_d, W=[r_const])
        DMA("sp", cT[:], cT_d, W=[r_const])
        DMA("sp", ada_bT[:, 0:192], ada_bT_d, W=[r_const])
        DMA("sp", ada_bT[:, 192:208], fada_bT_d, W=[r_const])
        DMA("sp", nw[:], nw_d, W=[r_const])
        DMA("sp", cw[:], cw_d, W=[r_const])
        DMA("sp", dnw[:], dnw_d, W=[r_const])
        DMA("sp", nA[:], alog_d, W=[r_const])
        DMA("sp", dtb[:], dtb_d, W=[r_const])
        MEMSET(onesrow_b[:], 1.0, [r_const])
        ACT(nA[:], nA[:], AF.Exp, [r_const], [r_const])
        TS(nA[:], nA[:], -1.0, None, ALU.mult, None, [r_const], [r_const])
        ACT(cact_b[:], cT[:], AF.Silu, [r_const], [r_const])

        ident_f = cons[:, C_ID, :]
        ones_f = cons[:, C_ONES, :]
        trils_f = cons[:, C_TRILS, :]
        triu_f = cons[:, C_TRIU, :]
        ident_b = cons_b[:, 0, :]
        ones_b = cons_b[:, 1, :]

        with contextlib.ExitStack() as ph:
            wbuf = [ph.enter_context(nc.sbuf_tensor(uname("adaw"), [128, KC, 1024], BF16)) for i in range(2)]
            wbuf_r = [Res(), Res()]
            mod_r = Res()
            pieces = []
            for li, l in enumerate(layers):
                for j in range(6):
                    pieces.append((ada_w_d[l, :, j * 1024:(j + 1) * 1024], li * 48 + j * 8))
            for j in range(2):
                pieces.append((fada_w_d[:, j * 1024:(j + 1) * 1024], 192 + j * 8))
            for pi, (src, col0) in enumerate(pieces):
                wb, wr = wbuf[pi % 2], wbuf_r[pi % 2]
                DMA("pool", wb[:], src.rearrange("(k p) n -> p k n", p=128), W=[wr])
                for oc in range(8):
                    bk = pi * 8 + oc
                    ps = banks[bk % 8][:, 0:NB]
                    pr = bank_r[bk % 8]
                    for k in range(KC):
                        MM(ps, wb[:, k, oc * 128:(oc + 1) * 128], cact_b[:, k * NB:(k + 1) * NB],
                           k == 0, k == KC - 1, [wr, r_const], [pr])
                    bcol = (layers[col0 // 48] * 48 + col0 % 48 + oc) if col0 < 192 else (192 + col0 - 192 + oc)
                    TS(mod[:, :, col0 + oc], ps, ada_bT[:, bcol:bcol + 1], None, ALU.add, None,
                       [pr, r_const], [mod_r])
            MEMSET(AB[:], 0.0, [mod_r])
            for li, l in enumerate(layers):
                for sub in range(2):
                    for b in range(NB):
                        sc = mod[:, b, li * 48 + (sub * 3 + 1) * 8: li * 48 + (sub * 3 + 2) * 8]
                        STT(AB[:, b, (li * 2 + sub) * 8:(li * 2 + sub + 1) * 8], sc, 1.0,
                            nw[:, l * 16 + sub * 8: l * 16 + sub * 8 + 8], ALU.add, ALU.mult, [mod_r, r_const], [mod_r])
            for b in range(NB):
                STT(AB[:, b, 64:72], mod[:, b, 200:208], 1.0, nw[:, 64:72], ALU.add, ALU.mult, [mod_r, r_const], [mod_r])
            TS(AB[:], AB[:], 32.0, None, ALU.mult, None, [mod_r], [mod_r])
            P.flush()

        def norm_mod(xt, xt_r, ncols, A, B, out_b, out_r, sq_ring, tmp_ring, rstd, rstd_r, ps, ps_r, out_f=None, out_f_r=None):
            for k in range(KC):
                sq, sq_r = sq_ring.get()
                ACT(sq[:, 0:ncols], xt[:, k, 0:ncols], AF.Square, [xt_r[k]], [sq_r])
                MM(ps[:, 0:ncols], ones_b, sq[:, 0:ncols], k == 0, k == KC - 1, [sq_r, r_const], [ps_r])
            ACT(rstd[:, 0:ncols], ps[:, 0:ncols], AF.Ln, [ps_r], [rstd_r], scale=1.0, bias=1024.0 * EPS)
            ACT(rstd[:, 0:ncols], rstd[:, 0:ncols], AF.Exp, [rstd_r], [rstd_r], scale=-0.5)
            for k in range(KC):
                tm, tm_r = tmp_ring.get()
                TT(tm[:, 0:ncols], xt[:, k, 0:ncols], rstd[:, 0:ncols], ALU.mult, [xt_r[k], rstd_r], [tm_r])
                if out_f is not None:
                    ACT(out_f[:, k, 0:ncols], tm[:, 0:ncols], AF.Identity, [tm_r], [out_f_r[k]],
                        scale=A[:, k:k + 1], bias=B[:, k:k + 1])
                ACT(out_b[:, k, 0:ncols], tm[:, 0:ncols], AF.Identity, [tm_r], [out_r[k]],
                    scale=A[:, k:k + 1], bias=B[:, k:k + 1])

        first = [True]

        def cur_src():
            return xT_in if first[0] else Xs

        def phase_A(li, l):
            TTK = 256
            NCH = TTK // 128
            src = cur_src()
            with contextlib.ExitStack() as ph:
                def pb(name, shape, dt=F32):
                    return ph.enter_context(nc.sbuf_tensor(uname(name), list(shape), dt))

                class _BankRing:
                    def __init__(self, ncol):
                        self.ncol = ncol

                    def get(self):
                        i = bring[0] % 8
                        bring[0] += 1
                        return (banks[i][:, 0:self.ncol], bank_r[i])
                bring = [0]
                wide = _BankRing(256)
                small = _BankRing(128)
                fullr = _BankRing(512)

                w_in_b = pb("w_in_b", [128, KC, DIN], BF16)
                w_out_b = pb("w_out_b", [128, KC, D], BF16)
                wsT_f = pb("wsT_f", [128, 512])
                wsT_b = pb("wsT_b", [128, 512], BF16)
                brow_b = pb("brow_b", [1, 512], BF16)
                lnw = pb("lnw", [128, 512])
                lnb = pb("lnb", [128, 512])
                r_w = Res()
                for k in range(KC):
                    DMA("pool", w_in_b[:, k, :], w_in_d[l, k * 128:(k + 1) * 128, :], W=[r_w])
                DMA("pool", w_out_b[:], w_out_d[l].rearrange("(k p) n -> p k n", p=128), W=[r_w])
                DMA("sp", wsT_f[:], wsT_d[l], W=[r_w])
                DMA("pool", brow_b[:], gsb_d[l:l + 1, :], W=[r_w])
                DMA("sp", lnw[:], lnw_d[l:l + 1, :].to_broadcast([128, 512]), W=[r_w])
                DMA("sp", lnb[:], lnb_d[l:l + 1, :].to_broadcast([128, 512]), W=[r_w])
                for g in range(4):
                    TT(wsT_b[:, g * 128:(g + 1) * 128], wsT_f[:, g * 128:(g + 1) * 128], triu_f, ALU.mult,
                       [r_w, r_const], [r_w])

                xt = pb("xt", [128, KC, TTK]); xt_r = [Res() for _ in range(KC)]
                hT = pb("hT", [128, KC, TTK], BF16); hT_r = [Res() for _ in range(KC)]
                sq_ring = Ring([(pb("sqa%d" % i, [128, TTK], BF16), Res()) for i in range(3)])
                tmp_ring = Ring([(pb("tma%d" % i, [128, TTK]), Res()) for i in range(3)])
                rstd = pb("rstd", [128, TTK]); rstd_r = Res()
                pre = pb("pre", [128, 12, TTK + 3]); pre_r = [Res() for _ in range(12)]
                qs = pb("qs", [128, 4, TTK]); ks = pb("ks", [128, 4, TTK]); vs_b = pb("vs_b", [128, 4, TTK], BF16)
                qkv_r = [Res() for _ in range(12)]
                qn_b = pb("qn_b", [128, 4, TTK], BF16); kn_b = pb("kn_b", [128, 4, TTK], BF16)
                qn_r = [Res() for _ in range(4)]; kn_r = [Res() for _ in range(4)]
                zs = pb("zs", [128, 4, TTK], BF16); zs_r = [Res() for _ in range(4)]
                gus = pb("gus", [128, 4, TTK], BF16); gus_r = [Res() for _ in range(4)]
                gvf_ring = Ring([(pb("gvf%d" % i, [128, 512]), Res()) for i in range(2)])
                gv_b = pb("gv_b", [128, NCH, 512], BF16); gv_r = [Res() for _ in range(NCH)]
                stat = pb("stat", [128, 8]); stat_r = Res()
                mixT = pb("mixT", [128, KC, TTK], BF16); mix_r = [Res() for _ in range(KC)]
                oT = pb("oT", [128, 4, TTK]); oT_r = [Res() for _ in range(4)]
                ba = pb("ba", [128, NCH * 8]); ba_r = Res()
                g_tm = pb("g_tm", [128, NCH * 4]); beta_tm = pb("beta_tm", [128, NCH * 4])
                G_tm = pb("G_tm", [128, NCH * 4]); e1 = pb("e1", [128, NCH * 4]); e2 = pb("e2", [128, NCH * 4])
                eGl = pb("eGl", [128, NCH * 4]); sm_r = Res()
                S_f = pb("S_f", [128, 4, 128]); S_b = pb("S_b", [128, 4, 128], BF16)
                S_r = [Res() for _ in range(4)]; Sb_r = [Res() for _ in range(4)]
                def hb(name, dt=F32):
                    return [(pb("%s%d" % (name, h), [128, 128], dt), Res()) for h in range(4 * NCH)]
                gbc = hb("gbc"); eG = hb("eG", BF16); dd = hb("dd", BF16); dt_ = hb("dt_", BF16)
                Am = hb("Am", BF16); AM = [hb("AM%d_" % lv, BF16) for lv in range(2)]
                U0 = hb("U0", BF16); U1 = hb("U1", BF16); T0 = hb("T0", BF16); T1 = hb("T1", BF16)
                P1 = hb("P1", BF16); attnT = hb("attnT", BF16); qd = hb("qd", BF16)
                kbd = hb("kbd", BF16); kdec = hb("kdec", BF16); vb = hb("vb", BF16)
                u_f = hb("u_f"); wT = hb("wT", BF16); vnew = hb("vnew", BF16)

                A1 = AB[:, :, (li * 2) * 8:(li * 2 + 1) * 8]
                c4 = l * 4

                for seq in range(NB):
                    for h in range(4):
                        MEMSET(S_f[:, h, :], 0.0, [S_r[h]])
                        MEMSET(S_b[:, h, :], 0.0, [Sb_r[h]])
                    for j in range(12):
                        MEMSET(pre[:, j, 0:3], 0.0, [pre_r[j]])
                    for it in range(L // TTK):
                        tok0 = seq * L + it * TTK
                        dsl = src[:, tok0:tok0 + TTK].rearrange("(k p) t -> p k t", p=128)
                        DMA("sp", xt[:], dsl, W=xt_r)
                        ps, ps_r = wide.get()
                        norm_mod(xt, xt_r, TTK, A1[:, seq, :], mod[:, seq, li * 48: li * 48 + 8], hT, hT_r,
                                 sq_ring, tmp_ring, rstd, rstd_r, ps, ps_r)
                        def proj(col0):
                            ps, ps_r = wide.get()
                            for k in range(KC):
                                MM(ps, w_in_b[:, k, col0:col0 + 128], hT[:, k, :], k == 0, k == KC - 1,
                                   [r_w, hT_r[k]], [ps_r])
                            return ps, ps_r
                        for j in range(12):
                            ps, ps_r = proj(j * 128)
                            CP(pre[:, j, 3:3 + TTK], ps, [ps_r], [pre_r[j]], eng="act")
                            acc_t, acc_r = tmp_ring.get()
                            acc = acc_t[:]
                            cwj = cw[:, l * 48 + j * 4: l * 48 + j * 4 + 4]
                            TS(acc, pre[:, j, 0:TTK], cwj[:, 0:1], None, ALU.mult, None, [pre_r[j], r_const], [acc_r])
                            for tp in range(1, 4):
                                STT(acc, pre[:, j, tp:tp + TTK], cwj[:, tp:tp + 1], acc, ALU.mult, ALU.add,
                                    [pre_r[j], acc_r, r_const], [acc_r])
                            dst = (qs, ks, vs_b)[j // 4][:, j % 4, :]
                            ACT(dst, acc, AF.Silu, [acc_r], [qkv_r[j]])
                            CP(pre[:, j, 0:3], pre[:, j, TTK:TTK + 3], [pre_r[j]], [pre_r[j]], eng="pool")
                        for h in range(4):
                            ps, ps_r = proj(OFF_Z + h * 128)
                            ACT(zs[:, h, :], ps, AF.Silu, [ps_r], [zs_r[h]])
                        for g in range(4):
                            ps, ps_r = proj(OFF_GU + g * 128)
                            ACT(gus[:, g, :], ps, AF.Gelu_apprx_tanh, [ps_r], [gus_r[g]])
                        P.mute = CFG.get('upto', 99) < 2
                        psba, psba_r = small.get()
                        for c in range(NCH):
                            for k in range(KC):
                                MM(psba[:, c * 8:(c + 1) * 8], hT[:, k, c * 128:(c + 1) * 128],
                                   w_in_b[:, k, OFF_BA:OFF_BA + 8], k == 0, k == KC - 1, [r_w, hT_r[k]], [psba_r])
                        CP(ba[:], psba[:, 0:NCH * 8], [psba_r], [ba_r])
                        for c in range(NCH):
                            ACT(beta_tm[:, c * 4:c * 4 + 4], ba[:, c * 8:c * 8 + 4], AF.Sigmoid, [ba_r], [sm_r])
                            TT(g_tm[:, c * 4:c * 4 + 4], ba[:, c * 8 + 4:c * 8 + 8], dtb[:, c4:c4 + 4], ALU.add, [ba_r, r_const], [sm_r])
                        g2d = g_tm[:]
                        ACT(g2d, g2d, AF.Exp, [sm_r], [sm_r])
                        ACT(g2d, g2d, AF.Ln, [sm_r], [sm_r], scale=1.0, bias=1.0)
                        for c in range(NCH):
                            TT(g_tm[:, c * 4:c * 4 + 4], g_tm[:, c * 4:c * 4 + 4], nA[:, c4:c4 + 4], ALU.mult, [sm_r, r_const], [sm_r])
                        for c in range(NCH):
                            psg, psg_r = fullr.get()
                            for k in range(KC):
                                MM(psg[:], hT[:, k, c * 128:(c + 1) * 128], w_in_b[:, k, OFF_GV:OFF_GV + 512],
                                   k == 0, k == KC - 1, [r_w, hT_r[k]], [psg_r])
                            gvf, gvf_r = gvf_ring.get()
                            ACT(gvf[:], psg[:], AF.Gelu_apprx_tanh, [psg_r], [gvf_r])
                            P.op("dve", lambda e, gvf=gvf, c=c: e.reduce_sum(stat[:, c:c + 1], gvf[:], AX.X), [gvf_r], [stat_r])
                            TS(stat[:, c:c + 1], stat[:, c:c + 1], 1.0 / 512, None, ALU.mult, None, [stat_r], [stat_r])
                            TS(gvf[:], gvf[:], stat[:, c:c + 1], None, ALU.subtract, None, [gvf_r, stat_r], [gvf_r])
                            sq2, sq2_r = gvf_ring.get()
                            TT(sq2[:], gvf[:], gvf[:], ALU.mult, [gvf_r], [sq2_r])
                            P.op("dve", lambda e, sq2=sq2, c=c: e.reduce_sum(stat[:, 4 + c:5 + c], sq2[:], AX.X), [sq2_r], [stat_r])
                            ACT(stat[:, 4 + c:5 + c], stat[:, 4 + c:5 + c], AF.Ln, [stat_r], [stat_r], scale=1.0 / 512, bias=EPS)
                            ACT(stat[:, 4 + c:5 + c], stat[:, 4 + c:5 + c], AF.Exp, [stat_r], [stat_r], scale=-0.5)
                            STT(gvf[:], gvf[:], stat[:, 4 + c:5 + c], lnw[:], ALU.mult, ALU.mult, [gvf_r, stat_r, r_w], [gvf_r])
                            TT(gv_b[:, c, :], gvf[:], lnb[:], ALU.add, [gvf_r, r_w], [gv_r[c]])
                        P.mute = CFG.get('upto', 99) < 3
                        for g in range(4):
                            ps, ps_r = wide.get()
                            for c in range(NCH):
                                MM(ps[:, c * 128:(c + 1) * 128], gv_b[:, c, g * 128:(g + 1) * 128],
                                   wsT_b[:, g * 128:(g + 1) * 128], True, False, [gv_r[c], r_w], [ps_r])
                                MM(ps[:, c * 128:(c + 1) * 128], onesrow_b[0:1, :], brow_b[0:1, g * 128:(g + 1) * 128],
                                   False, True, [r_const, r_w], [ps_r])
                            TT(mixT[:, 4 + g, :], ps, gus[:, g, :], ALU.mult, [ps_r, gus_r[g]], [mix_r[4 + g]])
                        P.mute = CFG.get('upto', 99) < 4
                        for h in range(4):
                            for (srcb, r_i, dstb, dst_r, scl) in ((qs, h, qn_b, qn_r, 128.0 ** -0.5), (ks, 4 + h, kn_b, kn_r, 1.0)):
                                sq, sq_r = sq_ring.get()
                                ACT(sq[:], srcb[:, h, :], AF.Square, [qkv_r[r_i]], [sq_r])
                                ps, ps_r = wide.get()
                                MM(ps, ones_b, sq[:], True, True, [sq_r, r_const], [ps_r])
                                tm, tm_r = tmp_ring.get()
                                ACT(tm[:], ps, AF.Ln, [ps_r], [tm_r], scale=1.0, bias=EPS)
                                ACT(tm[:], tm[:], AF.Exp, [tm_r], [tm_r], scale=-0.5)
                                STT(dstb[:, h, :], srcb[:, h, :], scl, tm[:], ALU.mult, ALU.mult, [qkv_r[r_i], tm_r], [dst_r[h]])
                        psl, psl_r = small.get()
                        for c in range(NCH):
                            MM(psl[:, c * 4:(c + 1) * 4], ones_f, g_tm[:, c * 4:c * 4 + 4], True, True, [sm_r, r_const], [psl_r])
                            MM(psl[:, 16 + c * 4:16 + (c + 1) * 4], triu_f, g_tm[:, c * 4:c * 4 + 4], True, True, [sm_r, r_const], [psl_r])
                        sm2_r = Res()
                        CP(G_tm[:], psl[:, 16:16 + NCH * 4], [psl_r], [sm2_r])
                        ACT(eGl[:], psl[:, 0:NCH * 4], AF.Exp, [psl_r], [sm2_r])
                        TT(e2[:], psl[:, 0:NCH * 4], G_tm[:],
                           ALU.subtract, [psl_r, sm2_r], [sm2_r])
                        ACT(e2[:], e2[:], AF.Exp, [sm2_r], [sm2_r])
                        ACT(e1[:], G_tm[:], AF.Exp, [sm2_r], [sm2_r])
                        TT(e1[:], e1[:],
                           beta_tm[:], ALU.mult, [sm2_r, sm_r], [sm2_r])
                        P.mute = CFG.get('upto', 99) < 5
                        CHN = [(c, h) for c in range(NCH) for h in range(4)]
                        def csl(c):
                            return slice(c * 128, (c + 1) * 128)
                        def col(c, h):
                            return slice(c * 4 + h, c * 4 + h + 1)
                        psG = {}
                        for (c, h) in CHN:
                            i = c * 4 + h
                            TS(gbc[i][0][:], ones_f, g_tm[:, col(c, h)], None, ALU.mult, None, [sm_r, r_const], [gbc[i][1]])
                            psG[i] = small.get()
                            MM(psG[i][0], gbc[i][0][:], triu_f, True, True, [gbc[i][1], r_const], [psG[i][1]])
                        for (c, h) in CHN:
                            i = c * 4 + h
                            pg, pg_r = psG[i]
                            Gc = G_tm[:, col(c, h)]
                            ACT(eG[i][0][:], pg, AF.Exp, [pg_r], [eG[i][1]])
                            TS(dd[i][0][:], pg, Gc, 0.0, ALU.subtract, ALU.max, [pg_r, sm2_r], [dd[i][1]])
                            TS(dt_[i][0][:], pg, Gc, 0.0, ALU.subtract, ALU.min, [pg_r, sm2_r], [dt_[i][1]])
                            ACT(dd[i][0][:], dd[i][0][:], AF.Exp, [dd[i][1]], [dd[i][1]], scale=-1.0)
                            ACT(dt_[i][0][:], dt_[i][0][:], AF.Exp, [dt_[i][1]], [dt_[i][1]])
                            TT(dd[i][0][:], dd[i][0][:], trils_f, ALU.mult, [dd[i][1], r_const], [dd[i][1]], eng="pool")
                            TT(dt_[i][0][:], dt_[i][0][:], triu_f, ALU.mult, [dt_[i][1], r_const], [dt_[i][1]], eng="pool")
                            TT(qd[i][0][:], qn_b[:, h, csl(c)], eG[i][0][:], ALU.mult, [qn_r[h], eG[i][1]], [qd[i][1]])
                        for (c, h) in CHN:
                            i = c * 4 + h
                            cs = csl(c)
                            pk, pk_r = small.get()
                            MM(pk, kn_b[:, h, cs], kn_b[:, h, cs], True, True, [kn_r[h]], [pk_r])
                            STT(Am[i][0][:], pk, beta_tm[:, col(c, h)], dd[i][0][:], ALU.mult, ALU.mult,
                                [pk_r, sm_r, dd[i][1]], [Am[i][1]])
                            pq, pq_r = small.get()
                            MM(pq, kn_b[:, h, cs], qn_b[:, h, cs], True, True, [kn_r[h], qn_r[h]], [pq_r])
                            TT(attnT[i][0][:], pq, dt_[i][0][:], ALU.mult, [pq_r, dt_[i][1]], [attnT[i][1]])
                            pt, pt_r = small.get()
                            MM(pt, kn_b[:, h, cs], ident_b, True, True, [kn_r[h], r_const], [pt_r])
                            TS(kbd[i][0][:], pt, e1[:, col(c, h)], None, ALU.mult, None, [pt_r, sm2_r], [kbd[i][1]])
                            ACT(kdec[i][0][:], pt, AF.Identity, [pt_r, sm2_r], [kdec[i][1]], scale=e2[:, col(c, h)])
                            pv, pv_r = small.get()
                            MM(pv, vs_b[:, h, cs], ident_b, True, True, [qkv_r[8 + h], r_const], [pv_r])
                            TS(vb[i][0][:], pv, beta_tm[:, col(c, h)], None, ALU.mult, None, [pv_r, sm_r], [vb[i][1]])
                        P.mute = CFG.get('upto', 99) < 6
                        Ucur = {}; Tcur = {}
                        for (c, h) in CHN:
                            i = c * 4 + h
                            a0, a0_r = AM[0][i]
                            TT(a0[:], Am[i][0][:], cons[:, C_M0, :], ALU.mult, [Am[i][1], r_const], [a0_r], eng="pool")
                            pt, pt_r = small.get()
                            MM(pt, a0[:], ident_b, True, True, [a0_r, r_const], [pt_r])
                            TT(U0[i][0][:], ident_f, pt, ALU.subtract, [pt_r, r_const], [U0[i][1]])
                            TT(T0[i][0][:], ident_f, a0[:], ALU.subtract, [a0_r, r_const], [T0[i][1]], eng="pool")
                            Ucur[i] = U0[i]; Tcur[i] = T0[i]
                        for lv in range(1, 7):
                            pp = {}
                            for i in range(NCH * 4):
                                al, al_r = AM[lv % 2][i]
                                TT(al[:], Am[i][0][:], cons[:, C_M0 + lv, :], ALU.mult, [Am[i][1], r_const], [al_r], eng="pool")
                                pp[i] = small.get()
                                MM(pp[i][0], al[:], Ucur[i][0][:], True, True, [al_r, Ucur[i][1]], [pp[i][1]])
                            for i in range(NCH * 4):
                                CP(P1[i][0][:], pp[i][0], [pp[i][1]], [P1[i][1]], eng="act")
                            px = {}
                            for i in range(NCH * 4):
                                px[i] = small.get()
                                MM(px[i][0], Tcur[i][0][:], P1[i][0][:], True, True, [Tcur[i][1], P1[i][1]], [px[i][1]])
                            for i in range(NCH * 4):
                                Un = U1[i] if Ucur[i] is U0[i] else U0[i]
                                TT(Un[0][:], Ucur[i][0][:], px[i][0], ALU.subtract, [Ucur[i][1], px[i][1]], [Un[1]])
                                Ucur[i] = Un
                            if lv < 6:
                                ptt = {}
                                for i in range(NCH * 4):
                                    ptt[i] = small.get()
                                    MM(ptt[i][0], Ucur[i][0][:], ident_b, True, True, [Ucur[i][1], r_const], [ptt[i][1]])
                                for i in range(NCH * 4):
                                    Tn = T1[i] if Tcur[i] is T0[i] else T0[i]
                                    CP(Tn[0][:], ptt[i][0], [ptt[i][1]], [Tn[1]], eng="act")
                                    Tcur[i] = Tn
                        P.mute = CFG.get('upto', 99) < 7
                        for i in range(NCH * 4):
                            pu, pu_r = small.get()
                            MM(pu, Ucur[i][0][:], vb[i][0][:], True, True, [Ucur[i][1], vb[i][1]], [pu_r])
                            CP(u_f[i][0][:], pu, [pu_r], [u_f[i][1]], eng="act")
                            pw, pw_r = small.get()
                            MM(pw, kbd[i][0][:], Ucur[i][0][:], True, True, [kbd[i][1], Ucur[i][1]], [pw_r])
                            CP(wT[i][0][:], pw, [pw_r], [wT[i][1]])
                        for c in range(NCH):
                            cs = csl(c)
                            pws = {}
                            for h in range(4):
                                i = c * 4 + h
                                pws[h] = small.get()
                                MM(pws[h][0], wT[i][0][:], S_b[:, h, :], True, True, [wT[i][1], Sb_r[h]], [pws[h][1]])
                            for h in range(4):
                                i = c * 4 + h
                                TT(vnew[i][0][:], u_f[i][0][:], pws[h][0], ALU.subtract, [u_f[i][1], pws[h][1]], [vnew[i][1]])
                            for h in range(4):
                                i = c * 4 + h
                                po, po_r = small.get()
                                MM(po, S_b[:, h, :], qd[i][0][:], True, False, [Sb_r[h], qd[i][1]], [po_r])
                                MM(po, vnew[i][0][:], attnT[i][0][:], False, True, [vnew[i][1], attnT[i][1]], [po_r])
                                CP(oT[:, h, cs], po, [po_r], [oT_r[h]], eng="act")
                                pS, pS_r = small.get()
                                MM(pS, kdec[i][0][:], vnew[i][0][:], True, True, [kdec[i][1], vnew[i][1]], [pS_r])
                                STT(S_f[:, h, :], S_f[:, h, :], eGl[:, col(c, h)], pS, ALU.mult, ALU.add,
                                    [S_r[h], sm2_r, pS_r], [S_r[h]])
                                CP(S_b[:, h, :], S_f[:, h, :], [S_r[h]], [Sb_r[h]], eng="act")
                        P.mute = CFG.get('upto', 99) < 8
                        for h in range(4):
                            sq, sq_r = sq_ring.get()
                            ACT(sq[:], oT[:, h, :], AF.Square, [oT_r[h]], [sq_r])
                            ps, ps_r = wide.get()
                            MM(ps, ones_b, sq[:], True, True, [sq_r, r_const], [ps_r])
                            tm, tm_r = tmp_ring.get()
                            ACT(tm[:], ps, AF.Ln, [ps_r], [tm_r], scale=1.0 / 128, bias=EPS)
                            ACT(tm[:], tm[:], AF.Exp, [tm_r], [tm_r], scale=-0.5)
                            TT(tm[:], tm[:], oT[:, h, :], ALU.mult, [tm_r, oT_r[h]], [tm_r])
                            STT(mixT[:, h, :], tm[:], dnw[:, l:l + 1], zs[:, h, :], ALU.mult, ALU.mult,
                                [tm_r, r_const, zs_r[h]], [mix_r[h]])
                        P.mute = CFG.get('upto', 99) < 0
                        for oc in range(KC):
                            ps, ps_r = wide.get()
                            for k in range(KC):
                                MM(ps, w_out_b[:, k, oc * 128:(oc + 1) * 128], mixT[:, k, :], k == 0, k == KC - 1,
                                   [r_w, mix_r[k]], [ps_r])
                            g1 = mod[:, seq, li * 48 + 16 + oc: li * 48 + 17 + oc]
                            STT(xt[:, oc, :], ps, g1, xt[:, oc, :], ALU.mult, ALU.add, [ps_r, xt_r[oc]], [xt_r[oc]])
                        DMA("sp", Xs[:, tok0:tok0 + TTK].rearrange("(k p) t -> p k t", p=128), xt[:], R=xt_r)
                first[0] = False
                P.flush()

        def phase_B(li, l):
            moe = (l % 2 == 1)
            j = l // 2
            FF = FF_MOE if moe else FF_DENSE
            nexp = NE if moe else 1
            TB = min(1024, L)
            NT = TB // 512
            src = cur_src()
            with contextlib.ExitStack() as ph:
                def pb(name, shape, dt=F32):
                    return ph.enter_context(nc.sbuf_tensor(uname(name), list(shape), dt))
                pbank = Ring([(banks[i], bank_r[i]) for i in range(8)])
                h2b = pb("h2b", [128, KC, TB], BF16); h2b_r = [[Res() for _ in range(KC)] for _ in range(NT)]
                yacc = pb("yacc", [128, KC, TB]); y_r = [[Res() for _ in range(KC)] for _ in range(NT)]
                xt2 = [pb("xtb%d" % i, [128, KC, 512]) for i in range(2)]
                xt2_r = [[Res() for _ in range(KC)] for _ in range(2)]
                sq_ring = Ring([(pb("sqb%d" % i, [128, 512], BF16), Res()) for i in range(3)])
                tmp_ring = Ring([(pb("tmb%d" % i, [128, 512]), Res()) for i in range(3)])
                sg_ring = Ring([(pb("sg%d" % i, [128, 512]), Res()) for i in range(3)])
                hid = [[(pb("hid%d_%d" % (i, f), [128, 512], BF16), Res()) for f in range(4)] for i in range(2)]
                rstd = pb("rstdb", [128, 512]); rstd_r = Res()
                wg = [pb("wg%d" % i, [128, KC, 512], BF16) for i in range(2)]
                wu = [pb("wu%d" % i, [128, KC, 512], BF16) for i in range(2)]
                wd = [pb("wd%d" % i, [128, 4, D], BF16) for i in range(2)]
                w_r = [Res(), Res()]
                if moe:
                    h2f = pb("h2f", [128, KC, 512]); h2f_r = [Res() for _ in range(KC)]
                    wr = pb("wr", [128, KC, NE]); wr_r = Res()
                    DMA("sp", wr[:], rt_d[j].rearrange("(k p) e -> p k e", p=128), W=[wr_r])
                    comb = pb("comb", [128, TB // 128, NE]); comb_r = Res()
                    combT = pb("combT", [8, TB]); combT_r = Res()
                    cbc = [pb("cbc%d" % i, [128, TB]) for i in range(2)]
                    cbc_r = [[Res() for _ in range(NT)] for _ in range(2)]
                    lg = pb("lg", [128, NE]); lg2 = pb("lg2", [128, NE]); mk1 = pb("mk1", [128, NE]); mk2 = pb("mk2", [128, NE])
                    m12 = pb("m12", [128, 4]); lg_r = Res()
                A2 = AB[:, :, (li * 2 + 1) * 8:(li * 2 + 2) * 8]
                pieces = []
                for e in range(nexp):
                    f0 = 0
                    while f0 < FF:
                        pw_ = min(512, FF - f0)
                        pieces.append((e, f0, pw_))
                        f0 += pw_

                def load_piece(pi):
                    e, f0, pw_ = pieces[pi]
                    bi = pi % 2
                    if moe:
                        g_src, u_src, d_src = mg_d[j, e], mu_d[j, e], md_d[j, e]
                    else:
                        g_src, u_src, d_src = fg_d[j], fu_d[j], fd_d[j]
                    DMA("pool", wg[bi][:, :, 0:pw_], g_src[:, f0:f0 + pw_].rearrange("(k p) n -> p k n", p=128), W=[w_r[bi]])
                    DMA("pool", wu[bi][:, :, 0:pw_], u_src[:, f0:f0 + pw_].rearrange("(k p) n -> p k n", p=128), W=[w_r[bi]])
                    DMA("pool", wd[bi][:, 0:pw_ // 128, :], d_src[f0:f0 + pw_, :].rearrange("(f p) n -> p f n", p=128), W=[w_r[bi]])

                xload = [0]
                for blk in range(T // TB):
                    seq = (blk * TB) // L
                    btok = blk * TB
                    load_piece(0)
                    for t in range(NT):
                        xb = xload[0] % 2; xload[0] += 1
                        xt, xt_r = xt2[xb], xt2_r[xb]
                        tok0 = btok + t * 512
                        DMA("sp", xt[:], src[:, tok0:tok0 + 512].rearrange("(k p) t -> p k t", p=128), W=xt_r)
                        ps, ps_r = pbank.get()
                        class _V:
                            def __getitem__(self, idx):
                                p_, k_, c_ = idx
                                return h2b[p_, k_, t * 512 + (c_.start or 0): t * 512 + (c_.stop or 512)]
                        norm_mod(xt, xt_r, 512, A2[:, seq, :], mod[:, seq, li * 48 + 24: li * 48 + 32], _V(), h2b_r[t],
                                 sq_ring, tmp_ring, rstd, rstd_r, ps, ps_r,
                                 out_f=(h2f if moe else None), out_f_r=(h2f_r if moe else None))
                        if moe:
                            P.mute = CFG.get('moe_upto', 99) < 1
                            for c in range(4):
                                gc = t * 4 + c
                                pl, pl_r = pbank.get()
                                for k in range(KC):
                                    MM(pl[:, 0:NE], h2f[:, k, c * 128:(c + 1) * 128], wr[:, k, :], k == 0, k == KC - 1,
                                       [h2f_r[k], wr_r], [pl_r])
                                CP(lg[:], pl[:, 0:NE], [pl_r], [lg_r])
                                P.op("dve", lambda e: e.reduce_max(m12[:, 0:1], lg[:], AX.X), [lg_r], [lg_r])
                                TS(mk1[:], lg[:], m12[:, 0:1], None, ALU.is_equal, None, [lg_r], [lg_r])
                                STT(lg2[:], mk1[:], -1e30, lg[:], ALU.mult, ALU.add, [lg_r], [lg_r])
                                P.op("dve", lambda e: e.reduce_max(m12[:, 1:2], lg2[:], AX.X), [lg_r], [lg_r])
                                TS(mk2[:], lg2[:], m12[:, 1:2], None, ALU.is_equal, None, [lg_r], [lg_r])
                                TT(m12[:, 2:3], m12[:, 1:2], m12[:, 0:1], ALU.subtract, [lg_r], [lg_r])
                                ACT(m12[:, 2:3], m12[:, 2:3], AF.Exp, [lg_r], [lg_r])
                                TS(m12[:, 2:3], m12[:, 2:3], 1.0, None, ALU.add, None, [lg_r], [lg_r])
                                RECIP(m12[:, 2:3], m12[:, 2:3], [lg_r], [lg_r])
                                TS(m12[:, 3:4], m12[:, 2:3], -1.0, 1.0, ALU.mult, ALU.add, [lg_r], [lg_r])
                                TS(mk1[:], mk1[:], m12[:, 2:3], None, ALU.mult, None, [lg_r], [lg_r])
                                STT(comb[:, gc, :], mk2[:], m12[:, 3:4], mk1[:], ALU.mult, ALU.add, [lg_r], [comb_r])
                                P.mute = CFG.get('moe_upto', 99) < 2
                                pc, pc_r = pbank.get()
                                MM(pc[0:8, 0:128], comb[:, gc, :], ident_f, True, True, [comb_r, r_const], [pc_r])
                                CP(combT[:, gc * 128:(gc + 1) * 128], pc[0:8, 0:128], [pc_r], [combT_r])
                    P.mute = False
                    first_acc = True
                    for pi, (e, f0, pw_) in enumerate(pieces):
                        if pi + 1 < len(pieces):
                            load_piece(pi + 1)
                        bi = pi % 2
                        nf = pw_ // 128
                        if moe and f0 == 0:
                            P.mute = CFG.get('moe_upto', 99) < 3
                            for t in range(NT):
                                pc, pc_r = pbank.get()
                                MM(pc[:], sel[0:8, e * 128:(e + 1) * 128], combT[:, t * 512:(t + 1) * 512], True, True,
                                   [r_const, combT_r], [pc_r])
                                CP(cbc[e % 2][:, t * 512:(t + 1) * 512], pc[:], [pc_r], [cbc_r[e % 2][t]], eng="act")
                        P.mute = False
                        for t in range(NT):
                            ts_ = slice(t * 512, (t + 1) * 512)
                            hb_ = hid[(pi * NT + t) % 2]
                            for f in range(nf):
                                pg, pg_r = pbank.get()
                                for k in range(KC):
                                    MM(pg[:], wg[bi][:, k, f * 128:(f + 1) * 128], h2b[:, k, ts_], k == 0, k == KC - 1,
                                       [w_r[bi], h2b_r[t][k]], [pg_r])
                                pu, pu_r = pbank.get()
                                for k in range(KC):
                                    MM(pu[:], wu[bi][:, k, f * 128:(f + 1) * 128], h2b[:, k, ts_], k == 0, k == KC - 1,
                                       [w_r[bi], h2b_r[t][k]], [pu_r])
                                sg, sg_r = sg_ring.get()
                                ACT(sg[:], pg[:], AF.Silu, [pg_r], [sg_r])
                                if moe:
                                    P.mute = CFG.get('moe_upto', 99) < 4
                                    TT(sg[:], sg[:], cbc[e % 2][:, ts_], ALU.mult, [sg_r, cbc_r[e % 2][t]], [sg_r])
                                P.mute = False
                                TT(hb_[f][0][:], pu[:], sg[:], ALU.mult, [pu_r, sg_r], [hb_[f][1]])
                            for oc in range(KC):
                                py, py_r = pbank.get()
                                for f in range(nf):
                                    MM(py[:], wd[bi][:, f, oc * 128:(oc + 1) * 128], hb_[f][0][:], f == 0, f == nf - 1,
                                       [w_r[bi], hb_[f][1]], [py_r])
                                if first_acc:
                                    CP(yacc[:, oc, ts_], py[:], [py_r], [y_r[t][oc]], eng="act")
                                else:
                                    TT(yacc[:, oc, ts_], yacc[:, oc, ts_], py[:], ALU.add, [y_r[t][oc], py_r], [y_r[t][oc]])
                        first_acc = False
                    for t in range(NT):
                        xb = xload[0] % 2; xload[0] += 1
                        xt, xt_r = xt2[xb], xt2_r[xb]
                        tok0 = btok + t * 512
                        DMA("sp", xt[:], src[:, tok0:tok0 + 512].rearrange("(k p) t -> p k t", p=128), W=xt_r)
                        for oc in range(KC):
                            g2 = mod[:, seq, li * 48 + 40 + oc: li * 48 + 41 + oc]
                            STT(xt[:, oc, :], yacc[:, oc, t * 512:(t + 1) * 512], g2, xt[:, oc, :], ALU.mult, ALU.add,
                                [y_r[t][oc], xt_r[oc]], [xt_r[oc]])
                        DMA("sp", Xs[:, tok0:tok0 + 512].rearrange("(k p) t -> p k t", p=128), xt[:], R=xt_r)
                first[0] = False
                P.flush()

        def phase_final():
            src = cur_src()
            with contextlib.ExitStack() as ph:
                def pb(name, shape, dt=F32):
                    return ph.enter_context(nc.sbuf_tensor(uname(name), list(shape), dt))
                pbank = Ring([(banks[i], bank_r[i]) for i in range(8)])
                xt2 = [pb("xtf%d" % i, [128, KC, 512]) for i in range(2)]
                xt2_r = [[Res() for _ in range(KC)] for _ in range(2)]
                ot2 = [pb("otf%d" % i, [128, KC, 512]) for i in range(2)]
                ot2_r = [[Res() for _ in range(KC)] for _ in range(2)]
                sq_ring = Ring([(pb("sqf%d" % i, [128, 512], BF16), Res()) for i in range(3)])
                tmp_ring = Ring([(pb("tmf%d" % i, [128, 512]), Res()) for i in range(3)])
                rstd = pb("rstdf", [128, 512]); rstd_r = Res()
                Af = AB[:, :, 64:72]
                for t in range(T // 512):
                    seq = (t * 512) // L
                    xt, xt_r = xt2[t % 2], xt2_r[t % 2]
                    ot, ot_r = ot2[t % 2], ot2_r[t % 2]
                    tok0 = t * 512
                    DMA("sp", xt[:], src[:, tok0:tok0 + 512].rearrange("(k p) t -> p k t", p=128), W=xt_r)
                    ps, ps_r = pbank.get()
                    for k in range(KC):
                        sq, sq_r = sq_ring.get()
                        ACT(sq[:], xt[:, k, :], AF.Square, [xt_r[k]], [sq_r])
                        MM(ps[:], ones_b, sq[:], k == 0, k == KC - 1, [sq_r, r_const], [ps_r])
                    ACT(rstd[:], ps[:], AF.Ln, [ps_r], [rstd_r], scale=1.0, bias=1024.0 * EPS)
                    ACT(rstd[:], rstd[:], AF.Exp, [rstd_r], [rstd_r], scale=-0.5)
                    for k in range(KC):
                        tm, tm_r = tmp_ring.get()
                        TT(tm[:], xt[:, k, :], rstd[:], ALU.mult, [xt_r[k], rstd_r], [tm_r])
                        ACT(ot[:, k, :], tm[:], AF.Identity, [tm_r], [ot_r[k]],
                            scale=Af[:, seq, k:k + 1], bias=mod[:, seq, 192 + k:193 + k])
                    DMA("sp", outT[:, tok0:tok0 + 512].rearrange("(k p) t -> p k t", p=128), ot[:], R=ot_r)
                P.flush()

        for li, l in enumerate(layers):
            phase_A(li, l)
            if CFG.get('upto', 99) >= 9:
                phase_B(li, l)
        phase_final()
    return nc


_PROG_CACHE = {}


def kernel(x, c, ada_w, ada_b, norm1_w, norm2_w, w_in, conv_w, a_log, dt_bias,
           dn_norm_w, gm_ln_w, gm_ln_b, gm_spatial_w, gm_spatial_b, w_out,
           ffn_w_gate, ffn_w_up, ffn_w_down, moe_router, moe_w_gate, moe_w_up,
           moe_w_down, final_ada_w, final_ada_b, final_norm_w):
    f = lambda a: np.ascontiguousarray(np.asarray(a, dtype=np.float32))
    x = f(x); c = f(c)
    B, L, _ = x.shape
    n_cores = CFG["n_cores"]
    NB = B // n_cores
    layers = list(CFG["layers"])
    key = (L, NB, tuple(layers))
    if key not in _PROG_CACHE:
        import time as _t, sys as _s
        _t0 = _t.time()
        _PROG_CACHE[key] = build(L, NB, layers)
        print("[kernel] build %.1fs" % (_t.time() - _t0), file=_s.stderr)
    nc = _PROG_CACHE[key]
    cons, sel = _consts()

    def pm(v, nchunk):
        v = f(v)
        lead = v.shape[:-1]
        return np.ascontiguousarray(np.moveaxis(v.reshape(*lead, nchunk, 128), -1, 0))

    ada_bT = pm(ada_b, 48).reshape(128, 4 * 48)
    fada_bT = pm(final_ada_b, 16).reshape(128, 16)
    nw = np.concatenate([np.stack([pm(norm1_w, 8), pm(norm2_w, 8)], axis=2).reshape(128, 64),
                         pm(final_norm_w, 8).reshape(128, 8)], axis=1)
    cwp = np.ascontiguousarray(np.transpose(pm(conv_w, 12), (0, 1, 3, 2))).reshape(128, 4 * 48)
    dnw = np.ascontiguousarray(f(dn_norm_w).T)
    alog_bc = np.ascontiguousarray(np.broadcast_to(f(a_log).reshape(1, 16), (128, 16)))
    dtb_bc = np.ascontiguousarray(np.broadcast_to(f(dt_bias).reshape(1, 16), (128, 16)))
    wsT = np.ascontiguousarray(np.transpose(f(gm_spatial_w), (0, 3, 1, 2))).reshape(4, 128, 512)
    gsb = f(gm_spatial_b).reshape(4, 512)
    shared = {
        "consts": cons.reshape(128, NCONST * 128), "sel": sel,
        "ada_w": f(ada_w), "ada_bT": ada_bT, "final_ada_w": f(final_ada_w), "fada_bT": fada_bT, "nw": nw,
        "w_in": f(w_in), "cw": cwp, "dnw": dnw, "alog_bc": alog_bc, "dtb_bc": dtb_bc,
        "gm_ln_w": f(gm_ln_w), "gm_ln_b": f(gm_ln_b), "wsT": wsT, "gsb": gsb, "w_out": f(w_out),
        "ffn_w_gate": f(ffn_w_gate), "ffn_w_up": f(ffn_w_up), "ffn_w_down": f(ffn_w_down),
        "moe_router": f(moe_router), "moe_w_gate": f(moe_w_gate), "moe_w_up": f(moe_w_up), "moe_w_down": f(moe_w_down),
    }
    if not any(l % 2 == 1 for l in layers):
        for k_ in ("moe_w_gate", "moe_w_up", "moe_w_down"):
            shared[k_] = np.zeros((1, 1, 1, 1), np.float32)
    in_maps = []
    for i in range(n_cores):
        xs = x[i * NB:(i + 1) * NB].reshape(NB * L, D)
        cs = c[i * NB:(i + 1) * NB]
        cTl = np.ascontiguousarray(np.transpose(cs.reshape(NB, KC, 128), (2, 1, 0))).reshape(128, KC * NB)
        m = dict(shared)
        m["xT"] = np.ascontiguousarray(xs.T)
        m["cT"] = cTl
        in_maps.append(m)
    import time as _t, sys as _s
    _t0 = _t.time()
    if CFG.get("sim"):
        return nc, in_maps
    res = run_bass_kernel_spmd(nc, in_maps, core_ids=list(range(n_cores)))
    print("[kernel] launch+transfer %.1fs" % (_t.time() - _t0), file=_s.stderr)
    out = np.empty((B, L, D), np.float32)
    for i in range(n_cores):
        out[i * NB:(i + 1) * NB] = res.results[i]["outT"].T.reshape(NB, L, D)
    return out
```

```python
import contextlib
import numpy as np
import concourse.bass as bass
import concourse.mybir as mybir
from concourse.bass_utils import run_bass_kernel_spmd

F32 = mybir.dt.float32
BF16 = mybir.dt.bfloat16
AF = mybir.ActivationFunctionType
ALU = mybir.AluOpType
AX = mybir.AxisListType

D = 1024
KC = 8
DIN = 3080
OFF_Z, OFF_BA, OFF_GU, OFF_GV = 1536, 2048, 2056, 2568
FF_DENSE = 2816
FF_MOE = 3584
NE = 8
EPS = 1e-6
N_CORES = 8

CFG = {"L": 4096, "NB": 2, "layers": [0, 1, 2, 3], "n_cores": 8}

ENGS = ["pe", "act", "dve", "pool", "sp"]
KEYS = ["pe", "act", "dve", "pool", "sp_dma", "pool_dma", "act_dma"]


class Res:
    __slots__ = ("w", "r", "x")

    def __init__(self, excl=False):
        self.w = None
        self.r = {}
        self.x = excl


class Prog:
    def __init__(self, nc, st):
        self.nc = nc
        self.q = {e: [] for e in ENGS}
        self.cnt = {k: 0 for k in KEYS}
        self.seen = {e: {} for e in ENGS}
        self.signaled = {k: set() for k in KEYS}
        self.rank_base = {k: 0 for k in KEYS}
        self.sems = {k: st.enter_context(nc.semaphore("s_" + k)) for k in KEYS}
        self.nops = 0
        self.mute = False

    def op(self, eng, fn, reads=(), writes=(), dma=False):
        if self.mute:
            return
        if any(r.x for r in reads):
            writes = list(writes) + [r for r in reads if r.x]
            reads = [r for r in reads if not r.x]
        key = eng + "_dma" if dma else eng
        idx = self.cnt[key] + 1
        self.cnt[key] = idx
        waits = {}
        for r in reads:
            if r.w is not None:
                k, v = r.w
                if not (k == "pe" and eng == "pe") and waits.get(k, 0) < v:
                    waits[k] = v
        for w in writes:
            if w.w is not None:
                k, v = w.w
                if not (k == "pe" and eng == "pe") and waits.get(k, 0) < v:
                    waits[k] = v
            for k, v in w.r.items():
                if k == eng:
                    continue
                if waits.get(k, 0) < v:
                    waits[k] = v
        wl = []
        seen = self.seen[eng]
        for k, v in waits.items():
            if seen.get(k, 0) >= v:
                continue
            seen[k] = v
            wl.append((k, v))
            self.signaled[k].add(v)
        for r in reads:
            if r.r.get(key, 0) < idx:
                r.r[key] = idx
        for w in writes:
            w.w = (key, idx)
            w.r = {}
        if dma:
            self.signaled[key].add(idx)
        self.q[eng].append((wl, fn, key, idx))
        self.nops += 1

    def barrier(self):
        tot = dict(self.cnt)
        for e in ENGS:
            wl = []
            for k, v in tot.items():
                if v == 0 or k == e:
                    continue
                if self.seen[e].get(k, 0) >= v:
                    continue
                self.seen[e][k] = v
                wl.append((k, v))
                self.signaled[k].add(v)
            if wl:
                self.q[e].append((wl, None, None, None))

    def flush(self):
        self.barrier()
        ranks = {}
        for k in KEYS:
            s = sorted(self.signaled[k])
            base = self.rank_base[k]
            ranks[k] = {v: base + i + 1 for i, v in enumerate(s)}
            self.rank_base[k] = base + len(s)
            self.signaled[k] = set()
        sems = self.sems
        q = self.q
        self.q = {e: [] for e in ENGS}

        def run(e):
            def body(engine):
                for wl, fn, key, idx in q[e]:
                    for k, v in wl:
                        engine.wait_ge(sems[k], ranks[k][v] * (16 if k.endswith("_dma") else 1))
                    if fn is None:
                        continue
                    ins = fn(engine)
                    if idx in ranks[key]:
                        ins.then_inc(sems[key], 16 if key.endswith("_dma") else 1)
            return body

        with self.nc.Block() as block:
            block.tensor(run("pe"))
            block.scalar(run("act"))
            block.vector(run("dve"))
            block.gpsimd(run("pool"))
            block.sync(run("sp"))


class Ring:
    def __init__(self, items):
        self.items = items
        self.i = 0

    def get(self):
        it = self.items[self.i % len(self.items)]
        self.i += 1
        return it


C_ID, C_ONES, C_TRIL, C_TRILS, C_TRIU, C_M0 = 0, 1, 2, 3, 4, 5
NCONST = 12


def _consts():
    c = np.zeros((NCONST, 128, 128), np.float32)
    i = np.arange(128)[:, None]
    j = np.arange(128)[None, :]
    c[C_ID] = (i == j)
    c[C_ONES] = 1.0
    c[C_TRIL] = (i >= j)
    c[C_TRILS] = (i > j)
    c[C_TRIU] = (i <= j)
    for l in range(7):
        b = 1 << l
        c[C_M0 + l] = ((i // (2 * b)) == (j // (2 * b))) & ((i % (2 * b)) >= b) & ((j % (2 * b)) < b)
    cons = np.ascontiguousarray(c.transpose(1, 0, 2))
    sel = np.zeros((8, 8, 128), np.float32)
    for e in range(8):
        sel[e, e, :] = 1.0
    return cons, sel.reshape(8, 1024)


def build(L, NB, layers):
    T = NB * L
    nl = len(layers)
    nc = bass.Bass("TRN2", target_bir_lowering=False)

    def din(name, shape, dt=F32):
        return nc.dram_tensor(name, list(shape), dt, kind="ExternalInput").ap()

    xT_in = din("xT", [D, T])
    outT = nc.dram_tensor("outT", [D, T], F32, kind="ExternalOutput").ap()
    Xs = nc.dram_tensor("Xs", [D, T], F32, kind="Internal").ap()
    cT_d = din("cT", [128, KC * NB])
    consts_d = din("consts", [128, NCONST * 128])
    sel_d = din("sel", [8, 1024])
    ada_w_d = din("ada_w", [4, D, 6 * D])
    ada_bT_d = din("ada_bT", [128, 4 * 48])
    fada_w_d = din("final_ada_w", [D, 2 * D])
    fada_bT_d = din("fada_bT", [128, 16])
    nw_d = din("nw", [128, 4 * 16 + 8])
    w_in_d = din("w_in", [4, D, DIN])
    cw_d = din("cw", [128, 4 * 48])
    dnw_d = din("dnw", [128, 4])
    alog_d = din("alog_bc", [128, 16])
    dtb_d = din("dtb_bc", [128, 16])
    lnw_d = din("gm_ln_w", [4, 512])
    lnb_d = din("gm_ln_b", [4, 512])
    wsT_d = din("wsT", [4, 128, 512])
    gsb_d = din("gsb", [4, 512])
    w_out_d = din("w_out", [4, D, D])
    fg_d = din("ffn_w_gate", [2, D, FF_DENSE])
    fu_d = din("ffn_w_up", [2, D, FF_DENSE])
    fd_d = din("ffn_w_down", [2, FF_DENSE, D])
    rt_d = din("moe_router", [2, D, NE])
    has_moe = any(l % 2 == 1 for l in layers)
    mg_d = din("moe_w_gate", [2, NE, D, FF_MOE] if has_moe else [1, 1, 1, 1])
    mu_d = din("moe_w_up", [2, NE, D, FF_MOE] if has_moe else [1, 1, 1, 1])
    md_d = din("moe_w_down", [2, NE, FF_MOE, D] if has_moe else [1, 1, 1, 1])

    with contextlib.ExitStack() as st:
        uid = [0]

        def uname(name):
            uid[0] += 1
            return "sb%d_%s" % (uid[0], name)

        def sb(name, shape, dt=F32):
            return st.enter_context(nc.sbuf_tensor(uname(name), list(shape), dt))

        P = Prog(nc, st)
        banks = [st.enter_context(nc.psum_tensor("bank%d" % i, [128, 512], F32)) for i in range(8)]
        bank_r = [Res(True) for _ in range(8)]

        cons = sb("cons", [128, NCONST, 128])
        cons_b = sb("cons_b", [128, 2, 128], BF16)
        onesrow_b = sb("onesrow_b", [1, 128], BF16)
        cT = sb("cT", [128, KC * NB])
        cact_b = sb("cact_b", [128, KC * NB], BF16)
        mod = sb("mod", [128, NB, 4 * 48 + 16])
        ada_bT = sb("ada_bT", [128, 4 * 48 + 16])
        nw = sb("nw", [128, 4 * 16 + 8])
        AB = sb("AB", [128, NB, (4 * 2 + 1) * 8])
        cw = sb("cw", [128, 4 * 48])
        dnw = sb("dnw", [128, 4])
        nA = sb("nA", [128, 16])
        dtb = sb("dtb", [128, 16])
        r_const = Res()

        def DMA(eng, out, in_, R=(), W=()):
            P.op(eng, lambda e: e.dma_start(out=out, in_=in_), R, W, dma=True)

        def MM(out, lhsT, rhs, start, stop, R, W):
            P.op("pe", lambda e: e.matmul(out, lhsT, rhs, start=start, stop=stop), R, W)

        def ACT(out, in_, func, R, W, scale=1.0, bias=0.0):
            P.op("act", lambda e: e.activation(out, in_, func, bias=bias, scale=scale), R, W)

        def TT(out, a, b, op, R, W, eng="dve"):
            P.op(eng, lambda e: e.tensor_tensor(out, a, b, op), R, W)

        def TS(out, a, s1, s2, op0, op1, R, W, eng="dve"):
            if s2 is None:
                P.op(eng, lambda e: e.tensor_scalar(out, a, s1, None, op0), R, W)
            else:
                P.op(eng, lambda e: e.tensor_scalar(out, a, s1, s2, op0, op1), R, W)

        def STT(out, a, s, b, op0, op1, R, W, eng="dve"):
            P.op(eng, lambda e: e.scalar_tensor_tensor(out, a, s, b, op0, op1), R, W)

        def CP(out, in_, R, W, eng="dve"):
            if eng == "act":
                P.op("act", lambda e: e.copy(out, in_), R, W)
            else:
                P.op(eng, lambda e: e.tensor_copy(out, in_), R, W)

        def RECIP(out, in_, R, W):
            P.op("dve", lambda e: e.reciprocal(out, in_), R, W)

        def MEMSET(ap, val, W, eng="pool"):
            P.op(eng, lambda e: e.memset(ap, val), (), W)

        DMA("sp", cons[:], consts_d.rearrange("p (a b) -> p a b", a=NCONST), W=[r_const])
        DMA("pool", cons_b[:], consts_d[:, 0:256].rearrange("p (a b) -> p a b", a=2), W=[r_const])
        DMA("sp", cT[:], cT_d, W=[r_const])
        DMA("sp", ada_bT[:, 0:192], ada_bT_d, W=[r_const])
        DMA("sp", ada_bT[:, 192:208], fada_bT_d, W=[r_const])
        DMA("sp", nw[:], nw_d, W=[r_const])
        DMA("sp", cw[:], cw_d, W=[r_const])
        DMA("sp", dnw[:], dnw_d, W=[r_const])
        DMA("sp", nA[:], alog_d, W=[r_const])
        DMA("sp", dtb[:], dtb_d, W=[r_const])
        MEMSET(onesrow_b[:], 1.0, [r_const])
        ACT(nA[:], nA[:], AF.Exp, [r_const], [r_const])
        TS(nA[:], nA[:], -1.0, None, ALU.mult, None, [r_const], [r_const])
        ACT(cact_b[:], cT[:], AF.Silu, [r_const], [r_const])

        ident_f = cons[:, C_ID, :]
        ones_f = cons[:, C_ONES, :]
        trils_f = cons[:, C_TRILS, :]
        triu_f = cons[:, C_TRIU, :]
        ident_b = cons_b[:, 0, :]
        ones_b = cons_b[:, 1, :]

        with contextlib.ExitStack() as ph:
            wbuf = [ph.enter_context(nc.sbuf_tensor(uname("adaw"), [128, KC, 1024], BF16)) for i in range(2)]
            wbuf_r = [Res(), Res()]
            mod_r = Res()
            pieces = []
            for li, l in enumerate(layers):
                for j in range(6):
                    pieces.append((ada_w_d[l, :, j * 1024:(j + 1) * 1024], li * 48 + j * 8))
            for j in range(2):
                pieces.append((fada_w_d[:, j * 1024:(j + 1) * 1024], 192 + j * 8))
            for pi, (src, col0) in enumerate(pieces):
                wb, wr = wbuf[pi % 2], wbuf_r[pi % 2]
                DMA("pool", wb[:], src.rearrange("(k p) n -> p k n", p=128), W=[wr])
                for oc in range(8):
                    bk = pi * 8 + oc
                    ps = banks[bk % 8][:, 0:NB]
                    pr = bank_r[bk % 8]
                    for k in range(KC):
                        MM(ps, wb[:, k, oc * 128:(oc + 1) * 128], cact_b[:, k * NB:(k + 1) * NB],
                           k == 0, k == KC - 1, [wr, r_const], [pr])
                    bcol = (layers[col0 // 48] * 48 + col0 % 48 + oc) if col0 < 192 else (192 + col0 - 192 + oc)
                    TS(mod[:, :, col0 + oc], ps, ada_bT[:, bcol:bcol + 1], None, ALU.add, None,
                       [pr, r_const], [mod_r])
            MEMSET(AB[:], 0.0, [mod_r])
            for li, l in enumerate(layers):
                for sub in range(2):
                    for b in range(NB):
                        sc = mod[:, b, li * 48 + (sub * 3 + 1) * 8: li * 48 + (sub * 3 + 2) * 8]
                        STT(AB[:, b, (li * 2 + sub) * 8:(li * 2 + sub + 1) * 8], sc, 1.0,
                            nw[:, l * 16 + sub * 8: l * 16 + sub * 8 + 8], ALU.add, ALU.mult, [mod_r, r_const], [mod_r])
            for b in range(NB):
                STT(AB[:, b, 64:72], mod[:, b, 200:208], 1.0, nw[:, 64:72], ALU.add, ALU.mult, [mod_r, r_const], [mod_r])
            TS(AB[:], AB[:], 32.0, None, ALU.mult, None, [mod_r], [mod_r])
            P.flush()

        def norm_mod(xt, xt_r, ncols, A, B, out_b, out_r, sq_ring, tmp_ring, rstd, rstd_r, ps, ps_r, out_f=None, out_f_r=None):
            for k in range(KC):
                sq, sq_r = sq_ring.get()
                ACT(sq[:, 0:ncols], xt[:, k, 0:ncols], AF.Square, [xt_r[k]], [sq_r])
                MM(ps[:, 0:ncols], ones_b, sq[:, 0:ncols], k == 0, k == KC - 1, [sq_r, r_const], [ps_r])
            ACT(rstd[:, 0:ncols], ps[:, 0:ncols], AF.Ln, [ps_r], [rstd_r], scale=1.0, bias=1024.0 * EPS)
            ACT(rstd[:, 0:ncols], rstd[:, 0:ncols], AF.Exp, [rstd_r], [rstd_r], scale=-0.5)
            for k in range(KC):
                tm, tm_r = tmp_ring.get()
                TT(tm[:, 0:ncols], xt[:, k, 0:ncols], rstd[:, 0:ncols], ALU.mult, [xt_r[k], rstd_r], [tm_r])
                if out_f is not None:
                    ACT(out_f[:, k, 0:ncols], tm[:, 0:ncols], AF.Identity, [tm_r], [out_f_r[k]],
                        scale=A[:, k:k + 1], bias=B[:, k:k + 1])
                ACT(out_b[:, k, 0:ncols], tm[:, 0:ncols], AF.Identity, [tm_r], [out_r[k]],
                    scale=A[:, k:k + 1], bias=B[:, k:k + 1])

        first = [True]

        def cur_src():
            return xT_in if first[0] else Xs

        def phase_A(li, l):
            TTK = 256
            NCH = TTK // 128
            src = cur_src()
            with contextlib.ExitStack() as ph:
                def pb(name, shape, dt=F32):
                    return ph.enter_context(nc.sbuf_tensor(uname(name), list(shape), dt))

                class _BankRing:
                    def __init__(self, ncol):
                        self.ncol = ncol

                    def get(self):
                        i = bring[0] % 8
                        bring[0] += 1
                        return (banks[i][:, 0:self.ncol], bank_r[i])
                bring = [0]
                wide = _BankRing(256)
                small = _BankRing(128)
                fullr = _BankRing(512)

                w_in_b = pb("w_in_b", [128, KC, DIN], BF16)
                w_out_b = pb("w_out_b", [128, KC, D], BF16)
                wsT_f = pb("wsT_f", [128, 512])
                wsT_b = pb("wsT_b", [128, 512], BF16)
                brow_b = pb("brow_b", [1, 512], BF16)
                lnw = pb("lnw", [128, 512])
                lnb = pb("lnb", [128, 512])
                r_w = Res()
                for k in range(KC):
                    DMA("pool", w_in_b[:, k, :], w_in_d[l, k * 128:(k + 1) * 128, :], W=[r_w])
                DMA("pool", w_out_b[:], w_out_d[l].rearrange("(k p) n -> p k n", p=128), W=[r_w])
                DMA("sp", wsT_f[:], wsT_d[l], W=[r_w])
                DMA("pool", brow_b[:], gsb_d[l:l + 1, :], W=[r_w])
                DMA("sp", lnw[:], lnw_d[l:l + 1, :].to_broadcast([128, 512]), W=[r_w])
                DMA("sp", lnb[:], lnb_d[l:l + 1, :].to_broadcast([128, 512]), W=[r_w])
                for g in range(4):
                    TT(wsT_b[:, g * 128:(g + 1) * 128], wsT_f[:, g * 128:(g + 1) * 128], triu_f, ALU.mult,
                       [r_w, r_const], [r_w])

                xtL = [(pb("xt%d" % i, [128, KC, TTK]), [Res() for _ in range(KC)]) for i in range(2)]
                hTL = [(pb("hT%d" % i, [128, KC, TTK], BF16), [Res() for _ in range(KC)]) for i in range(2)]
                sq_ring = Ring([(pb("sqa%d" % i, [128, TTK], BF16), Res()) for i in range(3)])
                tmp_ring = Ring([(pb("tma%d" % i, [128, TTK]), Res()) for i in range(3)])
                rstd = pb("rstd", [128, TTK]); rstd_r = Res()
                pre = pb("pre", [128, 12, TTK + 3], BF16); pre_r = [Res() for _ in range(12)]
                qs = pb("qs", [128, 4, TTK], BF16); ks = pb("ks", [128, 4, TTK], BF16); vs_b = pb("vs_b", [128, 4, TTK], BF16)
                qkv_r = [Res() for _ in range(12)]
                qn_b = pb("qn_b", [128, 4, TTK], BF16); kn_b = pb("kn_b", [128, 4, TTK], BF16)
                qn_r = [Res() for _ in range(4)]; kn_r = [Res() for _ in range(4)]
                zs = pb("zs", [128, 4, TTK], BF16); zs_r = [Res() for _ in range(4)]
                gus = pb("gus", [128, 4, TTK], BF16); gus_r = [Res() for _ in range(4)]
                gvf_ring = Ring([(pb("gvf%d" % i, [128, 512]), Res()) for i in range(2)])
                gv_b = pb("gv_b", [128, NCH, 512], BF16); gv_r = [Res() for _ in range(NCH)]
                stat = pb("stat", [128, 8]); stat_r = Res()
                mixT = pb("mixT", [128, KC, TTK], BF16); mix_r = [Res() for _ in range(KC)]
                oT = pb("oT", [128, 4, TTK]); oT_r = [Res() for _ in range(4)]
                ba = pb("ba", [128, NCH * 8]); ba_r = Res()
                g_tm = pb("g_tm", [128, NCH * 4]); beta_tm = pb("beta_tm", [128, NCH * 4])
                G_tm = pb("G_tm", [128, NCH * 4]); e1 = pb("e1", [128, NCH * 4]); e2 = pb("e2", [128, NCH * 4])
                eGl = pb("eGl", [128, NCH * 4]); sm_r = Res()
                S_f = pb("S_f", [128, 4, 128]); S_b = pb("S_b", [128, 4, 128], BF16)
                S_r = [Res() for _ in range(4)]; Sb_r = [Res() for _ in range(4)]
                def hb(name, dt=F32):
                    return [(pb("%s%d" % (name, h), [128, 128], dt), Res()) for h in range(4 * NCH)]
                gbc = hb("gbc"); eG = hb("eG", BF16); dd = hb("dd", BF16); dt_ = hb("dt_", BF16)
                Am = hb("Am", BF16); AM = [hb("AM%d_" % lv, BF16) for lv in range(2)]
                U0 = hb("U0", BF16); U1 = hb("U1", BF16); T0 = hb("T0", BF16); T1 = hb("T1", BF16)
                P1 = hb("P1", BF16); attnT = hb("attnT", BF16); qd = hb("qd", BF16)
                kbd = hb("kbd", BF16); kdec = hb("kdec", BF16); vb = hb("vb", BF16)
                u_f = hb("u_f"); wT = hb("wT", BF16); vnew = hb("vnew", BF16)

                A1 = AB[:, :, (li * 2) * 8:(li * 2 + 1) * 8]
                c4 = l * 4

                for seq in range(NB):
                    for h in range(4):
                        MEMSET(S_f[:, h, :], 0.0, [S_r[h]])
                        MEMSET(S_b[:, h, :], 0.0, [Sb_r[h]])
                    for j in range(12):
                        MEMSET(pre[:, j, 0:3], 0.0, [pre_r[j]])
                    def norm_part(seq_, it_, bi_):
                        xt_, xt_r_ = xtL[bi_]
                        hT_, hT_r_ = hTL[bi_]
                        tk = seq_ * L + it_ * TTK
                        DMA("sp", xt_[:], src[:, tk:tk + TTK].rearrange("(k p) t -> p k t", p=128), W=xt_r_)
                        ps_, ps_r_ = wide.get()
                        norm_mod(xt_, xt_r_, TTK, A1[:, seq_, :], mod[:, seq_, li * 48: li * 48 + 8], hT_, hT_r_,
                                 sq_ring, tmp_ring, rstd, rstd_r, ps_, ps_r_)
                    NTL = L // TTK
                    if seq == 0:
                        norm_part(0, 0, 0)
                    for it in range(NTL):
                        tok0 = seq * L + it * TTK
                        gidx = seq * NTL + it
                        xt, xt_r = xtL[gidx % 2]
                        hT, hT_r = hTL[gidx % 2]
                        def proj(col0):
                            ps, ps_r = wide.get()
                            for k in range(KC):
                                MM(ps, w_in_b[:, k, col0:col0 + 128], hT[:, k, :], k == 0, k == KC - 1,
                                   [r_w, hT_r[k]], [ps_r])
                            return ps, ps_r
                        for j in range(12):
                            ps, ps_r = proj(j * 128)
                            CP(pre[:, j, 3:3 + TTK], ps, [ps_r], [pre_r[j]], eng="act")
                            acc_t, acc_r = tmp_ring.get()
                            acc = acc_t[:]
                            cwj = cw[:, l * 48 + j * 4: l * 48 + j * 4 + 4]
                            TS(acc, pre[:, j, 0:TTK], cwj[:, 0:1], None, ALU.mult, None, [pre_r[j], r_const], [acc_r])
                            for tp in range(1, 4):
                                STT(acc, pre[:, j, tp:tp + TTK], cwj[:, tp:tp + 1], acc, ALU.mult, ALU.add,
                                    [pre_r[j], acc_r, r_const], [acc_r])
                            dst = (qs, ks, vs_b)[j // 4][:, j % 4, :]
                            ACT(dst, acc, AF.Silu, [acc_r], [qkv_r[j]])
                            CP(pre[:, j, 0:3], pre[:, j, TTK:TTK + 3], [pre_r[j]], [pre_r[j]], eng="pool")
                        for h in range(4):
                            ps, ps_r = proj(OFF_Z + h * 128)
                            ACT(zs[:, h, :], ps, AF.Silu, [ps_r], [zs_r[h]])
                        for g in range(4):
                            ps, ps_r = proj(OFF_GU + g * 128)
                            ACT(gus[:, g, :], ps, AF.Gelu_apprx_tanh, [ps_r], [gus_r[g]])
                        P.mute = CFG.get('upto', 99) < 2
                        psba, psba_r = small.get()
                        for c in range(NCH):
                            for k in range(KC):
                                MM(psba[:, c * 8:(c + 1) * 8], hT[:, k, c * 128:(c + 1) * 128],
                                   w_in_b[:, k, OFF_BA:OFF_BA + 8], k == 0, k == KC - 1, [r_w, hT_r[k]], [psba_r])
                        CP(ba[:], psba[:, 0:NCH * 8], [psba_r], [ba_r])
                        for c in range(NCH):
                            ACT(beta_tm[:, c * 4:c * 4 + 4], ba[:, c * 8:c * 8 + 4], AF.Sigmoid, [ba_r], [sm_r])
                            TT(g_tm[:, c * 4:c * 4 + 4], ba[:, c * 8 + 4:c * 8 + 8], dtb[:, c4:c4 + 4], ALU.add, [ba_r, r_const], [sm_r])
                        g2d = g_tm[:]
                        ACT(g2d, g2d, AF.Exp, [sm_r], [sm_r])
                        ACT(g2d, g2d, AF.Ln, [sm_r], [sm_r], scale=1.0, bias=1.0)
                        for c in range(NCH):
                            TT(g_tm[:, c * 4:c * 4 + 4], g_tm[:, c * 4:c * 4 + 4], nA[:, c4:c4 + 4], ALU.mult, [sm_r, r_const], [sm_r])
                        for c in range(NCH):
                            psg, psg_r = fullr.get()
                            for k in range(KC):
                                MM(psg[:], hT[:, k, c * 128:(c + 1) * 128], w_in_b[:, k, OFF_GV:OFF_GV + 512],
                                   k == 0, k == KC - 1, [r_w, hT_r[k]], [psg_r])
                            gvf, gvf_r = gvf_ring.get()
                            ACT(gvf[:], psg[:], AF.Gelu_apprx_tanh, [psg_r], [gvf_r])
                            P.op("dve", lambda e, gvf=gvf, c=c: e.reduce_sum(stat[:, c:c + 1], gvf[:], AX.X), [gvf_r], [stat_r])
                            TS(stat[:, c:c + 1], stat[:, c:c + 1], 1.0 / 512, None, ALU.mult, None, [stat_r], [stat_r])
                            TS(gvf[:], gvf[:], stat[:, c:c + 1], None, ALU.subtract, None, [gvf_r, stat_r], [gvf_r])
                            sq2, sq2_r = gvf_ring.get()
                            TT(sq2[:], gvf[:], gvf[:], ALU.mult, [gvf_r], [sq2_r])
                            P.op("dve", lambda e, sq2=sq2, c=c: e.reduce_sum(stat[:, 4 + c:5 + c], sq2[:], AX.X), [sq2_r], [stat_r])
                            ACT(stat[:, 4 + c:5 + c], stat[:, 4 + c:5 + c], AF.Ln, [stat_r], [stat_r], scale=1.0 / 512, bias=EPS)
                            ACT(stat[:, 4 + c:5 + c], stat[:, 4 + c:5 + c], AF.Exp, [stat_r], [stat_r], scale=-0.5)
                            STT(gvf[:], gvf[:], stat[:, 4 + c:5 + c], lnw[:], ALU.mult, ALU.mult, [gvf_r, stat_r, r_w], [gvf_r])
                            TT(gv_b[:, c, :], gvf[:], lnb[:], ALU.add, [gvf_r, r_w], [gv_r[c]])
                        P.mute = CFG.get('upto', 99) < 3
                        for g in range(4):
                            ps, ps_r = wide.get()
                            for c in range(NCH):
                                MM(ps[:, c * 128:(c + 1) * 128], gv_b[:, c, g * 128:(g + 1) * 128],
                                   wsT_b[:, g * 128:(g + 1) * 128], True, False, [gv_r[c], r_w], [ps_r])
                                MM(ps[:, c * 128:(c + 1) * 128], onesrow_b[0:1, :], brow_b[0:1, g * 128:(g + 1) * 128],
                                   False, True, [r_const, r_w], [ps_r])
                            TT(mixT[:, 4 + g, :], ps, gus[:, g, :], ALU.mult, [ps_r, gus_r[g]], [mix_r[4 + g]])
                        P.mute = CFG.get('upto', 99) < 4
                        for h in range(4):
                            for (srcb, r_i, dstb, dst_r, scl) in ((qs, h, qn_b, qn_r, 128.0 ** -0.5), (ks, 4 + h, kn_b, kn_r, 1.0)):
                                sq, sq_r = sq_ring.get()
                                ACT(sq[:], srcb[:, h, :], AF.Square, [qkv_r[r_i]], [sq_r])
                                ps, ps_r = wide.get()
                                MM(ps, ones_b, sq[:], True, True, [sq_r, r_const], [ps_r])
                                tm, tm_r = tmp_ring.get()
                                ACT(tm[:], ps, AF.Ln, [ps_r], [tm_r], scale=1.0, bias=EPS)
                                ACT(tm[:], tm[:], AF.Exp, [tm_r], [tm_r], scale=-0.5)
                                STT(dstb[:, h, :], srcb[:, h, :], scl, tm[:], ALU.mult, ALU.mult, [qkv_r[r_i], tm_r], [dst_r[h]])
                        psl, psl_r = small.get()
                        for c in range(NCH):
                            MM(psl[:, c * 4:(c + 1) * 4], ones_f, g_tm[:, c * 4:c * 4 + 4], True, True, [sm_r, r_const], [psl_r])
                            MM(psl[:, 16 + c * 4:16 + (c + 1) * 4], triu_f, g_tm[:, c * 4:c * 4 + 4], True, True, [sm_r, r_const], [psl_r])
                        sm2_r = Res()
                        CP(G_tm[:], psl[:, 16:16 + NCH * 4], [psl_r], [sm2_r])
                        ACT(eGl[:], psl[:, 0:NCH * 4], AF.Exp, [psl_r], [sm2_r])
                        TT(e2[:], psl[:, 0:NCH * 4], G_tm[:],
                           ALU.subtract, [psl_r, sm2_r], [sm2_r])
                        ACT(e2[:], e2[:], AF.Exp, [sm2_r], [sm2_r])
                        ACT(e1[:], G_tm[:], AF.Exp, [sm2_r], [sm2_r])
                        TT(e1[:], e1[:],
                           beta_tm[:], ALU.mult, [sm2_r, sm_r], [sm2_r])
                        P.mute = CFG.get('upto', 99) < 5
                        if gidx + 1 < NB * NTL:
                            mute_save = P.mute
                            P.mute = False
                            norm_part((gidx + 1) // NTL, (gidx + 1) % NTL, (gidx + 1) % 2)
                            P.mute = mute_save
                        CHN = [(c, h) for c in range(NCH) for h in range(4)]
                        def csl(c):
                            return slice(c * 128, (c + 1) * 128)
                        def col(c, h):
                            return slice(c * 4 + h, c * 4 + h + 1)
                        psG = {}
                        for (c, h) in CHN:
                            i = c * 4 + h
                            TS(gbc[i][0][:], ones_f, g_tm[:, col(c, h)], None, ALU.mult, None, [sm_r, r_const], [gbc[i][1]])
                            psG[i] = small.get()
                            MM(psG[i][0], gbc[i][0][:], triu_f, True, True, [gbc[i][1], r_const], [psG[i][1]])
                        for (c, h) in CHN:
                            i = c * 4 + h
                            pg, pg_r = psG[i]
                            Gc = G_tm[:, col(c, h)]
                            ACT(eG[i][0][:], pg, AF.Exp, [pg_r], [eG[i][1]])
                            TS(dd[i][0][:], pg, Gc, 0.0, ALU.subtract, ALU.max, [pg_r, sm2_r], [dd[i][1]])
                            TS(dt_[i][0][:], pg, Gc, 0.0, ALU.subtract, ALU.min, [pg_r, sm2_r], [dt_[i][1]])
                            ACT(dd[i][0][:], dd[i][0][:], AF.Exp, [dd[i][1]], [dd[i][1]], scale=-1.0)
                            ACT(dt_[i][0][:], dt_[i][0][:], AF.Exp, [dt_[i][1]], [dt_[i][1]])
                            TT(dd[i][0][:], dd[i][0][:], trils_f, ALU.mult, [dd[i][1], r_const], [dd[i][1]], eng="pool")
                            TT(dt_[i][0][:], dt_[i][0][:], triu_f, ALU.mult, [dt_[i][1], r_const], [dt_[i][1]], eng="pool")
                            TT(qd[i][0][:], qn_b[:, h, csl(c)], eG[i][0][:], ALU.mult, [qn_r[h], eG[i][1]], [qd[i][1]])
                        for (c, h) in CHN:
                            i = c * 4 + h
                            cs = csl(c)
                            pk, pk_r = small.get()
                            MM(pk, kn_b[:, h, cs], kn_b[:, h, cs], True, True, [kn_r[h]], [pk_r])
                            STT(Am[i][0][:], pk, beta_tm[:, col(c, h)], dd[i][0][:], ALU.mult, ALU.mult,
                                [pk_r, sm_r, dd[i][1]], [Am[i][1]])
                            pq, pq_r = small.get()
                            MM(pq, kn_b[:, h, cs], qn_b[:, h, cs], True, True, [kn_r[h], qn_r[h]], [pq_r])
                            TT(attnT[i][0][:], pq, dt_[i][0][:], ALU.mult, [pq_r, dt_[i][1]], [attnT[i][1]])
                            pt, pt_r = small.get()
                            MM(pt, kn_b[:, h, cs], ident_b, True, True, [kn_r[h], r_const], [pt_r])
                            TS(kbd[i][0][:], pt, e1[:, col(c, h)], None, ALU.mult, None, [pt_r, sm2_r], [kbd[i][1]])
                            ACT(kdec[i][0][:], pt, AF.Identity, [pt_r, sm2_r], [kdec[i][1]], scale=e2[:, col(c, h)])
                            pv, pv_r = small.get()
                            MM(pv, vs_b[:, h, cs], ident_b, True, True, [qkv_r[8 + h], r_const], [pv_r])
                            TS(vb[i][0][:], pv, beta_tm[:, col(c, h)], None, ALU.mult, None, [pv_r, sm_r], [vb[i][1]])
                        P.mute = CFG.get('upto', 99) < 6
                        Ucur = {}; Tcur = {}
                        for (c, h) in CHN:
                            i = c * 4 + h
                            a0, a0_r = AM[0][i]
                            TT(a0[:], Am[i][0][:], cons[:, C_M0, :], ALU.mult, [Am[i][1], r_const], [a0_r], eng="pool")
                            pt, pt_r = small.get()
                            MM(pt, a0[:], ident_b, True, True, [a0_r, r_const], [pt_r])
                            TT(U0[i][0][:], ident_f, pt, ALU.subtract, [pt_r, r_const], [U0[i][1]])
                            TT(T0[i][0][:], ident_f, a0[:], ALU.subtract, [a0_r, r_const], [T0[i][1]], eng="pool")
                            Ucur[i] = U0[i]; Tcur[i] = T0[i]
                        for lv in range(1, 7):
                            pp = {}
                            for i in range(NCH * 4):
                                al, al_r = AM[lv % 2][i]
                                TT(al[:], Am[i][0][:], cons[:, C_M0 + lv, :], ALU.mult, [Am[i][1], r_const], [al_r], eng="pool")
                                pp[i] = small.get()
                                MM(pp[i][0], al[:], Ucur[i][0][:], True, True, [al_r, Ucur[i][1]], [pp[i][1]])
                            for i in range(NCH * 4):
                                CP(P1[i][0][:], pp[i][0], [pp[i][1]], [P1[i][1]], eng="act")
                            px = {}
                            for i in range(NCH * 4):
                                px[i] = small.get()
                                MM(px[i][0], Tcur[i][0][:], P1[i][0][:], True, True, [Tcur[i][1], P1[i][1]], [px[i][1]])
                            for i in range(NCH * 4):
                                Un = U1[i] if Ucur[i] is U0[i] else U0[i]
                                TT(Un[0][:], Ucur[i][0][:], px[i][0], ALU.subtract, [Ucur[i][1], px[i][1]], [Un[1]])
                                Ucur[i] = Un
                            if lv < 6:
                                ptt = {}
                                for i in range(NCH * 4):
                                    ptt[i] = small.get()
                                    MM(ptt[i][0], Ucur[i][0][:], ident_b, True, True, [Ucur[i][1], r_const], [ptt[i][1]])
                                for i in range(NCH * 4):
                                    Tn = T1[i] if Tcur[i] is T0[i] else T0[i]
                                    CP(Tn[0][:], ptt[i][0], [ptt[i][1]], [Tn[1]], eng="act")
                                    Tcur[i] = Tn
                        P.mute = CFG.get('upto', 99) < 7
                        for i in range(NCH * 4):
                            pu, pu_r = small.get()
                            MM(pu, Ucur[i][0][:], vb[i][0][:], True, True, [Ucur[i][1], vb[i][1]], [pu_r])
                            CP(u_f[i][0][:], pu, [pu_r], [u_f[i][1]], eng="act")
                            pw, pw_r = small.get()
                            MM(pw, kbd[i][0][:], Ucur[i][0][:], True, True, [kbd[i][1], Ucur[i][1]], [pw_r])
                            CP(wT[i][0][:], pw, [pw_r], [wT[i][1]])
                        for c in range(NCH):
                            cs = csl(c)
                            pws = {}
                            for h in range(4):
                                i = c * 4 + h
                                pws[h] = small.get()
                                MM(pws[h][0], wT[i][0][:], S_b[:, h, :], True, True, [wT[i][1], Sb_r[h]], [pws[h][1]])
                            for h in range(4):
                                i = c * 4 + h
                                TT(vnew[i][0][:], u_f[i][0][:], pws[h][0], ALU.subtract, [u_f[i][1], pws[h][1]], [vnew[i][1]])
                            for h in range(4):
                                i = c * 4 + h
                                po, po_r = small.get()
                                MM(po, S_b[:, h, :], qd[i][0][:], True, False, [Sb_r[h], qd[i][1]], [po_r])
                                MM(po, vnew[i][0][:], attnT[i][0][:], False, True, [vnew[i][1], attnT[i][1]], [po_r])
                                CP(oT[:, h, cs], po, [po_r], [oT_r[h]], eng="act")
                                pS, pS_r = small.get()
                                MM(pS, kdec[i][0][:], vnew[i][0][:], True, True, [kdec[i][1], vnew[i][1]], [pS_r])
                                STT(S_f[:, h, :], S_f[:, h, :], eGl[:, col(c, h)], pS, ALU.mult, ALU.add,
                                    [S_r[h], sm2_r, pS_r], [S_r[h]])
                                CP(S_b[:, h, :], S_f[:, h, :], [S_r[h]], [Sb_r[h]], eng="act")
                        P.mute = CFG.get('upto', 99) < 8
                        for h in range(4):
                            sq, sq_r = sq_ring.get()
                            ACT(sq[:], oT[:, h, :], AF.Square, [oT_r[h]], [sq_r])
                            ps, ps_r = wide.get()
                            MM(ps, ones_b, sq[:], True, True, [sq_r, r_const], [ps_r])
                            tm, tm_r = tmp_ring.get()
                            ACT(tm[:], ps, AF.Ln, [ps_r], [tm_r], scale=1.0 / 128, bias=EPS)
                            ACT(tm[:], tm[:], AF.Exp, [tm_r], [tm_r], scale=-0.5)
                            TT(tm[:], tm[:], oT[:, h, :], ALU.mult, [tm_r, oT_r[h]], [tm_r])
                            STT(mixT[:, h, :], tm[:], dnw[:, l:l + 1], zs[:, h, :], ALU.mult, ALU.mult,
                                [tm_r, r_const, zs_r[h]], [mix_r[h]])
                        P.mute = CFG.get('upto', 99) < 0
                        for oc in range(KC):
                            ps, ps_r = wide.get()
                            for k in range(KC):
                                MM(ps, w_out_b[:, k, oc * 128:(oc + 1) * 128], mixT[:, k, :], k == 0, k == KC - 1,
                                   [r_w, mix_r[k]], [ps_r])
                            g1 = mod[:, seq, li * 48 + 16 + oc: li * 48 + 17 + oc]
                            STT(xt[:, oc, :], ps, g1, xt[:, oc, :], ALU.mult, ALU.add, [ps_r, xt_r[oc]], [xt_r[oc]])
                        DMA("sp", Xs[:, tok0:tok0 + TTK].rearrange("(k p) t -> p k t", p=128), xt[:], R=xt_r)
                first[0] = False
                P.flush()

        def phase_B(li, l):
            moe = (l % 2 == 1)
            j = l // 2
            FF = FF_MOE if moe else FF_DENSE
            nexp = NE if moe else 1
            TB = min(1024, L)
            NT = TB // 512
            src = cur_src()
            with contextlib.ExitStack() as ph:
                def pb(name, shape, dt=F32):
                    return ph.enter_context(nc.sbuf_tensor(uname(name), list(shape), dt))
                pbank = Ring([(banks[i], bank_r[i]) for i in range(8)])
                h2b = pb("h2b", [128, KC, TB], BF16); h2b_r = [[Res() for _ in range(KC)] for _ in range(NT)]
                yacc = pb("yacc", [128, KC, TB]); y_r = [[Res() for _ in range(KC)] for _ in range(NT)]
                xt2 = [pb("xtb%d" % i, [128, KC, 512]) for i in range(2)]
                xt2_r = [[Res() for _ in range(KC)] for _ in range(2)]
                sq_ring = Ring([(pb("sqb%d" % i, [128, 512], BF16), Res()) for i in range(3)])
                tmp_ring = Ring([(pb("tmb%d" % i, [128, 512]), Res()) for i in range(3)])
                sg_ring = Ring([(pb("sg%d" % i, [128, 512]), Res()) for i in range(3)])
                hid = [[(pb("hid%d_%d" % (i, f), [128, 512], BF16), Res()) for f in range(4)] for i in range(2)]
                rstd = pb("rstdb", [128, 512]); rstd_r = Res()
                wg = [pb("wg%d" % i, [128, KC, 512], BF16) for i in range(2)]
                wu = [pb("wu%d" % i, [128, KC, 512], BF16) for i in range(2)]
                wd = [pb("wd%d" % i, [128, 4, D], BF16) for i in range(2)]
                w_r = [Res(), Res()]
                if moe:
                    h2f = pb("h2f", [128, KC, 512]); h2f_r = [Res() for _ in range(KC)]
                    wr = pb("wr", [128, KC, NE]); wr_r = Res()
                    DMA("sp", wr[:], rt_d[j].rearrange("(k p) e -> p k e", p=128), W=[wr_r])
                    sel = pb("sel", [8, 1024]); sel_r = Res()
                    DMA("sp", sel[:], sel_d, W=[sel_r])
                    comb = pb("comb", [128, TB // 128, NE]); comb_r = Res()
                    combT = pb("combT", [8, TB]); combT_r = Res()
                    cbc = [pb("cbc%d" % i, [128, TB]) for i in range(2)]
                    cbc_r = [[Res() for _ in range(NT)] for _ in range(2)]
                    lg = pb("lg", [128, NE]); lg2 = pb("lg2", [128, NE]); mk1 = pb("mk1", [128, NE]); mk2 = pb("mk2", [128, NE])
                    m12 = pb("m12", [128, 4]); lg_r = Res()
                A2 = AB[:, :, (li * 2 + 1) * 8:(li * 2 + 2) * 8]
                pieces = []
                for e in range(nexp):
                    f0 = 0
                    while f0 < FF:
                        pw_ = min(512, FF - f0)
                        pieces.append((e, f0, pw_))
                        f0 += pw_

                def load_piece(pi):
                    e, f0, pw_ = pieces[pi]
                    bi = pi % 2
                    if moe:
                        g_src, u_src, d_src = mg_d[j, e], mu_d[j, e], md_d[j, e]
                    else:
                        g_src, u_src, d_src = fg_d[j], fu_d[j], fd_d[j]
                    DMA("pool", wg[bi][:, :, 0:pw_], g_src[:, f0:f0 + pw_].rearrange("(k p) n -> p k n", p=128), W=[w_r[bi]])
                    DMA("pool", wu[bi][:, :, 0:pw_], u_src[:, f0:f0 + pw_].rearrange("(k p) n -> p k n", p=128), W=[w_r[bi]])
                    DMA("pool", wd[bi][:, 0:pw_ // 128, :], d_src[f0:f0 + pw_, :].rearrange("(f p) n -> p f n", p=128), W=[w_r[bi]])

                xload = [0]
                for blk in range(T // TB):
                    seq = (blk * TB) // L
                    btok = blk * TB
                    load_piece(0)
                    for t in range(NT):
                        xb = xload[0] % 2; xload[0] += 1
                        xt, xt_r = xt2[xb], xt2_r[xb]
                        tok0 = btok + t * 512
                        DMA("sp", xt[:], src[:, tok0:tok0 + 512].rearrange("(k p) t -> p k t", p=128), W=xt_r)
                        ps, ps_r = pbank.get()
                        class _V:
                            def __getitem__(self, idx):
                                p_, k_, c_ = idx
                                return h2b[p_, k_, t * 512 + (c_.start or 0): t * 512 + (c_.stop or 512)]
                        norm_mod(xt, xt_r, 512, A2[:, seq, :], mod[:, seq, li * 48 + 24: li * 48 + 32], _V(), h2b_r[t],
                                 sq_ring, tmp_ring, rstd, rstd_r, ps, ps_r,
                                 out_f=(h2f if moe else None), out_f_r=(h2f_r if moe else None))
                        if moe:
                            P.mute = CFG.get('moe_upto', 99) < 1
                            for c in range(4):
                                gc = t * 4 + c
                                pl, pl_r = pbank.get()
                                for k in range(KC):
                                    MM(pl[:, 0:NE], h2f[:, k, c * 128:(c + 1) * 128], wr[:, k, :], k == 0, k == KC - 1,
                                       [h2f_r[k], wr_r], [pl_r])
                                CP(lg[:], pl[:, 0:NE], [pl_r], [lg_r])
                                P.op("dve", lambda e: e.reduce_max(m12[:, 0:1], lg[:], AX.X), [lg_r], [lg_r])
                                TS(mk1[:], lg[:], m12[:, 0:1], None, ALU.is_equal, None, [lg_r], [lg_r])
                                STT(lg2[:], mk1[:], -1e30, lg[:], ALU.mult, ALU.add, [lg_r], [lg_r])
                                P.op("dve", lambda e: e.reduce_max(m12[:, 1:2], lg2[:], AX.X), [lg_r], [lg_r])
                                TS(mk2[:], lg2[:], m12[:, 1:2], None, ALU.is_equal, None, [lg_r], [lg_r])
                                TT(m12[:, 2:3], m12[:, 1:2], m12[:, 0:1], ALU.subtract, [lg_r], [lg_r])
                                ACT(m12[:, 2:3], m12[:, 2:3], AF.Exp, [lg_r], [lg_r])
                                TS(m12[:, 2:3], m12[:, 2:3], 1.0, None, ALU.add, None, [lg_r], [lg_r])
                                RECIP(m12[:, 2:3], m12[:, 2:3], [lg_r], [lg_r])
                                TS(m12[:, 3:4], m12[:, 2:3], -1.0, 1.0, ALU.mult, ALU.add, [lg_r], [lg_r])
                                TS(mk1[:], mk1[:], m12[:, 2:3], None, ALU.mult, None, [lg_r], [lg_r])
                                STT(comb[:, gc, :], mk2[:], m12[:, 3:4], mk1[:], ALU.mult, ALU.add, [lg_r], [comb_r])
                                P.mute = CFG.get('moe_upto', 99) < 2
                                pc, pc_r = pbank.get()
                                MM(pc[0:8, 0:128], comb[:, gc, :], ident_f, True, True, [comb_r, r_const], [pc_r])
                                CP(combT[:, gc * 128:(gc + 1) * 128], pc[0:8, 0:128], [pc_r], [combT_r])
                    P.mute = False
                    first_acc = True
                    for pi, (e, f0, pw_) in enumerate(pieces):
                        if pi + 1 < len(pieces):
                            load_piece(pi + 1)
                        bi = pi % 2
                        nf = pw_ // 128
                        if moe and f0 == 0:
                            P.mute = CFG.get('moe_upto', 99) < 3
                            for t in range(NT):
                                pc, pc_r = pbank.get()
                                MM(pc[:], sel[0:8, e * 128:(e + 1) * 128], combT[:, t * 512:(t + 1) * 512], True, True,
                                   [sel_r, combT_r], [pc_r])
                                CP(cbc[e % 2][:, t * 512:(t + 1) * 512], pc[:], [pc_r], [cbc_r[e % 2][t]], eng="act")
                        P.mute = False
                        for t in range(NT):
                            ts_ = slice(t * 512, (t + 1) * 512)
                            hb_ = hid[(pi * NT + t) % 2]
                            for f in range(nf):
                                pg, pg_r = pbank.get()
                                for k in range(KC):
                                    MM(pg[:], wg[bi][:, k, f * 128:(f + 1) * 128], h2b[:, k, ts_], k == 0, k == KC - 1,
                                       [w_r[bi], h2b_r[t][k]], [pg_r])
                                pu, pu_r = pbank.get()
                                for k in range(KC):
                                    MM(pu[:], wu[bi][:, k, f * 128:(f + 1) * 128], h2b[:, k, ts_], k == 0, k == KC - 1,
                                       [w_r[bi], h2b_r[t][k]], [pu_r])
                                sg, sg_r = sg_ring.get()
                                ACT(sg[:], pg[:], AF.Silu, [pg_r], [sg_r])
                                if moe:
                                    P.mute = CFG.get('moe_upto', 99) < 4
                                    TT(sg[:], sg[:], cbc[e % 2][:, ts_], ALU.mult, [sg_r, cbc_r[e % 2][t]], [sg_r])
                                P.mute = False
                                TT(hb_[f][0][:], pu[:], sg[:], ALU.mult, [pu_r, sg_r], [hb_[f][1]])
                            for oc in range(KC):
                                py, py_r = pbank.get()
                                for f in range(nf):
                                    MM(py[:], wd[bi][:, f, oc * 128:(oc + 1) * 128], hb_[f][0][:], f == 0, f == nf - 1,
                                       [w_r[bi], hb_[f][1]], [py_r])
                                if first_acc:
                                    CP(yacc[:, oc, ts_], py[:], [py_r], [y_r[t][oc]], eng="act")
                                else:
                                    TT(yacc[:, oc, ts_], yacc[:, oc, ts_], py[:], ALU.add, [y_r[t][oc], py_r], [y_r[t][oc]])
                        first_acc = False
                    for t in range(NT):
                        xb = xload[0] % 2; xload[0] += 1
                        xt, xt_r = xt2[xb], xt2_r[xb]
                        tok0 = btok + t * 512
                        DMA("sp", xt[:], src[:, tok0:tok0 + 512].rearrange("(k p) t -> p k t", p=128), W=xt_r)
                        for oc in range(KC):
                            g2 = mod[:, seq, li * 48 + 40 + oc: li * 48 + 41 + oc]
                            STT(xt[:, oc, :], yacc[:, oc, t * 512:(t + 1) * 512], g2, xt[:, oc, :], ALU.mult, ALU.add,
                                [y_r[t][oc], xt_r[oc]], [xt_r[oc]])
                        DMA("sp", Xs[:, tok0:tok0 + 512].rearrange("(k p) t -> p k t", p=128), xt[:], R=xt_r)
                first[0] = False
                P.flush()

        def phase_final():
            src = cur_src()
            with contextlib.ExitStack() as ph:
                def pb(name, shape, dt=F32):
                    return ph.enter_context(nc.sbuf_tensor(uname(name), list(shape), dt))
                pbank = Ring([(banks[i], bank_r[i]) for i in range(8)])
                xt2 = [pb("xtf%d" % i, [128, KC, 512]) for i in range(2)]
                xt2_r = [[Res() for _ in range(KC)] for _ in range(2)]
                ot2 = [pb("otf%d" % i, [128, KC, 512]) for i in range(2)]
                ot2_r = [[Res() for _ in range(KC)] for _ in range(2)]
                sq_ring = Ring([(pb("sqf%d" % i, [128, 512], BF16), Res()) for i in range(3)])
                tmp_ring = Ring([(pb("tmf%d" % i, [128, 512]), Res()) for i in range(3)])
                rstd = pb("rstdf", [128, 512]); rstd_r = Res()
                Af = AB[:, :, 64:72]
                for t in range(T // 512):
                    seq = (t * 512) // L
                    xt, xt_r = xt2[t % 2], xt2_r[t % 2]
                    ot, ot_r = ot2[t % 2], ot2_r[t % 2]
                    tok0 = t * 512
                    DMA("sp", xt[:], src[:, tok0:tok0 + 512].rearrange("(k p) t -> p k t", p=128), W=xt_r)
                    ps, ps_r = pbank.get()
                    for k in range(KC):
                        sq, sq_r = sq_ring.get()
                        ACT(sq[:], xt[:, k, :], AF.Square, [xt_r[k]], [sq_r])
                        MM(ps[:], ones_b, sq[:], k == 0, k == KC - 1, [sq_r, r_const], [ps_r])
                    ACT(rstd[:], ps[:], AF.Ln, [ps_r], [rstd_r], scale=1.0, bias=1024.0 * EPS)
                    ACT(rstd[:], rstd[:], AF.Exp, [rstd_r], [rstd_r], scale=-0.5)
                    for k in range(KC):
                        tm, tm_r = tmp_ring.get()
                        TT(tm[:], xt[:, k, :], rstd[:], ALU.mult, [xt_r[k], rstd_r], [tm_r])
                        ACT(ot[:, k, :], tm[:], AF.Identity, [tm_r], [ot_r[k]],
                            scale=Af[:, seq, k:k + 1], bias=mod[:, seq, 192 + k:193 + k])
                    DMA("sp", outT[:, tok0:tok0 + 512].rearrange("(k p) t -> p k t", p=128), ot[:], R=ot_r)
                P.flush()

        for li, l in enumerate(layers):
            phase_A(li, l)
            if CFG.get('upto', 99) >= 9:
                phase_B(li, l)
        phase_final()
    return nc


_PROG_CACHE = {}


def kernel(x, c, ada_w, ada_b, norm1_w, norm2_w, w_in, conv_w, a_log, dt_bias,
           dn_norm_w, gm_ln_w, gm_ln_b, gm_spatial_w, gm_spatial_b, w_out,
           ffn_w_gate, ffn_w_up, ffn_w_down, moe_router, moe_w_gate, moe_w_up,
           moe_w_down, final_ada_w, final_ada_b, final_norm_w):
    f = lambda a: np.ascontiguousarray(np.asarray(a, dtype=np.float32))
    x = f(x); c = f(c)
    B, L, _ = x.shape
    n_cores = CFG["n_cores"]
    NB = B // n_cores
    layers = list(CFG["layers"])
    key = (L, NB, tuple(layers))
    if key not in _PROG_CACHE:
        import time as _t, sys as _s
        _t0 = _t.time()
        _PROG_CACHE[key] = build(L, NB, layers)
        print("[kernel] build %.1fs" % (_t.time() - _t0), file=_s.stderr)
    nc = _PROG_CACHE[key]
    cons, sel = _consts()

    def pm(v, nchunk):
        v = f(v)
        lead = v.shape[:-1]
        return np.ascontiguousarray(np.moveaxis(v.reshape(*lead, nchunk, 128), -1, 0))

    ada_bT = pm(ada_b, 48).reshape(128, 4 * 48)
    fada_bT = pm(final_ada_b, 16).reshape(128, 16)
    nw = np.concatenate([np.stack([pm(norm1_w, 8), pm(norm2_w, 8)], axis=2).reshape(128, 64),
                         pm(final_norm_w, 8).reshape(128, 8)], axis=1)
    cwp = np.ascontiguousarray(np.transpose(pm(conv_w, 12), (0, 1, 3, 2))).reshape(128, 4 * 48)
    dnw = np.ascontiguousarray(f(dn_norm_w).T)
    alog_bc = np.ascontiguousarray(np.broadcast_to(f(a_log).reshape(1, 16), (128, 16)))
    dtb_bc = np.ascontiguousarray(np.broadcast_to(f(dt_bias).reshape(1, 16), (128, 16)))
    wsT = np.ascontiguousarray(np.transpose(f(gm_spatial_w), (0, 3, 1, 2))).reshape(4, 128, 512)
    gsb = f(gm_spatial_b).reshape(4, 512)
    shared = {
        "consts": cons.reshape(128, NCONST * 128), "sel": sel,
        "ada_w": f(ada_w), "ada_bT": ada_bT, "final_ada_w": f(final_ada_w), "fada_bT": fada_bT, "nw": nw,
        "w_in": f(w_in), "cw": cwp, "dnw": dnw, "alog_bc": alog_bc, "dtb_bc": dtb_bc,
        "gm_ln_w": f(gm_ln_w), "gm_ln_b": f(gm_ln_b), "wsT": wsT, "gsb": gsb, "w_out": f(w_out),
        "ffn_w_gate": f(ffn_w_gate), "ffn_w_up": f(ffn_w_up), "ffn_w_down": f(ffn_w_down),
        "moe_router": f(moe_router), "moe_w_gate": f(moe_w_gate), "moe_w_up": f(moe_w_up), "moe_w_down": f(moe_w_down),
    }
    if not any(l % 2 == 1 for l in layers):
        for k_ in ("moe_w_gate", "moe_w_up", "moe_w_down"):
            shared[k_] = np.zeros((1, 1, 1, 1), np.float32)
    in_maps = []
    for i in range(n_cores):
        xs = x[i * NB:(i + 1) * NB].reshape(NB * L, D)
        cs = c[i * NB:(i + 1) * NB]
        cTl = np.ascontiguousarray(np.transpose(cs.reshape(NB, KC, 128), (2, 1, 0))).reshape(128, KC * NB)
        m = dict(shared)
        m["xT"] = np.ascontiguousarray(xs.T)
        m["cT"] = cTl
        in_maps.append(m)
    import time as _t, sys as _s
    _t0 = _t.time()
    if CFG.get("sim"):
        return nc, in_maps
    res = run_bass_kernel_spmd(nc, in_maps, core_ids=list(range(n_cores)))
    print("[kernel] launch+transfer %.1fs" % (_t.time() - _t0), file=_s.stderr)
    out = np.empty((B, L, D), np.float32)
    for i in range(n_cores):
        out[i * NB:(i + 1) * NB] = res.results[i]["outT"].T.reshape(NB, L, D)
    return out
```

```python
import contextlib
import numpy as np
import concourse.bass as bass
import concourse.mybir as mybir
from concourse.bass_utils import run_bass_kernel_spmd

F32 = mybir.dt.float32
BF16 = mybir.dt.bfloat16
AF = mybir.ActivationFunctionType
ALU = mybir.AluOpType
AX = mybir.AxisListType

D = 1024
KC = 8
DIN = 3080
OFF_Z, OFF_BA, OFF_GU, OFF_GV = 1536, 2048, 2056, 2568
FF_DENSE = 2816
FF_MOE = 3584
NE = 8
EPS = 1e-6
N_CORES = 8

CFG = {"L": 4096, "NB": 2, "layers": [0, 1, 2, 3], "n_cores": 8}

ENGS = ["pe", "act", "dve", "pool", "sp"]
KEYS = ["pe", "act", "dve", "pool", "sp_dma", "pool_dma", "act_dma"]


class Res:
    __slots__ = ("w", "r", "x")

    def __init__(self, excl=False):
        self.w = None
        self.r = {}
        self.x = excl


class Prog:
    def __init__(self, nc, st):
        self.nc = nc
        self.q = {e: [] for e in ENGS}
        self.cnt = {k: 0 for k in KEYS}
        self.seen = {e: {} for e in ENGS}
        self.signaled = {k: set() for k in KEYS}
        self.rank_base = {k: 0 for k in KEYS}
        self.sems = {k: st.enter_context(nc.semaphore("s_" + k)) for k in KEYS}
        self.nops = 0
        self.mute = False

    def op(self, eng, fn, reads=(), writes=(), dma=False):
        if self.mute:
            return
        if any(r.x for r in reads):
            writes = list(writes) + [r for r in reads if r.x]
            reads = [r for r in reads if not r.x]
        key = eng + "_dma" if dma else eng
        idx = self.cnt[key] + 1
        self.cnt[key] = idx
        waits = {}
        for r in reads:
            if r.w is not None:
                k, v = r.w
                if not (k == "pe" and eng == "pe") and waits.get(k, 0) < v:
                    waits[k] = v
        for w in writes:
            if w.w is not None:
                k, v = w.w
                if not (k == "pe" and eng == "pe") and waits.get(k, 0) < v:
                    waits[k] = v
            for k, v in w.r.items():
                if k == eng:
                    continue
                if waits.get(k, 0) < v:
                    waits[k] = v
        wl = []
        seen = self.seen[eng]
        for k, v in waits.items():
            if seen.get(k, 0) >= v:
                continue
            seen[k] = v
            wl.append((k, v))
            self.signaled[k].add(v)
        for r in reads:
            if r.r.get(key, 0) < idx:
                r.r[key] = idx
        for w in writes:
            w.w = (key, idx)
            w.r = {}
        if dma:
            self.signaled[key].add(idx)
        self.q[eng].append((wl, fn, key, idx))
        self.nops += 1

    def barrier(self):
        tot = dict(self.cnt)
        for e in ENGS:
            wl = []
            for k, v in tot.items():
                if v == 0 or k == e:
                    continue
                if self.seen[e].get(k, 0) >= v:
                    continue
                self.seen[e][k] = v
                wl.append((k, v))
                self.signaled[k].add(v)
            if wl:
                self.q[e].append((wl, None, None, None))

    def flush(self):
        self.barrier()
        ranks = {}
        for k in KEYS:
            s = sorted(self.signaled[k])
            base = self.rank_base[k]
            ranks[k] = {v: base + i + 1 for i, v in enumerate(s)}
            self.rank_base[k] = base + len(s)
            self.signaled[k] = set()
        sems = self.sems
        q = self.q
        self.q = {e: [] for e in ENGS}

        def run(e):
            def body(engine):
                for wl, fn, key, idx in q[e]:
                    for k, v in wl:
                        engine.wait_ge(sems[k], ranks[k][v] * (16 if k.endswith("_dma") else 1))
                    if fn is None:
                        continue
                    ins = fn(engine)
                    if idx in ranks[key]:
                        ins.then_inc(sems[key], 16 if key.endswith("_dma") else 1)
            return body

        with self.nc.Block() as block:
            block.tensor(run("pe"))
            block.scalar(run("act"))
            block.vector(run("dve"))
            block.gpsimd(run("pool"))
            block.sync(run("sp"))


class Ring:
    def __init__(self, items):
        self.items = items
        self.i = 0

    def get(self):
        it = self.items[self.i % len(self.items)]
        self.i += 1
        return it


C_ID, C_ONES, C_TRIL, C_TRILS, C_TRIU, C_M0 = 0, 1, 2, 3, 4, 5
NCONST = 12


def _consts():
    c = np.zeros((NCONST, 128, 128), np.float32)
    i = np.arange(128)[:, None]
    j = np.arange(128)[None, :]
    c[C_ID] = (i == j)
    c[C_ONES] = 1.0
    c[C_TRIL] = (i >= j)
    c[C_TRILS] = (i > j)
    c[C_TRIU] = (i <= j)
    for l in range(7):
        b = 1 << l
        c[C_M0 + l] = ((i // (2 * b)) == (j // (2 * b))) & ((i % (2 * b)) >= b) & ((j % (2 * b)) < b)
    cons = np.ascontiguousarray(c.transpose(1, 0, 2))
    sel = np.zeros((8, 8, 128), np.float32)
    for e in range(8):
        sel[e, e, :] = 1.0
    return cons, sel.reshape(8, 1024)


def build(L, NB, layers):
    T = NB * L
    nl = len(layers)
    nc = bass.Bass("TRN2", target_bir_lowering=False)

    def din(name, shape, dt=F32):
        return nc.dram_tensor(name, list(shape), dt, kind="ExternalInput").ap()

    xT_in = din("xT", [D, T])
    outT = nc.dram_tensor("outT", [D, T], F32, kind="ExternalOutput").ap()
    Xs = nc.dram_tensor("Xs", [D, T], F32, kind="Internal").ap()
    cT_d = din("cT", [128, KC * NB])
    consts_d = din("consts", [128, NCONST * 128])
    sel_d = din("sel", [8, 1024])
    ada_w_d = din("ada_w", [4, D, 6 * D])
    ada_bT_d = din("ada_bT", [128, 4 * 48])
    fada_w_d = din("final_ada_w", [D, 2 * D])
    fada_bT_d = din("fada_bT", [128, 16])
    nw_d = din("nw", [128, 4 * 16 + 8])
    w_in_d = din("w_in", [4, D, DIN])
    cw_d = din("cw", [128, 4 * 48])
    dnw_d = din("dnw", [128, 4])
    alog_d = din("alog_bc", [128, 16])
    dtb_d = din("dtb_bc", [128, 16])
    lnw_d = din("gm_ln_w", [4, 512])
    lnb_d = din("gm_ln_b", [4, 512])
    wsT_d = din("wsT", [4, 128, 512])
    gsb_d = din("gsb", [4, 512])
    w_out_d = din("w_out", [4, D, D])
    fg_d = din("ffn_w_gate", [2, D, FF_DENSE])
    fu_d = din("ffn_w_up", [2, D, FF_DENSE])
    fd_d = din("ffn_w_down", [2, FF_DENSE, D])
    rt_d = din("moe_router", [2, D, NE])
    has_moe = any(l % 2 == 1 for l in layers)
    mg_d = din("moe_w_gate", [2, NE, D, FF_MOE] if has_moe else [1, 1, 1, 1])
    mu_d = din("moe_w_up", [2, NE, D, FF_MOE] if has_moe else [1, 1, 1, 1])
    md_d = din("moe_w_down", [2, NE, FF_MOE, D] if has_moe else [1, 1, 1, 1])

    with contextlib.ExitStack() as st:
        uid = [0]

        def uname(name):
            uid[0] += 1
            return "sb%d_%s" % (uid[0], name)

        def sb(name, shape, dt=F32):
            return st.enter_context(nc.sbuf_tensor(uname(name), list(shape), dt))

        P = Prog(nc, st)
        banks = [st.enter_context(nc.psum_tensor("bank%d" % i, [128, 512], F32)) for i in range(8)]
        bank_r = [Res(True) for _ in range(8)]

        cons = sb("cons", [128, NCONST, 128])
        cons_b = sb("cons_b", [128, 2, 128], BF16)
        onesrow_b = sb("onesrow_b", [1, 128], BF16)
        cT = sb("cT", [128, KC * NB])
        cact_b = sb("cact_b", [128, KC * NB], BF16)
        mod = sb("mod", [128, NB, 4 * 48 + 16])
        ada_bT = sb("ada_bT", [128, 4 * 48 + 16])
        nw = sb("nw", [128, 4 * 16 + 8])
        AB = sb("AB", [128, NB, (4 * 2 + 1) * 8])
        cw = sb("cw", [128, 4 * 48])
        dnw = sb("dnw", [128, 4])
        nA = sb("nA", [128, 16])
        dtb = sb("dtb", [128, 16])
        r_const = Res()

        def DMA(eng, out, in_, R=(), W=()):
            P.op(eng, lambda e: e.dma_start(out=out, in_=in_), R, W, dma=True)

        def MM(out, lhsT, rhs, start, stop, R, W):
            P.op("pe", lambda e: e.matmul(out, lhsT, rhs, start=start, stop=stop), R, W)

        def ACT(out, in_, func, R, W, scale=1.0, bias=0.0):
            P.op("act", lambda e: e.activation(out, in_, func, bias=bias, scale=scale), R, W)

        def TT(out, a, b, op, R, W, eng="dve"):
            P.op(eng, lambda e: e.tensor_tensor(out, a, b, op), R, W)

        def TS(out, a, s1, s2, op0, op1, R, W, eng="dve"):
            if s2 is None:
                P.op(eng, lambda e: e.tensor_scalar(out, a, s1, None, op0), R, W)
            else:
                P.op(eng, lambda e: e.tensor_scalar(out, a, s1, s2, op0, op1), R, W)

        def STT(out, a, s, b, op0, op1, R, W, eng="dve"):
            P.op(eng, lambda e: e.scalar_tensor_tensor(out, a, s, b, op0, op1), R, W)

        def CP(out, in_, R, W, eng="dve"):
            if eng == "act":
                P.op("act", lambda e: e.copy(out, in_), R, W)
            else:
                P.op(eng, lambda e: e.tensor_copy(out, in_), R, W)

        def RECIP(out, in_, R, W):
            P.op("dve", lambda e: e.reciprocal(out, in_), R, W)

        def MEMSET(ap, val, W, eng="pool"):
            P.op(eng, lambda e: e.memset(ap, val), (), W)

        DMA("sp", cons[:], consts_d.rearrange("p (a b) -> p a b", a=NCONST), W=[r_const])
        DMA("pool", cons_b[:], consts_d[:, 0:256].rearrange("p (a b) -> p a b", a=2), W=[r_const])
        DMA("sp", cT[:], cT_d, W=[r_const])
        DMA("sp", ada_bT[:, 0:192], ada_bT_d, W=[r_const])
        DMA("sp", ada_bT[:, 192:208], fada_bT_d, W=[r_const])
        DMA("sp", nw[:], nw_d, W=[r_const])
        DMA("sp", cw[:], cw_d, W=[r_const])
        DMA("sp", dnw[:], dnw_d, W=[r_const])
        DMA("sp", nA[:], alog_d, W=[r_const])
        DMA("sp", dtb[:], dtb_d, W=[r_const])
        MEMSET(onesrow_b[:], 1.0, [r_const])
        ACT(nA[:], nA[:], AF.Exp, [r_const], [r_const])
        TS(nA[:], nA[:], -1.0, None, ALU.mult, None, [r_const], [r_const])
        ACT(cact_b[:], cT[:], AF.Silu, [r_const], [r_const])

        ident_f = cons[:, C_ID, :]
        ones_f = cons[:, C_ONES, :]
        trils_f = cons[:, C_TRILS, :]
        triu_f = cons[:, C_TRIU, :]
        ident_b = cons_b[:, 0, :]
        ones_b = cons_b[:, 1, :]

        with contextlib.ExitStack() as ph:
            wbuf = [ph.enter_context(nc.sbuf_tensor(uname("adaw"), [128, KC, 1024], BF16)) for i in range(2)]
            wbuf_r = [Res(), Res()]
            mod_r = Res()
            pieces = []
            for li, l in enumerate(layers):
                for j in range(6):
                    pieces.append((ada_w_d[l, :, j * 1024:(j + 1) * 1024], li * 48 + j * 8))
            for j in range(2):
                pieces.append((fada_w_d[:, j * 1024:(j + 1) * 1024], 192 + j * 8))
            for pi, (src, col0) in enumerate(pieces):
                wb, wr = wbuf[pi % 2], wbuf_r[pi % 2]
                DMA("pool", wb[:], src.rearrange("(k p) n -> p k n", p=128), W=[wr])
                for oc in range(8):
                    bk = pi * 8 + oc
                    ps = banks[bk % 8][:, 0:NB]
                    pr = bank_r[bk % 8]
                    for k in range(KC):
                        MM(ps, wb[:, k, oc * 128:(oc + 1) * 128], cact_b[:, k * NB:(k + 1) * NB],
                           k == 0, k == KC - 1, [wr, r_const], [pr])
                    bcol = (layers[col0 // 48] * 48 + col0 % 48 + oc) if col0 < 192 else (192 + col0 - 192 + oc)
                    TS(mod[:, :, col0 + oc], ps, ada_bT[:, bcol:bcol + 1], None, ALU.add, None,
                       [pr, r_const], [mod_r])
            MEMSET(AB[:], 0.0, [mod_r])
            for li, l in enumerate(layers):
                for sub in range(2):
                    for b in range(NB):
                        sc = mod[:, b, li * 48 + (sub * 3 + 1) * 8: li * 48 + (sub * 3 + 2) * 8]
                        STT(AB[:, b, (li * 2 + sub) * 8:(li * 2 + sub + 1) * 8], sc, 1.0,
                            nw[:, l * 16 + sub * 8: l * 16 + sub * 8 + 8], ALU.add, ALU.mult, [mod_r, r_const], [mod_r])
            for b in range(NB):
                STT(AB[:, b, 64:72], mod[:, b, 200:208], 1.0, nw[:, 64:72], ALU.add, ALU.mult, [mod_r, r_const], [mod_r])
            TS(AB[:], AB[:], 32.0, None, ALU.mult, None, [mod_r], [mod_r])
            P.flush()

        def norm_mod(xt, xt_r, ncols, A, B, out_b, out_r, sq_ring, tmp_ring, rstd, rstd_r, ps, ps_r, out_f=None, out_f_r=None):
            for k in range(KC):
                sq, sq_r = sq_ring.get()
                ACT(sq[:, 0:ncols], xt[:, k, 0:ncols], AF.Square, [xt_r[k]], [sq_r])
                MM(ps[:, 0:ncols], ones_b, sq[:, 0:ncols], k == 0, k == KC - 1, [sq_r, r_const], [ps_r])
            ACT(rstd[:, 0:ncols], ps[:, 0:ncols], AF.Ln, [ps_r], [rstd_r], scale=1.0, bias=1024.0 * EPS)
            ACT(rstd[:, 0:ncols], rstd[:, 0:ncols], AF.Exp, [rstd_r], [rstd_r], scale=-0.5)
            for k in range(KC):
                tm, tm_r = tmp_ring.get()
                TT(tm[:, 0:ncols], xt[:, k, 0:ncols], rstd[:, 0:ncols], ALU.mult, [xt_r[k], rstd_r], [tm_r])
                if out_f is not None:
                    ACT(out_f[:, k, 0:ncols], tm[:, 0:ncols], AF.Identity, [tm_r], [out_f_r[k]],
                        scale=A[:, k:k + 1], bias=B[:, k:k + 1])
                ACT(out_b[:, k, 0:ncols], tm[:, 0:ncols], AF.Identity, [tm_r], [out_r[k]],
                    scale=A[:, k:k + 1], bias=B[:, k:k + 1])

        first = [True]

        def cur_src():
            return xT_in if first[0] else Xs

        def phase_A(li, l):
            TTK = 256
            NCH = TTK // 128
            src = cur_src()
            with contextlib.ExitStack() as ph:
                def pb(name, shape, dt=F32):
                    return ph.enter_context(nc.sbuf_tensor(uname(name), list(shape), dt))

                class _BankRing:
                    def __init__(self, ncol):
                        self.ncol = ncol

                    def get(self):
                        i = bring[0] % 8
                        bring[0] += 1
                        return (banks[i][:, 0:self.ncol], bank_r[i])
                bring = [0]
                wide = _BankRing(256)
                small = _BankRing(128)
                fullr = _BankRing(512)

                w_in_b = pb("w_in_b", [128, KC, DIN], BF16)
                w_out_b = pb("w_out_b", [128, KC, D], BF16)
                wsT_f = pb("wsT_f", [128, 512])
                wsT_b = pb("wsT_b", [128, 512], BF16)
                brow_b = pb("brow_b", [1, 512], BF16)
                lnw = pb("lnw", [128, 512])
                lnb = pb("lnb", [128, 512])
                r_w = Res()
                for k in range(KC):
                    DMA("pool", w_in_b[:, k, :], w_in_d[l, k * 128:(k + 1) * 128, :], W=[r_w])
                DMA("pool", w_out_b[:], w_out_d[l].rearrange("(k p) n -> p k n", p=128), W=[r_w])
                DMA("sp", wsT_f[:], wsT_d[l], W=[r_w])
                DMA("pool", brow_b[:], gsb_d[l:l + 1, :], W=[r_w])
                DMA("sp", lnw[:], lnw_d[l:l + 1, :].to_broadcast([128, 512]), W=[r_w])
                DMA("sp", lnb[:], lnb_d[l:l + 1, :].to_broadcast([128, 512]), W=[r_w])
                for g in range(4):
                    TT(wsT_b[:, g * 128:(g + 1) * 128], wsT_f[:, g * 128:(g + 1) * 128], triu_f, ALU.mult,
                       [r_w, r_const], [r_w])

                xtL = [(pb("xt%d" % i, [128, KC, TTK]), [Res() for _ in range(KC)]) for i in range(2)]
                hTL = [(pb("hT%d" % i, [128, KC, TTK], BF16), [Res() for _ in range(KC)]) for i in range(2)]
                sq_ring = Ring([(pb("sqa%d" % i, [128, TTK], BF16), Res()) for i in range(3)])
                tmp_ring = Ring([(pb("tma%d" % i, [128, TTK]), Res()) for i in range(3)])
                rstd = pb("rstd", [128, TTK]); rstd_r = Res()
                pre = pb("pre", [128, 12, TTK + 3], BF16); pre_r = [Res() for _ in range(12)]
                qs = pb("qs", [128, 4, TTK], BF16); ks = pb("ks", [128, 4, TTK], BF16); vs_b = pb("vs_b", [128, 4, TTK], BF16)
                qkv_r = [Res() for _ in range(12)]
                qn_b = pb("qn_b", [128, 4, TTK], BF16); kn_b = pb("kn_b", [128, 4, TTK], BF16)
                qn_r = [Res() for _ in range(4)]; kn_r = [Res() for _ in range(4)]
                zs = pb("zs", [128, 4, TTK], BF16); zs_r = [Res() for _ in range(4)]
                gus = pb("gus", [128, 4, TTK], BF16); gus_r = [Res() for _ in range(4)]
                gvf_ring = Ring([(pb("gvf%d" % i, [128, 512]), Res()) for i in range(2)])
                gv_b = pb("gv_b", [128, NCH, 512], BF16); gv_r = [Res() for _ in range(NCH)]
                stat = pb("stat", [128, 8]); stat_r = Res()
                mixT = pb("mixT", [128, KC, TTK], BF16); mix_r = [Res() for _ in range(KC)]
                oT = pb("oT", [128, 4, TTK]); oT_r = [Res() for _ in range(4)]
                ba = pb("ba", [128, NCH * 8]); ba_r = Res()
                g_tm = pb("g_tm", [128, NCH * 4]); beta_tm = pb("beta_tm", [128, NCH * 4])
                G_tm = pb("G_tm", [128, NCH * 4]); e1 = pb("e1", [128, NCH * 4]); e2 = pb("e2", [128, NCH * 4])
                eGl = pb("eGl", [128, NCH * 4]); sm_r = Res()
                S_f = pb("S_f", [128, 4, 128]); S_b = pb("S_b", [128, 4, 128], BF16)
                S_r = [Res() for _ in range(4)]; Sb_r = [Res() for _ in range(4)]
                def hb(name, dt=F32):
                    return [(pb("%s%d" % (name, h), [128, 128], dt), Res()) for h in range(4 * NCH)]
                gbc = hb("gbc"); eG = hb("eG", BF16); dd = hb("dd", BF16); dt_ = hb("dt_", BF16)
                Am = hb("Am", BF16); AM = [hb("AM%d_" % lv, BF16) for lv in range(2)]
                U0 = hb("U0", BF16); U1 = hb("U1", BF16); T0 = hb("T0", BF16); T1 = hb("T1", BF16)
                P1 = hb("P1", BF16); attnT = hb("attnT", BF16); qd = hb("qd", BF16)
                kbd = hb("kbd", BF16); kdec = hb("kdec", BF16); vb = hb("vb", BF16)
                u_f = hb("u_f"); wT = hb("wT", BF16); vnew = hb("vnew", BF16)

                A1 = AB[:, :, (li * 2) * 8:(li * 2 + 1) * 8]
                c4 = l * 4

                for seq in range(NB):
                    for h in range(4):
                        MEMSET(S_f[:, h, :], 0.0, [S_r[h]])
                        MEMSET(S_b[:, h, :], 0.0, [Sb_r[h]])
                    for j in range(12):
                        MEMSET(pre[:, j, 0:3], 0.0, [pre_r[j]])
                    def norm_part(seq_, it_, bi_):
                        xt_, xt_r_ = xtL[bi_]
                        hT_, hT_r_ = hTL[bi_]
                        tk = seq_ * L + it_ * TTK
                        DMA("sp", xt_[:], src[:, tk:tk + TTK].rearrange("(k p) t -> p k t", p=128), W=xt_r_)
                        ps_, ps_r_ = wide.get()
                        norm_mod(xt_, xt_r_, TTK, A1[:, seq_, :], mod[:, seq_, li * 48: li * 48 + 8], hT_, hT_r_,
                                 sq_ring, tmp_ring, rstd, rstd_r, ps_, ps_r_)
                    NTL = L // TTK
                    if seq == 0:
                        norm_part(0, 0, 0)
                    for it in range(NTL):
                        tok0 = seq * L + it * TTK
                        gidx = seq * NTL + it
                        xt, xt_r = xtL[gidx % 2]
                        hT, hT_r = hTL[gidx % 2]
                        def proj(col0):
                            ps, ps_r = wide.get()
                            for k in range(KC):
                                MM(ps, w_in_b[:, k, col0:col0 + 128], hT[:, k, :], k == 0, k == KC - 1,
                                   [r_w, hT_r[k]], [ps_r])
                            return ps, ps_r
                        for j in range(12):
                            ps, ps_r = proj(j * 128)
                            CP(pre[:, j, 3:3 + TTK], ps, [ps_r], [pre_r[j]], eng="act")
                            acc_t, acc_r = tmp_ring.get()
                            acc = acc_t[:]
                            cwj = cw[:, l * 48 + j * 4: l * 48 + j * 4 + 4]
                            TS(acc, pre[:, j, 0:TTK], cwj[:, 0:1], None, ALU.mult, None, [pre_r[j], r_const], [acc_r])
                            for tp in range(1, 4):
                                STT(acc, pre[:, j, tp:tp + TTK], cwj[:, tp:tp + 1], acc, ALU.mult, ALU.add,
                                    [pre_r[j], acc_r, r_const], [acc_r])
                            dst = (qs, ks, vs_b)[j // 4][:, j % 4, :]
                            ACT(dst, acc, AF.Silu, [acc_r], [qkv_r[j]])
                            CP(pre[:, j, 0:3], pre[:, j, TTK:TTK + 3], [pre_r[j]], [pre_r[j]], eng="pool")
                        for h in range(4):
                            ps, ps_r = proj(OFF_Z + h * 128)
                            ACT(zs[:, h, :], ps, AF.Silu, [ps_r], [zs_r[h]])
                        for g in range(4):
                            ps, ps_r = proj(OFF_GU + g * 128)
                            ACT(gus[:, g, :], ps, AF.Gelu_apprx_tanh, [ps_r], [gus_r[g]])
                        P.mute = CFG.get('upto', 99) < 2
                        psba, psba_r = small.get()
                        for c in range(NCH):
                            for k in range(KC):
                                MM(psba[:, c * 8:(c + 1) * 8], hT[:, k, c * 128:(c + 1) * 128],
                                   w_in_b[:, k, OFF_BA:OFF_BA + 8], k == 0, k == KC - 1, [r_w, hT_r[k]], [psba_r])
                        CP(ba[:], psba[:, 0:NCH * 8], [psba_r], [ba_r])
                        for c in range(NCH):
                            ACT(beta_tm[:, c * 4:c * 4 + 4], ba[:, c * 8:c * 8 + 4], AF.Sigmoid, [ba_r], [sm_r])
                            TT(g_tm[:, c * 4:c * 4 + 4], ba[:, c * 8 + 4:c * 8 + 8], dtb[:, c4:c4 + 4], ALU.add, [ba_r, r_const], [sm_r])
                        g2d = g_tm[:]
                        ACT(g2d, g2d, AF.Exp, [sm_r], [sm_r])
                        ACT(g2d, g2d, AF.Ln, [sm_r], [sm_r], scale=1.0, bias=1.0)
                        for c in range(NCH):
                            TT(g_tm[:, c * 4:c * 4 + 4], g_tm[:, c * 4:c * 4 + 4], nA[:, c4:c4 + 4], ALU.mult, [sm_r, r_const], [sm_r])
                        for c in range(NCH):
                            psg, psg_r = fullr.get()
                            for k in range(KC):
                                MM(psg[:], hT[:, k, c * 128:(c + 1) * 128], w_in_b[:, k, OFF_GV:OFF_GV + 512],
                                   k == 0, k == KC - 1, [r_w, hT_r[k]], [psg_r])
                            gvf, gvf_r = gvf_ring.get()
                            ACT(gvf[:], psg[:], AF.Gelu_apprx_tanh, [psg_r], [gvf_r])
                            P.op("dve", lambda e, gvf=gvf, c=c: e.reduce_sum(stat[:, c:c + 1], gvf[:], AX.X), [gvf_r], [stat_r])
                            TS(stat[:, c:c + 1], stat[:, c:c + 1], 1.0 / 512, None, ALU.mult, None, [stat_r], [stat_r])
                            TS(gvf[:], gvf[:], stat[:, c:c + 1], None, ALU.subtract, None, [gvf_r, stat_r], [gvf_r])
                            sq2, sq2_r = gvf_ring.get()
                            TT(sq2[:], gvf[:], gvf[:], ALU.mult, [gvf_r], [sq2_r])
                            P.op("dve", lambda e, sq2=sq2, c=c: e.reduce_sum(stat[:, 4 + c:5 + c], sq2[:], AX.X), [sq2_r], [stat_r])
                            ACT(stat[:, 4 + c:5 + c], stat[:, 4 + c:5 + c], AF.Ln, [stat_r], [stat_r], scale=1.0 / 512, bias=EPS)
                            ACT(stat[:, 4 + c:5 + c], stat[:, 4 + c:5 + c], AF.Exp, [stat_r], [stat_r], scale=-0.5)
                            STT(gvf[:], gvf[:], stat[:, 4 + c:5 + c], lnw[:], ALU.mult, ALU.mult, [gvf_r, stat_r, r_w], [gvf_r])
                            TT(gv_b[:, c, :], gvf[:], lnb[:], ALU.add, [gvf_r, r_w], [gv_r[c]])
                        P.mute = CFG.get('upto', 99) < 3
                        for g in range(4):
                            ps, ps_r = wide.get()
                            for c in range(NCH):
                                MM(ps[:, c * 128:(c + 1) * 128], gv_b[:, c, g * 128:(g + 1) * 128],
                                   wsT_b[:, g * 128:(g + 1) * 128], True, False, [gv_r[c], r_w], [ps_r])
                                MM(ps[:, c * 128:(c + 1) * 128], onesrow_b[0:1, :], brow_b[0:1, g * 128:(g + 1) * 128],
                                   False, True, [r_const, r_w], [ps_r])
                            TT(mixT[:, 4 + g, :], ps, gus[:, g, :], ALU.mult, [ps_r, gus_r[g]], [mix_r[4 + g]])
                        P.mute = CFG.get('upto', 99) < 4
                        for h in range(4):
                            for (srcb, r_i, dstb, dst_r, scl) in ((qs, h, qn_b, qn_r, 128.0 ** -0.5), (ks, 4 + h, kn_b, kn_r, 1.0)):
                                sq, sq_r = sq_ring.get()
                                ACT(sq[:], srcb[:, h, :], AF.Square, [qkv_r[r_i]], [sq_r])
                                ps, ps_r = wide.get()
                                MM(ps, ones_b, sq[:], True, True, [sq_r, r_const], [ps_r])
                                tm, tm_r = tmp_ring.get()
                                ACT(tm[:], ps, AF.Ln, [ps_r], [tm_r], scale=1.0, bias=EPS)
                                ACT(tm[:], tm[:], AF.Exp, [tm_r], [tm_r], scale=-0.5)
                                STT(dstb[:, h, :], srcb[:, h, :], scl, tm[:], ALU.mult, ALU.mult, [qkv_r[r_i], tm_r], [dst_r[h]])
                        psl, psl_r = small.get()
                        for c in range(NCH):
                            MM(psl[:, c * 4:(c + 1) * 4], ones_f, g_tm[:, c * 4:c * 4 + 4], True, True, [sm_r, r_const], [psl_r])
                            MM(psl[:, 16 + c * 4:16 + (c + 1) * 4], triu_f, g_tm[:, c * 4:c * 4 + 4], True, True, [sm_r, r_const], [psl_r])
                        sm2_r = Res()
                        CP(G_tm[:], psl[:, 16:16 + NCH * 4], [psl_r], [sm2_r])
                        ACT(eGl[:], psl[:, 0:NCH * 4], AF.Exp, [psl_r], [sm2_r])
                        TT(e2[:], psl[:, 0:NCH * 4], G_tm[:],
                           ALU.subtract, [psl_r, sm2_r], [sm2_r])
                        ACT(e2[:], e2[:], AF.Exp, [sm2_r], [sm2_r])
                        ACT(e1[:], G_tm[:], AF.Exp, [sm2_r], [sm2_r])
                        TT(e1[:], e1[:],
                           beta_tm[:], ALU.mult, [sm2_r, sm_r], [sm2_r])
                        P.mute = CFG.get('upto', 99) < 5
                        if gidx + 1 < NB * NTL:
                            mute_save = P.mute
                            P.mute = False
                            norm_part((gidx + 1) // NTL, (gidx + 1) % NTL, (gidx + 1) % 2)
                            P.mute = mute_save
                        CHN = [(c, h) for c in range(NCH) for h in range(4)]
                        def csl(c):
                            return slice(c * 128, (c + 1) * 128)
                        def col(c, h):
                            return slice(c * 4 + h, c * 4 + h + 1)
                        psG = {}
                        for (c, h) in CHN:
                            i = c * 4 + h
                            TS(gbc[i][0][:], ones_f, g_tm[:, col(c, h)], None, ALU.mult, None, [sm_r, r_const], [gbc[i][1]])
                            psG[i] = small.get()
                            MM(psG[i][0], gbc[i][0][:], triu_f, True, True, [gbc[i][1], r_const], [psG[i][1]])
                        for (c, h) in CHN:
                            i = c * 4 + h
                            pg, pg_r = psG[i]
                            Gc = G_tm[:, col(c, h)]
                            ACT(eG[i][0][:], pg, AF.Exp, [pg_r], [eG[i][1]])
                            TS(dd[i][0][:], pg, Gc, 0.0, ALU.subtract, ALU.max, [pg_r, sm2_r], [dd[i][1]])
                            TS(dt_[i][0][:], pg, Gc, 0.0, ALU.subtract, ALU.min, [pg_r, sm2_r], [dt_[i][1]])
                            ACT(dd[i][0][:], dd[i][0][:], AF.Exp, [dd[i][1]], [dd[i][1]], scale=-1.0)
                            ACT(dt_[i][0][:], dt_[i][0][:], AF.Exp, [dt_[i][1]], [dt_[i][1]])
                            TT(dd[i][0][:], dd[i][0][:], trils_f, ALU.mult, [dd[i][1], r_const], [dd[i][1]], eng="pool")
                            TT(dt_[i][0][:], dt_[i][0][:], triu_f, ALU.mult, [dt_[i][1], r_const], [dt_[i][1]], eng="pool")
                            TT(qd[i][0][:], qn_b[:, h, csl(c)], eG[i][0][:], ALU.mult, [qn_r[h], eG[i][1]], [qd[i][1]])
                        for (c, h) in CHN:
                            i = c * 4 + h
                            cs = csl(c)
                            pk, pk_r = small.get()
                            MM(pk, kn_b[:, h, cs], kn_b[:, h, cs], True, True, [kn_r[h]], [pk_r])
                            STT(Am[i][0][:], pk, beta_tm[:, col(c, h)], dd[i][0][:], ALU.mult, ALU.mult,
                                [pk_r, sm_r, dd[i][1]], [Am[i][1]])
                            pq, pq_r = small.get()
                            MM(pq, kn_b[:, h, cs], qn_b[:, h, cs], True, True, [kn_r[h], qn_r[h]], [pq_r])
                            TT(attnT[i][0][:], pq, dt_[i][0][:], ALU.mult, [pq_r, dt_[i][1]], [attnT[i][1]])
                            pt, pt_r = small.get()
                            MM(pt, kn_b[:, h, cs], ident_b, True, True, [kn_r[h], r_const], [pt_r])
                            TS(kbd[i][0][:], pt, e1[:, col(c, h)], None, ALU.mult, None, [pt_r, sm2_r], [kbd[i][1]])
                            ACT(kdec[i][0][:], pt, AF.Identity, [pt_r, sm2_r], [kdec[i][1]], scale=e2[:, col(c, h)])
                            pv, pv_r = small.get()
                            MM(pv, vs_b[:, h, cs], ident_b, True, True, [qkv_r[8 + h], r_const], [pv_r])
                            TS(vb[i][0][:], pv, beta_tm[:, col(c, h)], None, ALU.mult, None, [pv_r, sm_r], [vb[i][1]])
                        P.mute = CFG.get('upto', 99) < 6
                        Ucur = {}; Tcur = {}
                        for (c, h) in CHN:
                            i = c * 4 + h
                            a0, a0_r = AM[0][i]
                            TT(a0[:], Am[i][0][:], cons[:, C_M0, :], ALU.mult, [Am[i][1], r_const], [a0_r], eng="pool")
                            pt, pt_r = small.get()
                            MM(pt, a0[:], ident_b, True, True, [a0_r, r_const], [pt_r])
                            TT(U0[i][0][:], ident_f, pt, ALU.subtract, [pt_r, r_const], [U0[i][1]])
                            TT(T0[i][0][:], ident_f, a0[:], ALU.subtract, [a0_r, r_const], [T0[i][1]], eng="pool")
                            Ucur[i] = U0[i]; Tcur[i] = T0[i]
                        for lv in range(1, 7):
                            pp = {}
                            for i in range(NCH * 4):
                                al, al_r = AM[lv % 2][i]
                                TT(al[:], Am[i][0][:], cons[:, C_M0 + lv, :], ALU.mult, [Am[i][1], r_const], [al_r], eng="pool")
                                pp[i] = small.get()
                                MM(pp[i][0], al[:], Ucur[i][0][:], True, True, [al_r, Ucur[i][1]], [pp[i][1]])
                            for i in range(NCH * 4):
                                CP(P1[i][0][:], pp[i][0], [pp[i][1]], [P1[i][1]], eng="act")
                            px = {}
                            for i in range(NCH * 4):
                                px[i] = small.get()
                                MM(px[i][0], Tcur[i][0][:], P1[i][0][:], True, True, [Tcur[i][1], P1[i][1]], [px[i][1]])
                            for i in range(NCH * 4):
                                Un = U1[i] if Ucur[i] is U0[i] else U0[i]
                                TT(Un[0][:], Ucur[i][0][:], px[i][0], ALU.subtract, [Ucur[i][1], px[i][1]], [Un[1]])
                                Ucur[i] = Un
                            if lv < 6:
                                ptt = {}
                                for i in range(NCH * 4):
                                    ptt[i] = small.get()
                                    MM(ptt[i][0], Ucur[i][0][:], ident_b, True, True, [Ucur[i][1], r_const], [ptt[i][1]])
                                for i in range(NCH * 4):
                                    Tn = T1[i] if Tcur[i] is T0[i] else T0[i]
                                    CP(Tn[0][:], ptt[i][0], [ptt[i][1]], [Tn[1]], eng="act")
                                    Tcur[i] = Tn
                        P.mute = CFG.get('upto', 99) < 7
                        for i in range(NCH * 4):
                            pu, pu_r = small.get()
                            MM(pu, Ucur[i][0][:], vb[i][0][:], True, True, [Ucur[i][1], vb[i][1]], [pu_r])
                            CP(u_f[i][0][:], pu, [pu_r], [u_f[i][1]], eng="act")
                            pw, pw_r = small.get()
                            MM(pw, kbd[i][0][:], Ucur[i][0][:], True, True, [kbd[i][1], Ucur[i][1]], [pw_r])
                            CP(wT[i][0][:], pw, [pw_r], [wT[i][1]])
                        for c in range(NCH):
                            cs = csl(c)
                            pws = {}
                            for h in range(4):
                                i = c * 4 + h
                                pws[h] = small.get()
                                MM(pws[h][0], wT[i][0][:], S_b[:, h, :], True, True, [wT[i][1], Sb_r[h]], [pws[h][1]])
                            for h in range(4):
                                i = c * 4 + h
                                TT(vnew[i][0][:], u_f[i][0][:], pws[h][0], ALU.subtract, [u_f[i][1], pws[h][1]], [vnew[i][1]])
                            for h in range(4):
                                i = c * 4 + h
                                po, po_r = small.get()
                                MM(po, S_b[:, h, :], qd[i][0][:], True, False, [Sb_r[h], qd[i][1]], [po_r])
                                MM(po, vnew[i][0][:], attnT[i][0][:], False, True, [vnew[i][1], attnT[i][1]], [po_r])
                                CP(oT[:, h, cs], po, [po_r], [oT_r[h]], eng="act")
                                pS, pS_r = small.get()
                                MM(pS, kdec[i][0][:], vnew[i][0][:], True, True, [kdec[i][1], vnew[i][1]], [pS_r])
                                STT(S_f[:, h, :], S_f[:, h, :], eGl[:, col(c, h)], pS, ALU.mult, ALU.add,
                                    [S_r[h], sm2_r, pS_r], [S_r[h]])
                                CP(S_b[:, h, :], S_f[:, h, :], [S_r[h]], [Sb_r[h]], eng="act")
                        P.mute = CFG.get('upto', 99) < 8
                        for h in range(4):
                            sq, sq_r = sq_ring.get()
                            ACT(sq[:], oT[:, h, :], AF.Square, [oT_r[h]], [sq_r])
                            ps, ps_r = wide.get()
                            MM(ps, ones_b, sq[:], True, True, [sq_r, r_const], [ps_r])
                            tm, tm_r = tmp_ring.get()
                            ACT(tm[:], ps, AF.Ln, [ps_r], [tm_r], scale=1.0 / 128, bias=EPS)
                            ACT(tm[:], tm[:], AF.Exp, [tm_r], [tm_r], scale=-0.5)
                            TT(tm[:], tm[:], oT[:, h, :], ALU.mult, [tm_r, oT_r[h]], [tm_r])
                            STT(mixT[:, h, :], tm[:], dnw[:, l:l + 1], zs[:, h, :], ALU.mult, ALU.mult,
                                [tm_r, r_const, zs_r[h]], [mix_r[h]])
                        P.mute = CFG.get('upto', 99) < 0
                        for oc in range(KC):
                            ps, ps_r = wide.get()
                            for k in range(KC):
                                MM(ps, w_out_b[:, k, oc * 128:(oc + 1) * 128], mixT[:, k, :], k == 0, k == KC - 1,
                                   [r_w, mix_r[k]], [ps_r])
                            g1 = mod[:, seq, li * 48 + 16 + oc: li * 48 + 17 + oc]
                            STT(xt[:, oc, :], ps, g1, xt[:, oc, :], ALU.mult, ALU.add, [ps_r, xt_r[oc]], [xt_r[oc]])
                        DMA("sp", Xs[:, tok0:tok0 + TTK].rearrange("(k p) t -> p k t", p=128), xt[:], R=xt_r)
                first[0] = False
                P.flush()

        def phase_B(li, l):
            moe = (l % 2 == 1)
            j = l // 2
            FF = FF_MOE if moe else FF_DENSE
            nexp = NE if moe else 1
            TB = min(1024, L)
            NT = TB // 512
            src = cur_src()
            with contextlib.ExitStack() as ph:
                def pb(name, shape, dt=F32):
                    return ph.enter_context(nc.sbuf_tensor(uname(name), list(shape), dt))
                pbank = Ring([(banks[i], bank_r[i]) for i in range(8)])
                h2b = pb("h2b", [128, KC, TB], BF16); h2b_r = [[Res() for _ in range(KC)] for _ in range(NT)]
                yacc = pb("yacc", [128, KC, TB]); y_r = [[Res() for _ in range(KC)] for _ in range(NT)]
                xt2 = [pb("xtb%d" % i, [128, KC, 512]) for i in range(2)]
                xt2_r = [[Res() for _ in range(KC)] for _ in range(2)]
                sq_ring = Ring([(pb("sqb%d" % i, [128, 512], BF16), Res()) for i in range(3)])
                tmp_ring = Ring([(pb("tmb%d" % i, [128, 512]), Res()) for i in range(3)])
                sg_ring = Ring([(pb("sg%d" % i, [128, 512]), Res()) for i in range(3)])
                hid = [[(pb("hid%d_%d" % (i, f), [128, 512], BF16), Res()) for f in range(4)] for i in range(2)]
                rstd = pb("rstdb", [128, 512]); rstd_r = Res()
                wg = [pb("wg%d" % i, [128, KC, 512], BF16) for i in range(2)]
                wu = [pb("wu%d" % i, [128, KC, 512], BF16) for i in range(2)]
                wd = [pb("wd%d" % i, [128, 4, D], BF16) for i in range(2)]
                w_r = [Res(), Res()]
                wd_r = [Res(), Res()]
                if moe:
                    h2f = pb("h2f", [128, KC, 512]); h2f_r = [Res() for _ in range(KC)]
                    wr = pb("wr", [128, KC, NE]); wr_r = Res()
                    DMA("sp", wr[:], rt_d[j].rearrange("(k p) e -> p k e", p=128), W=[wr_r])
                    sel = pb("sel", [8, 1024]); sel_r = Res()
                    DMA("sp", sel[:], sel_d, W=[sel_r])
                    comb = pb("comb", [128, TB // 128, NE]); comb_r = Res()
                    combT = pb("combT", [8, TB]); combT_r = Res()
                    cbc = [pb("cbc%d" % i, [128, TB]) for i in range(2)]
                    cbc_r = [[Res() for _ in range(NT)] for _ in range(2)]
                    lg = pb("lg", [128, NE]); lg2 = pb("lg2", [128, NE]); mk1 = pb("mk1", [128, NE]); mk2 = pb("mk2", [128, NE])
                    m12 = pb("m12", [128, 4]); lg_r = Res()
                A2 = AB[:, :, (li * 2 + 1) * 8:(li * 2 + 2) * 8]
                pieces = []
                for e in range(nexp):
                    f0 = 0
                    while f0 < FF:
                        pw_ = min(512, FF - f0)
                        pieces.append((e, f0, pw_))
                        f0 += pw_

                def load_piece(pi, which="gud"):
                    e, f0, pw_ = pieces[pi]
                    bi = pi % 2
                    if moe:
                        g_src, u_src, d_src = mg_d[j, e], mu_d[j, e], md_d[j, e]
                    else:
                        g_src, u_src, d_src = fg_d[j], fu_d[j], fd_d[j]
                    if "g" in which:
                        DMA("pool", wg[bi][:, :, 0:pw_], g_src[:, f0:f0 + pw_].rearrange("(k p) n -> p k n", p=128), W=[w_r[bi]])
                        DMA("pool", wu[bi][:, :, 0:pw_], u_src[:, f0:f0 + pw_].rearrange("(k p) n -> p k n", p=128), W=[w_r[bi]])
                    if "d" in which:
                        DMA("pool", wd[bi][:, 0:pw_ // 128, :], d_src[f0:f0 + pw_, :].rearrange("(f p) n -> p f n", p=128), W=[wd_r[bi]])

                xload = [0]
                for blk in range(T // TB):
                    seq = (blk * TB) // L
                    btok = blk * TB
                    load_piece(0)
                    for t in range(NT):
                        xb = xload[0] % 2; xload[0] += 1
                        xt, xt_r = xt2[xb], xt2_r[xb]
                        tok0 = btok + t * 512
                        DMA("sp", xt[:], src[:, tok0:tok0 + 512].rearrange("(k p) t -> p k t", p=128), W=xt_r)
                        ps, ps_r = pbank.get()
                        class _V:
                            def __getitem__(self, idx):
                                p_, k_, c_ = idx
                                return h2b[p_, k_, t * 512 + (c_.start or 0): t * 512 + (c_.stop or 512)]
                        norm_mod(xt, xt_r, 512, A2[:, seq, :], mod[:, seq, li * 48 + 24: li * 48 + 32], _V(), h2b_r[t],
                                 sq_ring, tmp_ring, rstd, rstd_r, ps, ps_r,
                                 out_f=(h2f if moe else None), out_f_r=(h2f_r if moe else None))
                        if moe:
                            P.mute = CFG.get('moe_upto', 99) < 1
                            for c in range(4):
                                gc = t * 4 + c
                                pl, pl_r = pbank.get()
                                for k in range(KC):
                                    MM(pl[:, 0:NE], h2f[:, k, c * 128:(c + 1) * 128], wr[:, k, :], k == 0, k == KC - 1,
                                       [h2f_r[k], wr_r], [pl_r])
                                CP(lg[:], pl[:, 0:NE], [pl_r], [lg_r])
                                P.op("dve", lambda e: e.reduce_max(m12[:, 0:1], lg[:], AX.X), [lg_r], [lg_r])
                                TS(mk1[:], lg[:], m12[:, 0:1], None, ALU.is_equal, None, [lg_r], [lg_r])
                                STT(lg2[:], mk1[:], -1e30, lg[:], ALU.mult, ALU.add, [lg_r], [lg_r])
                                P.op("dve", lambda e: e.reduce_max(m12[:, 1:2], lg2[:], AX.X), [lg_r], [lg_r])
                                TS(mk2[:], lg2[:], m12[:, 1:2], None, ALU.is_equal, None, [lg_r], [lg_r])
                                TT(m12[:, 2:3], m12[:, 1:2], m12[:, 0:1], ALU.subtract, [lg_r], [lg_r])
                                ACT(m12[:, 2:3], m12[:, 2:3], AF.Exp, [lg_r], [lg_r])
                                TS(m12[:, 2:3], m12[:, 2:3], 1.0, None, ALU.add, None, [lg_r], [lg_r])
                                RECIP(m12[:, 2:3], m12[:, 2:3], [lg_r], [lg_r])
                                TS(m12[:, 3:4], m12[:, 2:3], -1.0, 1.0, ALU.mult, ALU.add, [lg_r], [lg_r])
                                TS(mk1[:], mk1[:], m12[:, 2:3], None, ALU.mult, None, [lg_r], [lg_r])
                                STT(comb[:, gc, :], mk2[:], m12[:, 3:4], mk1[:], ALU.mult, ALU.add, [lg_r], [comb_r])
                                P.mute = CFG.get('moe_upto', 99) < 2
                                pc, pc_r = pbank.get()
                                MM(pc[0:8, 0:128], comb[:, gc, :], ident_f, True, True, [comb_r, r_const], [pc_r])
                                CP(combT[:, gc * 128:(gc + 1) * 128], pc[0:8, 0:128], [pc_r], [combT_r])
                    P.mute = False
                    P.mute = False
                    units = [(pi, t) for pi in range(len(pieces)) for t in range(NT)]
                    if len(pieces) > 1:
                        load_piece(1)

                    def unit_G(u):
                        pi, t = units[u]
                        e, f0, pw_ = pieces[pi]
                        bi = pi % 2
                        nf = pw_ // 128
                        if moe and f0 == 0 and t == 0:
                            for t2 in range(NT):
                                pc, pc_r = pbank.get()
                                MM(pc[:], sel[0:8, e * 128:(e + 1) * 128], combT[:, t2 * 512:(t2 + 1) * 512], True, True,
                                   [sel_r, combT_r], [pc_r])
                                CP(cbc[e % 2][:, t2 * 512:(t2 + 1) * 512], pc[:], [pc_r], [cbc_r[e % 2][t2]], eng="act")
                        ts_ = slice(t * 512, (t + 1) * 512)
                        hb_ = hid[u % 2]
                        for f in range(nf):
                            pg, pg_r = pbank.get()
                            for k in range(KC):
                                MM(pg[:], wg[bi][:, k, f * 128:(f + 1) * 128], h2b[:, k, ts_], k == 0, k == KC - 1,
                                   [w_r[bi], h2b_r[t][k]], [pg_r])
                            pu, pu_r = pbank.get()
                            for k in range(KC):
                                MM(pu[:], wu[bi][:, k, f * 128:(f + 1) * 128], h2b[:, k, ts_], k == 0, k == KC - 1,
                                   [w_r[bi], h2b_r[t][k]], [pu_r])
                            sg, sg_r = sg_ring.get()
                            ACT(sg[:], pg[:], AF.Silu, [pg_r], [sg_r])
                            if moe:
                                TT(sg[:], sg[:], cbc[e % 2][:, ts_], ALU.mult, [sg_r, cbc_r[e % 2][t]], [sg_r])
                            TT(hb_[f][0][:], pu[:], sg[:], ALU.mult, [pu_r, sg_r], [hb_[f][1]])
                        if t == NT - 1 and pi + 2 < len(pieces):
                            load_piece(pi + 2, "g")

                    def unit_D(u):
                        pi, t = units[u]
                        e, f0, pw_ = pieces[pi]
                        bi = pi % 2
                        nf = pw_ // 128
                        ts_ = slice(t * 512, (t + 1) * 512)
                        hb_ = hid[u % 2]
                        for oc in range(KC):
                            py, py_r = pbank.get()
                            for f in range(nf):
                                MM(py[:], wd[bi][:, f, oc * 128:(oc + 1) * 128], hb_[f][0][:], f == 0, f == nf - 1,
                                   [wd_r[bi], hb_[f][1]], [py_r])
                            if pi == 0:
                                CP(yacc[:, oc, ts_], py[:], [py_r], [y_r[t][oc]], eng="act")
                            else:
                                TT(yacc[:, oc, ts_], yacc[:, oc, ts_], py[:], ALU.add, [y_r[t][oc], py_r], [y_r[t][oc]])
                        if t == NT - 1 and pi + 2 < len(pieces):
                            load_piece(pi + 2, "d")

                    unit_G(0)
                    for u in range(len(units)):
                        if u + 1 < len(units):
                            unit_G(u + 1)
                        unit_D(u)
                    for t in range(NT):
                        xb = xload[0] % 2; xload[0] += 1
                        xt, xt_r = xt2[xb], xt2_r[xb]
                        tok0 = btok + t * 512
                        DMA("sp", xt[:], src[:, tok0:tok0 + 512].rearrange("(k p) t -> p k t", p=128), W=xt_r)
                        for oc in range(KC):
                            g2 = mod[:, seq, li * 48 + 40 + oc: li * 48 + 41 + oc]
                            STT(xt[:, oc, :], yacc[:, oc, t * 512:(t + 1) * 512], g2, xt[:, oc, :], ALU.mult, ALU.add,
                                [y_r[t][oc], xt_r[oc]], [xt_r[oc]])
                        DMA("sp", Xs[:, tok0:tok0 + 512].rearrange("(k p) t -> p k t", p=128), xt[:], R=xt_r)
                first[0] = False
                P.flush()

        def phase_final():
            src = cur_src()
            with contextlib.ExitStack() as ph:
                def pb(name, shape, dt=F32):
                    return ph.enter_context(nc.sbuf_tensor(uname(name), list(shape), dt))
                pbank = Ring([(banks[i], bank_r[i]) for i in range(8)])
                xt2 = [pb("xtf%d" % i, [128, KC, 512]) for i in range(2)]
                xt2_r = [[Res() for _ in range(KC)] for _ in range(2)]
                ot2 = [pb("otf%d" % i, [128, KC, 512]) for i in range(2)]
                ot2_r = [[Res() for _ in range(KC)] for _ in range(2)]
                sq_ring = Ring([(pb("sqf%d" % i, [128, 512], BF16), Res()) for i in range(3)])
                tmp_ring = Ring([(pb("tmf%d" % i, [128, 512]), Res()) for i in range(3)])
                rstd = pb("rstdf", [128, 512]); rstd_r = Res()
                Af = AB[:, :, 64:72]
                for t in range(T // 512):
                    seq = (t * 512) // L
                    xt, xt_r = xt2[t % 2], xt2_r[t % 2]
                    ot, ot_r = ot2[t % 2], ot2_r[t % 2]
                    tok0 = t * 512
                    DMA("sp", xt[:], src[:, tok0:tok0 + 512].rearrange("(k p) t -> p k t", p=128), W=xt_r)
                    ps, ps_r = pbank.get()
                    for k in range(KC):
                        sq, sq_r = sq_ring.get()
                        ACT(sq[:], xt[:, k, :], AF.Square, [xt_r[k]], [sq_r])
                        MM(ps[:], ones_b, sq[:], k == 0, k == KC - 1, [sq_r, r_const], [ps_r])
                    ACT(rstd[:], ps[:], AF.Ln, [ps_r], [rstd_r], scale=1.0, bias=1024.0 * EPS)
                    ACT(rstd[:], rstd[:], AF.Exp, [rstd_r], [rstd_r], scale=-0.5)
                    for k in range(KC):
                        tm, tm_r = tmp_ring.get()
                        TT(tm[:], xt[:, k, :], rstd[:], ALU.mult, [xt_r[k], rstd_r], [tm_r])
                        ACT(ot[:, k, :], tm[:], AF.Identity, [tm_r], [ot_r[k]],
                            scale=Af[:, seq, k:k + 1], bias=mod[:, seq, 192 + k:193 + k])
                    DMA("sp", outT[:, tok0:tok0 + 512].rearrange("(k p) t -> p k t", p=128), ot[:], R=ot_r)
                P.flush()

        for li, l in enumerate(layers):
            phase_A(li, l)
            if CFG.get('upto', 99) >= 9:
                phase_B(li, l)
        phase_final()
    return nc


_PROG_CACHE = {}


def kernel(x, c, ada_w, ada_b, norm1_w, norm2_w, w_in, conv_w, a_log, dt_bias,
           dn_norm_w, gm_ln_w, gm_ln_b, gm_spatial_w, gm_spatial_b, w_out,
           ffn_w_gate, ffn_w_up, ffn_w_down, moe_router, moe_w_gate, moe_w_up,
           moe_w_down, final_ada_w, final_ada_b, final_norm_w):
    f = lambda a: np.ascontiguousarray(np.asarray(a, dtype=np.float32))
    x = f(x); c = f(c)
    B, L, _ = x.shape
    n_cores = CFG["n_cores"]
    NB = B // n_cores
    layers = list(CFG["layers"])
    key = (L, NB, tuple(layers))
    if key not in _PROG_CACHE:
        import time as _t, sys as _s
        _t0 = _t.time()
        _PROG_CACHE[key] = build(L, NB, layers)
        print("[kernel] build %.1fs" % (_t.time() - _t0), file=_s.stderr)
    nc = _PROG_CACHE[key]
    cons, sel = _consts()

    def pm(v, nchunk):
        v = f(v)
        lead = v.shape[:-1]
        return np.ascontiguousarray(np.moveaxis(v.reshape(*lead, nchunk, 128), -1, 0))

    ada_bT = pm(ada_b, 48).reshape(128, 4 * 48)
    fada_bT = pm(final_ada_b, 16).reshape(128, 16)
    nw = np.concatenate([np.stack([pm(norm1_w, 8), pm(norm2_w, 8)], axis=2).reshape(128, 64),
                         pm(final_norm_w, 8).reshape(128, 8)], axis=1)
    cwp = np.ascontiguousarray(np.transpose(pm(conv_w, 12), (0, 1, 3, 2))).reshape(128, 4 * 48)
    dnw = np.ascontiguousarray(f(dn_norm_w).T)
    alog_bc = np.ascontiguousarray(np.broadcast_to(f(a_log).reshape(1, 16), (128, 16)))
    dtb_bc = np.ascontiguousarray(np.broadcast_to(f(dt_bias).reshape(1, 16), (128, 16)))
    wsT = np.ascontiguousarray(np.transpose(f(gm_spatial_w), (0, 3, 1, 2))).reshape(4, 128, 512)
    gsb = f(gm_spatial_b).reshape(4, 512)
    shared = {
        "consts": cons.reshape(128, NCONST * 128), "sel": sel,
        "ada_w": f(ada_w), "ada_bT": ada_bT, "final_ada_w": f(final_ada_w), "fada_bT": fada_bT, "nw": nw,
        "w_in": f(w_in), "cw": cwp, "dnw": dnw, "alog_bc": alog_bc, "dtb_bc": dtb_bc,
        "gm_ln_w": f(gm_ln_w), "gm_ln_b": f(gm_ln_b), "wsT": wsT, "gsb": gsb, "w_out": f(w_out),
        "ffn_w_gate": f(ffn_w_gate), "ffn_w_up": f(ffn_w_up), "ffn_w_down": f(ffn_w_down),
        "moe_router": f(moe_router), "moe_w_gate": f(moe_w_gate), "moe_w_up": f(moe_w_up), "moe_w_down": f(moe_w_down),
    }
    if not any(l % 2 == 1 for l in layers):
        for k_ in ("moe_w_gate", "moe_w_up", "moe_w_down"):
            shared[k_] = np.zeros((1, 1, 1, 1), np.float32)
    in_maps = []
    for i in range(n_cores):
        xs = x[i * NB:(i + 1) * NB].reshape(NB * L, D)
        cs = c[i * NB:(i + 1) * NB]
        cTl = np.ascontiguousarray(np.transpose(cs.reshape(NB, KC, 128), (2, 1, 0))).reshape(128, KC * NB)
        m = dict(shared)
        m["xT"] = np.ascontiguousarray(xs.T)
        m["cT"] = cTl
        in_maps.append(m)
    import time as _t, sys as _s
    _t0 = _t.time()
    if CFG.get("sim"):
        return nc, in_maps
    res = run_bass_kernel_spmd(nc, in_maps, core_ids=list(range(n_cores)))
    print("[kernel] launch+transfer %.1fs" % (_t.time() - _t0), file=_s.stderr)
    out = np.empty((B, L, D), np.float32)
    for i in range(n_cores):
        out[i * NB:(i + 1) * NB] = res.results[i]["outT"].T.reshape(NB, L, D)
    return out
```

```python
import contextlib
import numpy as np
import concourse.bass as bass
import concourse.mybir as mybir
from concourse.bass_utils import run_bass_kernel_spmd

F32 = mybir.dt.float32
BF16 = mybir.dt.bfloat16
AF = mybir.ActivationFunctionType
ALU = mybir.AluOpType
AX = mybir.AxisListType

D = 1024
KC = 8
DIN = 3080
OFF_Z, OFF_BA, OFF_GU, OFF_GV = 1536, 2048, 2056, 2568
FF_DENSE = 2816
FF_MOE = 3584
NE = 8
EPS = 1e-6
N_CORES = 8

CFG = {"L": 4096, "NB": 2, "layers": [0, 1, 2, 3], "n_cores": 8}

ENGS = ["pe", "act", "dve", "pool", "sp"]
KEYS = ["pe", "act", "dve", "pool", "sp_dma", "pool_dma", "act_dma"]


class Res:
    __slots__ = ("w", "r", "x")

    def __init__(self, excl=False):
        self.w = None
        self.r = {}
        self.x = excl


class Prog:
    def __init__(self, nc, st):
        self.nc = nc
        self.q = {e: [] for e in ENGS}
        self.cnt = {k: 0 for k in KEYS}
        self.seen = {e: {} for e in ENGS}
        self.signaled = {k: set() for k in KEYS}
        self.rank_base = {k: 0 for k in KEYS}
        self.sems = {k: st.enter_context(nc.semaphore("s_" + k)) for k in KEYS}
        self.nops = 0
        self.mute = False

    def op(self, eng, fn, reads=(), writes=(), dma=False):
        if self.mute:
            return
        if any(r.x for r in reads):
            writes = list(writes) + [r for r in reads if r.x]
            reads = [r for r in reads if not r.x]
        key = eng + "_dma" if dma else eng
        idx = self.cnt[key] + 1
        self.cnt[key] = idx
        waits = {}
        for r in reads:
            if r.w is not None:
                k, v = r.w
                if not (k == "pe" and eng == "pe") and waits.get(k, 0) < v:
                    waits[k] = v
        for w in writes:
            if w.w is not None:
                k, v = w.w
                if not (k == "pe" and eng == "pe") and waits.get(k, 0) < v:
                    waits[k] = v
            for k, v in w.r.items():
                if k == eng:
                    continue
                if waits.get(k, 0) < v:
                    waits[k] = v
        wl = []
        seen = self.seen[eng]
        for k, v in waits.items():
            if seen.get(k, 0) >= v:
                continue
            seen[k] = v
            wl.append((k, v))
            self.signaled[k].add(v)
        for r in reads:
            if r.r.get(key, 0) < idx:
                r.r[key] = idx
        for w in writes:
            w.w = (key, idx)
            w.r = {}
        if dma:
            self.signaled[key].add(idx)
        self.q[eng].append((wl, fn, key, idx))
        self.nops += 1

    def barrier(self):
        tot = dict(self.cnt)
        for e in ENGS:
            wl = []
            for k, v in tot.items():
                if v == 0 or k == e:
                    continue
                if self.seen[e].get(k, 0) >= v:
                    continue
                self.seen[e][k] = v
                wl.append((k, v))
                self.signaled[k].add(v)
            if wl:
                self.q[e].append((wl, None, None, None))

    def flush(self):
        self.barrier()
        ranks = {}
        for k in KEYS:
            s = sorted(self.signaled[k])
            base = self.rank_base[k]
            ranks[k] = {v: base + i + 1 for i, v in enumerate(s)}
            self.rank_base[k] = base + len(s)
            self.signaled[k] = set()
        sems = self.sems
        q = self.q
        self.q = {e: [] for e in ENGS}

        def run(e):
            def body(engine):
                for wl, fn, key, idx in q[e]:
                    for k, v in wl:
                        engine.wait_ge(sems[k], ranks[k][v] * (16 if k.endswith("_dma") else 1))
                    if fn is None:
                        continue
                    ins = fn(engine)
                    if idx in ranks[key]:
                        ins.then_inc(sems[key], 16 if key.endswith("_dma") else 1)
            return body

        with self.nc.Block() as block:
            block.tensor(run("pe"))
            block.scalar(run("act"))
            block.vector(run("dve"))
            block.gpsimd(run("pool"))
            block.sync(run("sp"))


class Ring:
    def __init__(self, items):
        self.items = items
        self.i = 0

    def get(self):
        it = self.items[self.i % len(self.items)]
        self.i += 1
        return it


C_ID, C_ONES, C_TRIL, C_TRILS, C_TRIU, C_M0 = 0, 1, 2, 3, 4, 5
NCONST = 12


def _consts():
    c = np.zeros((NCONST, 128, 128), np.float32)
    i = np.arange(128)[:, None]
    j = np.arange(128)[None, :]
    c[C_ID] = (i == j)
    c[C_ONES] = 1.0
    c[C_TRIL] = (i >= j)
    c[C_TRILS] = (i > j)
    c[C_TRIU] = (i <= j)
    for l in range(7):
        b = 1 << l
        c[C_M0 + l] = ((i // (2 * b)) == (j // (2 * b))) & ((i % (2 * b)) >= b) & ((j % (2 * b)) < b)
    cons = np.ascontiguousarray(c.transpose(1, 0, 2))
    sel = np.zeros((8, 8, 128), np.float32)
    for e in range(8):
        sel[e, e, :] = 1.0
    return cons, sel.reshape(8, 1024)


def build(L, NB, layers):
    T = NB * L
    nl = len(layers)
    nc = bass.Bass("TRN2", target_bir_lowering=False)

    def din(name, shape, dt=F32):
        return nc.dram_tensor(name, list(shape), dt, kind="ExternalInput").ap()

    xT_in = din("xT", [D, T])
    outT = nc.dram_tensor("outT", [D, T], F32, kind="ExternalOutput").ap()
    Xs = nc.dram_tensor("Xs", [D, T], F32, kind="Internal").ap()
    cT_d = din("cT", [128, KC * NB])
    consts_d = din("consts", [128, NCONST * 128])
    sel_d = din("sel", [8, 1024])
    ada_w_d = din("ada_w", [4, D, 6 * D])
    ada_bT_d = din("ada_bT", [128, 4 * 48])
    fada_w_d = din("final_ada_w", [D, 2 * D])
    fada_bT_d = din("fada_bT", [128, 16])
    nw_d = din("nw", [128, 4 * 16 + 8])
    w_in_d = din("w_in", [4, D, DIN])
    cw_d = din("cw", [128, 4 * 48])
    dnw_d = din("dnw", [128, 4])
    alog_d = din("alog_bc", [128, 16])
    dtb_d = din("dtb_bc", [128, 16])
    lnw_d = din("gm_ln_w", [4, 512])
    lnb_d = din("gm_ln_b", [4, 512])
    wsT_d = din("wsT", [4, 128, 512])
    gsb_d = din("gsb", [4, 512])
    w_out_d = din("w_out", [4, D, D])
    fg_d = din("ffn_w_gate", [2, D, FF_DENSE])
    fu_d = din("ffn_w_up", [2, D, FF_DENSE])
    fd_d = din("ffn_w_down", [2, FF_DENSE, D])
    rt_d = din("moe_router", [2, D, NE])
    has_moe = any(l % 2 == 1 for l in layers)
    mg_d = din("moe_w_gate", [2, NE, D, FF_MOE] if has_moe else [1, 1, 1, 1])
    mu_d = din("moe_w_up", [2, NE, D, FF_MOE] if has_moe else [1, 1, 1, 1])
    md_d = din("moe_w_down", [2, NE, FF_MOE, D] if has_moe else [1, 1, 1, 1])

    with contextlib.ExitStack() as st:
        uid = [0]

        def uname(name):
            uid[0] += 1
            return "sb%d_%s" % (uid[0], name)

        def sb(name, shape, dt=F32):
            return st.enter_context(nc.sbuf_tensor(uname(name), list(shape), dt))

        P = Prog(nc, st)
        banks = [st.enter_context(nc.psum_tensor("bank%d" % i, [128, 512], F32)) for i in range(8)]
        bank_r = [Res(True) for _ in range(8)]

        cons = sb("cons", [128, NCONST, 128])
        cons_b = sb("cons_b", [128, 2, 128], BF16)
        onesrow_b = sb("onesrow_b", [1, 128], BF16)
        cT = sb("cT", [128, KC * NB])
        cact_b = sb("cact_b", [128, KC * NB], BF16)
        mod = sb("mod", [128, NB, 4 * 48 + 16])
        ada_bT = sb("ada_bT", [128, 4 * 48 + 16])
        nw = sb("nw", [128, 4 * 16 + 8])
        AB = sb("AB", [128, NB, (4 * 2 + 1) * 8])
        cw = sb("cw", [128, 4 * 48])
        dnw = sb("dnw", [128, 4])
        nA = sb("nA", [128, 16])
        dtb = sb("dtb", [128, 16])
        r_const = Res()

        def DMA(eng, out, in_, R=(), W=()):
            P.op(eng, lambda e: e.dma_start(out=out, in_=in_), R, W, dma=True)

        def MM(out, lhsT, rhs, start, stop, R, W):
            P.op("pe", lambda e: e.matmul(out, lhsT, rhs, start=start, stop=stop), R, W)

        def ACT(out, in_, func, R, W, scale=1.0, bias=0.0):
            P.op("act", lambda e: e.activation(out, in_, func, bias=bias, scale=scale), R, W)

        def TT(out, a, b, op, R, W, eng="dve"):
            P.op(eng, lambda e: e.tensor_tensor(out, a, b, op), R, W)

        def TS(out, a, s1, s2, op0, op1, R, W, eng="dve"):
            if s2 is None:
                P.op(eng, lambda e: e.tensor_scalar(out, a, s1, None, op0), R, W)
            else:
                P.op(eng, lambda e: e.tensor_scalar(out, a, s1, s2, op0, op1), R, W)

        def STT(out, a, s, b, op0, op1, R, W, eng="dve"):
            P.op(eng, lambda e: e.scalar_tensor_tensor(out, a, s, b, op0, op1), R, W)

        def CP(out, in_, R, W, eng="dve"):
            if eng == "act":
                P.op("act", lambda e: e.copy(out, in_), R, W)
            else:
                P.op(eng, lambda e: e.tensor_copy(out, in_), R, W)

        def RECIP(out, in_, R, W):
            P.op("dve", lambda e: e.reciprocal(out, in_), R, W)

        def MEMSET(ap, val, W, eng="pool"):
            P.op(eng, lambda e: e.memset(ap, val), (), W)

        DMA("sp", cons[:], consts_d.rearrange("p (a b) -> p a b", a=NCONST), W=[r_const])
        DMA("pool", cons_b[:], consts_d[:, 0:256].rearrange("p (a b) -> p a b", a=2), W=[r_const])
        DMA("sp", cT[:], cT_d, W=[r_const])
        DMA("sp", ada_bT[:, 0:192], ada_bT_d, W=[r_const])
        DMA("sp", ada_bT[:, 192:208], fada_bT_d, W=[r_const])
        DMA("sp", nw[:], nw_d, W=[r_const])
        DMA("sp", cw[:], cw_d, W=[r_const])
        DMA("sp", dnw[:], dnw_d, W=[r_const])
        DMA("sp", nA[:], alog_d, W=[r_const])
        DMA("sp", dtb[:], dtb_d, W=[r_const])
        MEMSET(onesrow_b[:], 1.0, [r_const])
        ACT(nA[:], nA[:], AF.Exp, [r_const], [r_const])
        TS(nA[:], nA[:], -1.0, None, ALU.mult, None, [r_const], [r_const])
        ACT(cact_b[:], cT[:], AF.Silu, [r_const], [r_const])

        ident_f = cons[:, C_ID, :]
        ones_f = cons[:, C_ONES, :]
        trils_f = cons[:, C_TRILS, :]
        triu_f = cons[:, C_TRIU, :]
        ident_b = cons_b[:, 0, :]
        ones_b = cons_b[:, 1, :]

        with contextlib.ExitStack() as ph:
            wbuf = [ph.enter_context(nc.sbuf_tensor(uname("adaw"), [128, KC, 1024], BF16)) for i in range(2)]
            wbuf_r = [Res(), Res()]
            mod_r = Res()
            pieces = []
            for li, l in enumerate(layers):
                for j in range(6):
                    pieces.append((ada_w_d[l, :, j * 1024:(j + 1) * 1024], li * 48 + j * 8))
            for j in range(2):
                pieces.append((fada_w_d[:, j * 1024:(j + 1) * 1024], 192 + j * 8))
            for pi, (src, col0) in enumerate(pieces):
                wb, wr = wbuf[pi % 2], wbuf_r[pi % 2]
                DMA("pool", wb[:], src.rearrange("(k p) n -> p k n", p=128), W=[wr])
                for oc in range(8):
                    bk = pi * 8 + oc
                    ps = banks[bk % 8][:, 0:NB]
                    pr = bank_r[bk % 8]
                    for k in range(KC):
                        MM(ps, wb[:, k, oc * 128:(oc + 1) * 128], cact_b[:, k * NB:(k + 1) * NB],
                           k == 0, k == KC - 1, [wr, r_const], [pr])
                    bcol = (layers[col0 // 48] * 48 + col0 % 48 + oc) if col0 < 192 else (192 + col0 - 192 + oc)
                    TS(mod[:, :, col0 + oc], ps, ada_bT[:, bcol:bcol + 1], None, ALU.add, None,
                       [pr, r_const], [mod_r])
            MEMSET(AB[:], 0.0, [mod_r])
            for li, l in enumerate(layers):
                for sub in range(2):
                    for b in range(NB):
                        sc = mod[:, b, li * 48 + (sub * 3 + 1) * 8: li * 48 + (sub * 3 + 2) * 8]
                        STT(AB[:, b, (li * 2 + sub) * 8:(li * 2 + sub + 1) * 8], sc, 1.0,
                            nw[:, l * 16 + sub * 8: l * 16 + sub * 8 + 8], ALU.add, ALU.mult, [mod_r, r_const], [mod_r])
            for b in range(NB):
                STT(AB[:, b, 64:72], mod[:, b, 200:208], 1.0, nw[:, 64:72], ALU.add, ALU.mult, [mod_r, r_const], [mod_r])
            TS(AB[:], AB[:], 32.0, None, ALU.mult, None, [mod_r], [mod_r])
            P.flush()

        def norm_mod(xt, xt_r, ncols, A, B, out_b, out_r, sq_ring, tmp_ring, rstd, rstd_r, ps, ps_r, out_f=None, out_f_r=None):
            for k in range(KC):
                sq, sq_r = sq_ring.get()
                ACT(sq[:, 0:ncols], xt[:, k, 0:ncols], AF.Square, [xt_r[k]], [sq_r])
                MM(ps[:, 0:ncols], ones_b, sq[:, 0:ncols], k == 0, k == KC - 1, [sq_r, r_const], [ps_r])
            ACT(rstd[:, 0:ncols], ps[:, 0:ncols], AF.Ln, [ps_r], [rstd_r], scale=1.0, bias=1024.0 * EPS)
            ACT(rstd[:, 0:ncols], rstd[:, 0:ncols], AF.Exp, [rstd_r], [rstd_r], scale=-0.5)
            for k in range(KC):
                tm, tm_r = tmp_ring.get()
                TT(tm[:, 0:ncols], xt[:, k, 0:ncols], rstd[:, 0:ncols], ALU.mult, [xt_r[k], rstd_r], [tm_r])
                if out_f is not None:
                    ACT(out_f[:, k, 0:ncols], tm[:, 0:ncols], AF.Identity, [tm_r], [out_f_r[k]],
                        scale=A[:, k:k + 1], bias=B[:, k:k + 1])
                ACT(out_b[:, k, 0:ncols], tm[:, 0:ncols], AF.Identity, [tm_r], [out_r[k]],
                    scale=A[:, k:k + 1], bias=B[:, k:k + 1])

        first = [True]

        def cur_src():
            return xT_in if first[0] else Xs

        def phase_A(li, l):
            TTK = 256
            NCH = TTK // 128
            src = cur_src()
            with contextlib.ExitStack() as ph:
                def pb(name, shape, dt=F32):
                    return ph.enter_context(nc.sbuf_tensor(uname(name), list(shape), dt))

                class _BankRing:
                    def __init__(self, ncol):
                        self.ncol = ncol

                    def get(self):
                        i = bring[0] % 8
                        bring[0] += 1
                        return (banks[i][:, 0:self.ncol], bank_r[i])
                bring = [0]
                wide = _BankRing(256)
                small = _BankRing(128)
                fullr = _BankRing(512)

                w_in_b = pb("w_in_b", [128, KC, DIN], BF16)
                w_out_b = pb("w_out_b", [128, KC, D], BF16)
                wsT_f = pb("wsT_f", [128, 512])
                wsT_b = pb("wsT_b", [128, 512], BF16)
                brow_b = pb("brow_b", [1, 512], BF16)
                lnw = pb("lnw", [128, 512])
                lnb = pb("lnb", [128, 512])
                r_w = Res()
                for k in range(KC):
                    DMA("pool", w_in_b[:, k, :], w_in_d[l, k * 128:(k + 1) * 128, :], W=[r_w])
                DMA("pool", w_out_b[:], w_out_d[l].rearrange("(k p) n -> p k n", p=128), W=[r_w])
                DMA("sp", wsT_f[:], wsT_d[l], W=[r_w])
                DMA("pool", brow_b[:], gsb_d[l:l + 1, :], W=[r_w])
                DMA("sp", lnw[:], lnw_d[l:l + 1, :].to_broadcast([128, 512]), W=[r_w])
                DMA("sp", lnb[:], lnb_d[l:l + 1, :].to_broadcast([128, 512]), W=[r_w])
                for g in range(4):
                    TT(wsT_b[:, g * 128:(g + 1) * 128], wsT_f[:, g * 128:(g + 1) * 128], triu_f, ALU.mult,
                       [r_w, r_const], [r_w])

                xtL = [(pb("xt%d" % i, [128, KC, TTK]), [Res() for _ in range(KC)]) for i in range(2)]
                hTL = [(pb("hT%d" % i, [128, KC, TTK], BF16), [Res() for _ in range(KC)]) for i in range(2)]
                sq_ring = Ring([(pb("sqa%d" % i, [128, TTK], BF16), Res()) for i in range(3)])
                tmp_ring = Ring([(pb("tma%d" % i, [128, TTK]), Res()) for i in range(3)])
                rstd = pb("rstd", [128, TTK]); rstd_r = Res()
                pre = pb("pre", [128, 12, TTK + 3], BF16); pre_r = [Res() for _ in range(12)]
                qs = pb("qs", [128, 4, TTK], BF16); ks = pb("ks", [128, 4, TTK], BF16); vs_b = pb("vs_b", [128, 4, TTK], BF16)
                qkv_r = [Res() for _ in range(12)]
                qn_b = pb("qn_b", [128, 4, TTK], BF16); kn_b = pb("kn_b", [128, 4, TTK], BF16)
                qn_r = [Res() for _ in range(4)]; kn_r = [Res() for _ in range(4)]
                zs = pb("zs", [128, 4, TTK], BF16); zs_r = [Res() for _ in range(4)]
                gus = pb("gus", [128, 4, TTK], BF16); gus_r = [Res() for _ in range(4)]
                gvf_ring = Ring([(pb("gvf%d" % i, [128, 512]), Res()) for i in range(2)])
                gv_b = pb("gv_b", [128, NCH, 512], BF16); gv_r = [Res() for _ in range(NCH)]
                stat = pb("stat", [128, 8]); stat_r = Res()
                mixT = pb("mixT", [128, KC, TTK], BF16); mix_r = [Res() for _ in range(KC)]
                oT = pb("oT", [128, 4, TTK]); oT_r = [Res() for _ in range(4)]
                ba = pb("ba", [128, NCH * 8]); ba_r = Res()
                g_tm = pb("g_tm", [128, NCH * 4]); beta_tm = pb("beta_tm", [128, NCH * 4])
                G_tm = pb("G_tm", [128, NCH * 4]); e1 = pb("e1", [128, NCH * 4]); e2 = pb("e2", [128, NCH * 4])
                eGl = pb("eGl", [128, NCH * 4]); sm_r = Res()
                S_f = pb("S_f", [128, 4, 128]); S_b = pb("S_b", [128, 4, 128], BF16)
                S_r = [Res() for _ in range(4)]; Sb_r = [Res() for _ in range(4)]
                def hb(name, dt=F32):
                    return [(pb("%s%d" % (name, h), [128, 128], dt), Res()) for h in range(4 * NCH)]
                gbc = hb("gbc"); eG = hb("eG", BF16); dd = hb("dd", BF16); dt_ = hb("dt_", BF16)
                Am = hb("Am", BF16); AM = [hb("AM%d_" % lv, BF16) for lv in range(2)]
                U0 = hb("U0", BF16); U1 = hb("U1", BF16); T0 = hb("T0", BF16); T1 = hb("T1", BF16)
                P1 = hb("P1", BF16); attnT = hb("attnT", BF16); qd = hb("qd", BF16)
                kbd = hb("kbd", BF16); kdec = hb("kdec", BF16); vb = hb("vb", BF16)
                u_f = hb("u_f"); wT = hb("wT", BF16); vnew = hb("vnew", BF16)

                A1 = AB[:, :, (li * 2) * 8:(li * 2 + 1) * 8]
                c4 = l * 4

                for seq in range(NB):
                    for h in range(4):
                        MEMSET(S_f[:, h, :], 0.0, [S_r[h]])
                        MEMSET(S_b[:, h, :], 0.0, [Sb_r[h]])
                    for j in range(12):
                        MEMSET(pre[:, j, 0:3], 0.0, [pre_r[j]])
                    def norm_part(seq_, it_, bi_):
                        xt_, xt_r_ = xtL[bi_]
                        hT_, hT_r_ = hTL[bi_]
                        tk = seq_ * L + it_ * TTK
                        DMA("sp", xt_[:], src[:, tk:tk + TTK].rearrange("(k p) t -> p k t", p=128), W=xt_r_)
                        ps_, ps_r_ = wide.get()
                        norm_mod(xt_, xt_r_, TTK, A1[:, seq_, :], mod[:, seq_, li * 48: li * 48 + 8], hT_, hT_r_,
                                 sq_ring, tmp_ring, rstd, rstd_r, ps_, ps_r_)
                    NTL = L // TTK
                    if seq == 0:
                        norm_part(0, 0, 0)
                    for it in range(NTL):
                        tok0 = seq * L + it * TTK
                        gidx = seq * NTL + it
                        xt, xt_r = xtL[gidx % 2]
                        hT, hT_r = hTL[gidx % 2]
                        def proj(col0):
                            ps, ps_r = wide.get()
                            for k in range(KC):
                                MM(ps, w_in_b[:, k, col0:col0 + 128], hT[:, k, :], k == 0, k == KC - 1,
                                   [r_w, hT_r[k]], [ps_r])
                            return ps, ps_r
                        for j in range(12):
                            ps, ps_r = proj(j * 128)
                            CP(pre[:, j, 3:3 + TTK], ps, [ps_r], [pre_r[j]], eng="act")
                            acc_t, acc_r = tmp_ring.get()
                            acc = acc_t[:]
                            cwj = cw[:, l * 48 + j * 4: l * 48 + j * 4 + 4]
                            TS(acc, pre[:, j, 0:TTK], cwj[:, 0:1], None, ALU.mult, None, [pre_r[j], r_const], [acc_r])
                            for tp in range(1, 4):
                                STT(acc, pre[:, j, tp:tp + TTK], cwj[:, tp:tp + 1], acc, ALU.mult, ALU.add,
                                    [pre_r[j], acc_r, r_const], [acc_r])
                            dst = (qs, ks, vs_b)[j // 4][:, j % 4, :]
                            ACT(dst, acc, AF.Silu, [acc_r], [qkv_r[j]])
                            CP(pre[:, j, 0:3], pre[:, j, TTK:TTK + 3], [pre_r[j]], [pre_r[j]], eng="pool")
                        for h in range(4):
                            ps, ps_r = proj(OFF_Z + h * 128)
                            ACT(zs[:, h, :], ps, AF.Silu, [ps_r], [zs_r[h]])
                        for g in range(4):
                            ps, ps_r = proj(OFF_GU + g * 128)
                            ACT(gus[:, g, :], ps, AF.Gelu_apprx_tanh, [ps_r], [gus_r[g]])
                        P.mute = CFG.get('upto', 99) < 2
                        psba, psba_r = small.get()
                        for c in range(NCH):
                            for k in range(KC):
                                MM(psba[:, c * 8:(c + 1) * 8], hT[:, k, c * 128:(c + 1) * 128],
                                   w_in_b[:, k, OFF_BA:OFF_BA + 8], k == 0, k == KC - 1, [r_w, hT_r[k]], [psba_r])
                        CP(ba[:], psba[:, 0:NCH * 8], [psba_r], [ba_r])
                        for c in range(NCH):
                            ACT(beta_tm[:, c * 4:c * 4 + 4], ba[:, c * 8:c * 8 + 4], AF.Sigmoid, [ba_r], [sm_r])
                            TT(g_tm[:, c * 4:c * 4 + 4], ba[:, c * 8 + 4:c * 8 + 8], dtb[:, c4:c4 + 4], ALU.add, [ba_r, r_const], [sm_r])
                        g2d = g_tm[:]
                        ACT(g2d, g2d, AF.Exp, [sm_r], [sm_r])
                        ACT(g2d, g2d, AF.Ln, [sm_r], [sm_r], scale=1.0, bias=1.0)
                        for c in range(NCH):
                            TT(g_tm[:, c * 4:c * 4 + 4], g_tm[:, c * 4:c * 4 + 4], nA[:, c4:c4 + 4], ALU.mult, [sm_r, r_const], [sm_r])
                        for c in range(NCH):
                            psg, psg_r = fullr.get()
                            for k in range(KC):
                                MM(psg[:], hT[:, k, c * 128:(c + 1) * 128], w_in_b[:, k, OFF_GV:OFF_GV + 512],
                                   k == 0, k == KC - 1, [r_w, hT_r[k]], [psg_r])
                            gvf, gvf_r = gvf_ring.get()
                            ACT(gvf[:], psg[:], AF.Gelu_apprx_tanh, [psg_r], [gvf_r])
                            P.op("dve", lambda e, gvf=gvf, c=c: e.reduce_sum(stat[:, c:c + 1], gvf[:], AX.X), [gvf_r], [stat_r])
                            TS(stat[:, c:c + 1], stat[:, c:c + 1], 1.0 / 512, None, ALU.mult, None, [stat_r], [stat_r])
                            TS(gvf[:], gvf[:], stat[:, c:c + 1], None, ALU.subtract, None, [gvf_r, stat_r], [gvf_r])
                            sq2, sq2_r = gvf_ring.get()
                            TT(sq2[:], gvf[:], gvf[:], ALU.mult, [gvf_r], [sq2_r])
                            P.op("dve", lambda e, sq2=sq2, c=c: e.reduce_sum(stat[:, 4 + c:5 + c], sq2[:], AX.X), [sq2_r], [stat_r])
                            ACT(stat[:, 4 + c:5 + c], stat[:, 4 + c:5 + c], AF.Ln, [stat_r], [stat_r], scale=1.0 / 512, bias=EPS)
                            ACT(stat[:, 4 + c:5 + c], stat[:, 4 + c:5 + c], AF.Exp, [stat_r], [stat_r], scale=-0.5)
                            STT(gvf[:], gvf[:], stat[:, 4 + c:5 + c], lnw[:], ALU.mult, ALU.mult, [gvf_r, stat_r, r_w], [gvf_r])
                            TT(gv_b[:, c, :], gvf[:], lnb[:], ALU.add, [gvf_r, r_w], [gv_r[c]])
                        P.mute = CFG.get('upto', 99) < 3
                        for g in range(4):
                            ps, ps_r = wide.get()
                            for c in range(NCH):
                                MM(ps[:, c * 128:(c + 1) * 128], gv_b[:, c, g * 128:(g + 1) * 128],
                                   wsT_b[:, g * 128:(g + 1) * 128], True, False, [gv_r[c], r_w], [ps_r])
                                MM(ps[:, c * 128:(c + 1) * 128], onesrow_b[0:1, :], brow_b[0:1, g * 128:(g + 1) * 128],
                                   False, True, [r_const, r_w], [ps_r])
                            TT(mixT[:, 4 + g, :], ps, gus[:, g, :], ALU.mult, [ps_r, gus_r[g]], [mix_r[4 + g]])
                        P.mute = CFG.get('upto', 99) < 4
                        for h in range(4):
                            for (srcb, r_i, dstb, dst_r, scl) in ((qs, h, qn_b, qn_r, 128.0 ** -0.5), (ks, 4 + h, kn_b, kn_r, 1.0)):
                                sq, sq_r = sq_ring.get()
                                ACT(sq[:], srcb[:, h, :], AF.Square, [qkv_r[r_i]], [sq_r])
                                ps, ps_r = wide.get()
                                MM(ps, ones_b, sq[:], True, True, [sq_r, r_const], [ps_r])
                                tm, tm_r = tmp_ring.get()
                                ACT(tm[:], ps, AF.Ln, [ps_r], [tm_r], scale=1.0, bias=EPS)
                                ACT(tm[:], tm[:], AF.Exp, [tm_r], [tm_r], scale=-0.5)
                                STT(dstb[:, h, :], srcb[:, h, :], scl, tm[:], ALU.mult, ALU.mult, [qkv_r[r_i], tm_r], [dst_r[h]])
                        psl, psl_r = small.get()
                        for c in range(NCH):
                            MM(psl[:, c * 4:(c + 1) * 4], ones_f, g_tm[:, c * 4:c * 4 + 4], True, True, [sm_r, r_const], [psl_r])
                            MM(psl[:, 16 + c * 4:16 + (c + 1) * 4], triu_f, g_tm[:, c * 4:c * 4 + 4], True, True, [sm_r, r_const], [psl_r])
                        sm2_r = Res()
                        CP(G_tm[:], psl[:, 16:16 + NCH * 4], [psl_r], [sm2_r])
                        ACT(eGl[:], psl[:, 0:NCH * 4], AF.Exp, [psl_r], [sm2_r])
                        TT(e2[:], psl[:, 0:NCH * 4], G_tm[:],
                           ALU.subtract, [psl_r, sm2_r], [sm2_r])
                        ACT(e2[:], e2[:], AF.Exp, [sm2_r], [sm2_r])
                        ACT(e1[:], G_tm[:], AF.Exp, [sm2_r], [sm2_r])
                        TT(e1[:], e1[:],
                           beta_tm[:], ALU.mult, [sm2_r, sm_r], [sm2_r])
                        P.mute = CFG.get('upto', 99) < 5
                        if gidx + 1 < NB * NTL:
                            mute_save = P.mute
                            P.mute = False
                            norm_part((gidx + 1) // NTL, (gidx + 1) % NTL, (gidx + 1) % 2)
                            P.mute = mute_save
                        CHN = [(c, h) for c in range(NCH) for h in range(4)]
                        def csl(c):
                            return slice(c * 128, (c + 1) * 128)
                        def col(c, h):
                            return slice(c * 4 + h, c * 4 + h + 1)
                        psG = {}
                        for (c, h) in CHN:
                            i = c * 4 + h
                            TS(gbc[i][0][:], ones_f, g_tm[:, col(c, h)], None, ALU.mult, None, [sm_r, r_const], [gbc[i][1]])
                            psG[i] = small.get()
                            MM(psG[i][0], gbc[i][0][:], triu_f, True, True, [gbc[i][1], r_const], [psG[i][1]])
                        for (c, h) in CHN:
                            i = c * 4 + h
                            pg, pg_r = psG[i]
                            Gc = G_tm[:, col(c, h)]
                            ACT(eG[i][0][:], pg, AF.Exp, [pg_r], [eG[i][1]])
                            TS(dd[i][0][:], pg, Gc, 0.0, ALU.subtract, ALU.max, [pg_r, sm2_r], [dd[i][1]])
                            TS(dt_[i][0][:], pg, Gc, 0.0, ALU.subtract, ALU.min, [pg_r, sm2_r], [dt_[i][1]])
                            ACT(dd[i][0][:], dd[i][0][:], AF.Exp, [dd[i][1]], [dd[i][1]], scale=-1.0)
                            ACT(dt_[i][0][:], dt_[i][0][:], AF.Exp, [dt_[i][1]], [dt_[i][1]])
                            TT(dd[i][0][:], dd[i][0][:], trils_f, ALU.mult, [dd[i][1], r_const], [dd[i][1]], eng="pool")
                            TT(dt_[i][0][:], dt_[i][0][:], triu_f, ALU.mult, [dt_[i][1], r_const], [dt_[i][1]], eng="pool")
                            TT(qd[i][0][:], qn_b[:, h, csl(c)], eG[i][0][:], ALU.mult, [qn_r[h], eG[i][1]], [qd[i][1]])
                        for (c, h) in CHN:
                            i = c * 4 + h
                            cs = csl(c)
                            pk, pk_r = small.get()
                            MM(pk, kn_b[:, h, cs], kn_b[:, h, cs], True, True, [kn_r[h]], [pk_r])
                            STT(Am[i][0][:], pk, beta_tm[:, col(c, h)], dd[i][0][:], ALU.mult, ALU.mult,
                                [pk_r, sm_r, dd[i][1]], [Am[i][1]])
                            pq, pq_r = small.get()
                            MM(pq, kn_b[:, h, cs], qn_b[:, h, cs], True, True, [kn_r[h], qn_r[h]], [pq_r])
                            TT(attnT[i][0][:], pq, dt_[i][0][:], ALU.mult, [pq_r, dt_[i][1]], [attnT[i][1]])
                            pt, pt_r = small.get()
                            MM(pt, kn_b[:, h, cs], ident_b, True, True, [kn_r[h], r_const], [pt_r])
                            TS(kbd[i][0][:], pt, e1[:, col(c, h)], None, ALU.mult, None, [pt_r, sm2_r], [kbd[i][1]])
                            ACT(kdec[i][0][:], pt, AF.Identity, [pt_r, sm2_r], [kdec[i][1]], scale=e2[:, col(c, h)])
                            pv, pv_r = small.get()
                            MM(pv, vs_b[:, h, cs], ident_b, True, True, [qkv_r[8 + h], r_const], [pv_r])
                            TS(vb[i][0][:], pv, beta_tm[:, col(c, h)], None, ALU.mult, None, [pv_r, sm_r], [vb[i][1]])
                        P.mute = CFG.get('upto', 99) < 6
                        Ucur = {}; Tcur = {}
                        for (c, h) in CHN:
                            i = c * 4 + h
                            a0, a0_r = AM[0][i]
                            TT(a0[:], Am[i][0][:], cons[:, C_M0, :], ALU.mult, [Am[i][1], r_const], [a0_r], eng="pool")
                            pt, pt_r = small.get()
                            MM(pt, a0[:], ident_b, True, True, [a0_r, r_const], [pt_r])
                            TT(U0[i][0][:], ident_f, pt, ALU.subtract, [pt_r, r_const], [U0[i][1]])
                            TT(T0[i][0][:], ident_f, a0[:], ALU.subtract, [a0_r, r_const], [T0[i][1]], eng="pool")
                            Ucur[i] = U0[i]; Tcur[i] = T0[i]
                        for lv in range(1, 7):
                            pp = {}
                            for i in range(NCH * 4):
                                al, al_r = AM[lv % 2][i]
                                TT(al[:], Am[i][0][:], cons[:, C_M0 + lv, :], ALU.mult, [Am[i][1], r_const], [al_r],
                                   eng=("pool" if i % 2 == 0 else "dve"))
                                pp[i] = small.get()
                                MM(pp[i][0], al[:], Ucur[i][0][:], True, True, [al_r, Ucur[i][1]], [pp[i][1]])
                            for i in range(NCH * 4):
                                CP(P1[i][0][:], pp[i][0], [pp[i][1]], [P1[i][1]], eng="act")
                            px = {}
                            for i in range(NCH * 4):
                                px[i] = small.get()
                                MM(px[i][0], Tcur[i][0][:], P1[i][0][:], True, True, [Tcur[i][1], P1[i][1]], [px[i][1]])
                            for i in range(NCH * 4):
                                Un = U1[i] if Ucur[i] is U0[i] else U0[i]
                                TT(Un[0][:], Ucur[i][0][:], px[i][0], ALU.subtract, [Ucur[i][1], px[i][1]], [Un[1]])
                                Ucur[i] = Un
                            if lv < 6:
                                ptt = {}
                                for i in range(NCH * 4):
                                    ptt[i] = small.get()
                                    MM(ptt[i][0], Ucur[i][0][:], ident_b, True, True, [Ucur[i][1], r_const], [ptt[i][1]])
                                for i in range(NCH * 4):
                                    Tn = T1[i] if Tcur[i] is T0[i] else T0[i]
                                    CP(Tn[0][:], ptt[i][0], [ptt[i][1]], [Tn[1]], eng="act")
                                    Tcur[i] = Tn
                        P.mute = CFG.get('upto', 99) < 7
                        for i in range(NCH * 4):
                            pu, pu_r = small.get()
                            MM(pu, Ucur[i][0][:], vb[i][0][:], True, True, [Ucur[i][1], vb[i][1]], [pu_r])
                            CP(u_f[i][0][:], pu, [pu_r], [u_f[i][1]], eng="act")
                            pw, pw_r = small.get()
                            MM(pw, kbd[i][0][:], Ucur[i][0][:], True, True, [kbd[i][1], Ucur[i][1]], [pw_r])
                            CP(wT[i][0][:], pw, [pw_r], [wT[i][1]])
                        for c in range(NCH):
                            cs = csl(c)
                            pws = {}
                            for h in range(4):
                                i = c * 4 + h
                                pws[h] = small.get()
                                MM(pws[h][0], wT[i][0][:], S_b[:, h, :], True, True, [wT[i][1], Sb_r[h]], [pws[h][1]])
                            for h in range(4):
                                i = c * 4 + h
                                TT(vnew[i][0][:], u_f[i][0][:], pws[h][0], ALU.subtract, [u_f[i][1], pws[h][1]], [vnew[i][1]])
                            for h in range(4):
                                i = c * 4 + h
                                po, po_r = small.get()
                                MM(po, S_b[:, h, :], qd[i][0][:], True, False, [Sb_r[h], qd[i][1]], [po_r])
                                MM(po, vnew[i][0][:], attnT[i][0][:], False, True, [vnew[i][1], attnT[i][1]], [po_r])
                                CP(oT[:, h, cs], po, [po_r], [oT_r[h]], eng="act")
                                pS, pS_r = small.get()
                                MM(pS, kdec[i][0][:], vnew[i][0][:], True, True, [kdec[i][1], vnew[i][1]], [pS_r])
                                STT(S_f[:, h, :], S_f[:, h, :], eGl[:, col(c, h)], pS, ALU.mult, ALU.add,
                                    [S_r[h], sm2_r, pS_r], [S_r[h]])
                                CP(S_b[:, h, :], S_f[:, h, :], [S_r[h]], [Sb_r[h]], eng="act")
                        P.mute = CFG.get('upto', 99) < 8
                        for h in range(4):
                            sq, sq_r = sq_ring.get()
                            ACT(sq[:], oT[:, h, :], AF.Square, [oT_r[h]], [sq_r])
                            ps, ps_r = wide.get()
                            MM(ps, ones_b, sq[:], True, True, [sq_r, r_const], [ps_r])
                            tm, tm_r = tmp_ring.get()
                            ACT(tm[:], ps, AF.Ln, [ps_r], [tm_r], scale=1.0 / 128, bias=EPS)
                            ACT(tm[:], tm[:], AF.Exp, [tm_r], [tm_r], scale=-0.5)
                            TT(tm[:], tm[:], oT[:, h, :], ALU.mult, [tm_r, oT_r[h]], [tm_r])
                            STT(mixT[:, h, :], tm[:], dnw[:, l:l + 1], zs[:, h, :], ALU.mult, ALU.mult,
                                [tm_r, r_const, zs_r[h]], [mix_r[h]])
                        P.mute = CFG.get('upto', 99) < 0
                        for oc in range(KC):
                            ps, ps_r = wide.get()
                            for k in range(KC):
                                MM(ps, w_out_b[:, k, oc * 128:(oc + 1) * 128], mixT[:, k, :], k == 0, k == KC - 1,
                                   [r_w, mix_r[k]], [ps_r])
                            g1 = mod[:, seq, li * 48 + 16 + oc: li * 48 + 17 + oc]
                            STT(xt[:, oc, :], ps, g1, xt[:, oc, :], ALU.mult, ALU.add, [ps_r, xt_r[oc]], [xt_r[oc]])
                        DMA("sp", Xs[:, tok0:tok0 + TTK].rearrange("(k p) t -> p k t", p=128), xt[:], R=xt_r)
                first[0] = False
                P.flush()

        def phase_B(li, l):
            moe = (l % 2 == 1)
            j = l // 2
            FF = FF_MOE if moe else FF_DENSE
            nexp = NE if moe else 1
            TB = min(1024, L)
            NT = TB // 512
            src = cur_src()
            with contextlib.ExitStack() as ph:
                def pb(name, shape, dt=F32):
                    return ph.enter_context(nc.sbuf_tensor(uname(name), list(shape), dt))
                pbank = Ring([(banks[i], bank_r[i]) for i in range(8)])
                h2b = pb("h2b", [128, KC, TB], BF16); h2b_r = [[Res() for _ in range(KC)] for _ in range(NT)]
                yacc = pb("yacc", [128, KC, TB]); y_r = [[Res() for _ in range(KC)] for _ in range(NT)]
                xt2 = [pb("xtb%d" % i, [128, KC, 512]) for i in range(2)]
                xt2_r = [[Res() for _ in range(KC)] for _ in range(2)]
                sq_ring = Ring([(pb("sqb%d" % i, [128, 512], BF16), Res()) for i in range(3)])
                tmp_ring = Ring([(pb("tmb%d" % i, [128, 512]), Res()) for i in range(3)])
                sg_ring = Ring([(pb("sg%d" % i, [128, 512]), Res()) for i in range(3)])
                hid = [[(pb("hid%d_%d" % (i, f), [128, 512], BF16), Res()) for f in range(4)] for i in range(2)]
                rstd = pb("rstdb", [128, 512]); rstd_r = Res()
                wg = [pb("wg%d" % i, [128, KC, 512], BF16) for i in range(2)]
                wu = [pb("wu%d" % i, [128, KC, 512], BF16) for i in range(2)]
                wd = [pb("wd%d" % i, [128, 4, D], BF16) for i in range(2)]
                w_r = [Res(), Res()]
                wd_r = [Res(), Res()]
                if moe:
                    h2f = pb("h2f", [128, KC, 512]); h2f_r = [Res() for _ in range(KC)]
                    wr = pb("wr", [128, KC, NE]); wr_r = Res()
                    DMA("sp", wr[:], rt_d[j].rearrange("(k p) e -> p k e", p=128), W=[wr_r])
                    sel = pb("sel", [8, 1024]); sel_r = Res()
                    DMA("sp", sel[:], sel_d, W=[sel_r])
                    comb = pb("comb", [128, TB // 128, NE]); comb_r = Res()
                    combT = pb("combT", [8, TB]); combT_r = Res()
                    cbc = [pb("cbc%d" % i, [128, TB]) for i in range(2)]
                    cbc_r = [[Res() for _ in range(NT)] for _ in range(2)]
                    lg = pb("lg", [128, NE]); lg2 = pb("lg2", [128, NE]); mk1 = pb("mk1", [128, NE]); mk2 = pb("mk2", [128, NE])
                    m12 = pb("m12", [128, 4]); lg_r = Res()
                A2 = AB[:, :, (li * 2 + 1) * 8:(li * 2 + 2) * 8]
                pieces = []
                for e in range(nexp):
                    f0 = 0
                    while f0 < FF:
                        pw_ = min(512, FF - f0)
                        pieces.append((e, f0, pw_))
                        f0 += pw_

                def load_piece(pi, which="gud"):
                    e, f0, pw_ = pieces[pi]
                    bi = pi % 2
                    if moe:
                        g_src, u_src, d_src = mg_d[j, e], mu_d[j, e], md_d[j, e]
                    else:
                        g_src, u_src, d_src = fg_d[j], fu_d[j], fd_d[j]
                    if "g" in which:
                        DMA("pool", wg[bi][:, :, 0:pw_], g_src[:, f0:f0 + pw_].rearrange("(k p) n -> p k n", p=128), W=[w_r[bi]])
                        DMA("pool", wu[bi][:, :, 0:pw_], u_src[:, f0:f0 + pw_].rearrange("(k p) n -> p k n", p=128), W=[w_r[bi]])
                    if "d" in which:
                        DMA("pool", wd[bi][:, 0:pw_ // 128, :], d_src[f0:f0 + pw_, :].rearrange("(f p) n -> p f n", p=128), W=[wd_r[bi]])

                xload = [0]
                for blk in range(T // TB):
                    seq = (blk * TB) // L
                    btok = blk * TB
                    load_piece(0)
                    for t in range(NT):
                        xb = xload[0] % 2; xload[0] += 1
                        xt, xt_r = xt2[xb], xt2_r[xb]
                        tok0 = btok + t * 512
                        DMA("sp", xt[:], src[:, tok0:tok0 + 512].rearrange("(k p) t -> p k t", p=128), W=xt_r)
                        ps, ps_r = pbank.get()
                        class _V:
                            def __getitem__(self, idx):
                                p_, k_, c_ = idx
                                return h2b[p_, k_, t * 512 + (c_.start or 0): t * 512 + (c_.stop or 512)]
                        norm_mod(xt, xt_r, 512, A2[:, seq, :], mod[:, seq, li * 48 + 24: li * 48 + 32], _V(), h2b_r[t],
                                 sq_ring, tmp_ring, rstd, rstd_r, ps, ps_r,
                                 out_f=(h2f if moe else None), out_f_r=(h2f_r if moe else None))
                        if moe:
                            P.mute = CFG.get('moe_upto', 99) < 1
                            for c in range(4):
                                gc = t * 4 + c
                                pl, pl_r = pbank.get()
                                for k in range(KC):
                                    MM(pl[:, 0:NE], h2f[:, k, c * 128:(c + 1) * 128], wr[:, k, :], k == 0, k == KC - 1,
                                       [h2f_r[k], wr_r], [pl_r])
                                CP(lg[:], pl[:, 0:NE], [pl_r], [lg_r])
                                P.op("dve", lambda e: e.reduce_max(m12[:, 0:1], lg[:], AX.X), [lg_r], [lg_r])
                                TS(mk1[:], lg[:], m12[:, 0:1], None, ALU.is_equal, None, [lg_r], [lg_r])
                                STT(lg2[:], mk1[:], -1e30, lg[:], ALU.mult, ALU.add, [lg_r], [lg_r])
                                P.op("dve", lambda e: e.reduce_max(m12[:, 1:2], lg2[:], AX.X), [lg_r], [lg_r])
                                TS(mk2[:], lg2[:], m12[:, 1:2], None, ALU.is_equal, None, [lg_r], [lg_r])
                                TT(m12[:, 2:3], m12[:, 1:2], m12[:, 0:1], ALU.subtract, [lg_r], [lg_r])
                                ACT(m12[:, 2:3], m12[:, 2:3], AF.Exp, [lg_r], [lg_r])
                                TS(m12[:, 2:3], m12[:, 2:3], 1.0, None, ALU.add, None, [lg_r], [lg_r])
                                RECIP(m12[:, 2:3], m12[:, 2:3], [lg_r], [lg_r])
                                TS(m12[:, 3:4], m12[:, 2:3], -1.0, 1.0, ALU.mult, ALU.add, [lg_r], [lg_r])
                                TS(mk1[:], mk1[:], m12[:, 2:3], None, ALU.mult, None, [lg_r], [lg_r])
                                STT(comb[:, gc, :], mk2[:], m12[:, 3:4], mk1[:], ALU.mult, ALU.add, [lg_r], [comb_r])
                                P.mute = CFG.get('moe_upto', 99) < 2
                                pc, pc_r = pbank.get()
                                MM(pc[0:8, 0:128], comb[:, gc, :], ident_f, True, True, [comb_r, r_const], [pc_r])
                                CP(combT[:, gc * 128:(gc + 1) * 128], pc[0:8, 0:128], [pc_r], [combT_r])
                    P.mute = False
                    P.mute = False
                    units = [(pi, t) for pi in range(len(pieces)) for t in range(NT)]
                    if len(pieces) > 1:
                        load_piece(1)

                    def unit_G(u):
                        pi, t = units[u]
                        e, f0, pw_ = pieces[pi]
                        bi = pi % 2
                        nf = pw_ // 128
                        if moe and f0 == 0 and t == 0:
                            for t2 in range(NT):
                                pc, pc_r = pbank.get()
                                MM(pc[:], sel[0:8, e * 128:(e + 1) * 128], combT[:, t2 * 512:(t2 + 1) * 512], True, True,
                                   [sel_r, combT_r], [pc_r])
                                CP(cbc[e % 2][:, t2 * 512:(t2 + 1) * 512], pc[:], [pc_r], [cbc_r[e % 2][t2]], eng="act")
                        ts_ = slice(t * 512, (t + 1) * 512)
                        hb_ = hid[u % 2]
                        for f in range(nf):
                            pg, pg_r = pbank.get()
                            for k in range(KC):
                                MM(pg[:], wg[bi][:, k, f * 128:(f + 1) * 128], h2b[:, k, ts_], k == 0, k == KC - 1,
                                   [w_r[bi], h2b_r[t][k]], [pg_r])
                            pu, pu_r = pbank.get()
                            for k in range(KC):
                                MM(pu[:], wu[bi][:, k, f * 128:(f + 1) * 128], h2b[:, k, ts_], k == 0, k == KC - 1,
                                   [w_r[bi], h2b_r[t][k]], [pu_r])
                            sg, sg_r = sg_ring.get()
                            ACT(sg[:], pg[:], AF.Silu, [pg_r], [sg_r])
                            if moe:
                                TT(sg[:], sg[:], cbc[e % 2][:, ts_], ALU.mult, [sg_r, cbc_r[e % 2][t]], [sg_r])
                            TT(hb_[f][0][:], pu[:], sg[:], ALU.mult, [pu_r, sg_r], [hb_[f][1]])
                        if t == NT - 1 and pi + 2 < len(pieces):
                            load_piece(pi + 2, "g")

                    def unit_D(u):
                        pi, t = units[u]
                        e, f0, pw_ = pieces[pi]
                        bi = pi % 2
                        nf = pw_ // 128
                        ts_ = slice(t * 512, (t + 1) * 512)
                        hb_ = hid[u % 2]
                        for oc in range(KC):
                            py, py_r = pbank.get()
                            for f in range(nf):
                                MM(py[:], wd[bi][:, f, oc * 128:(oc + 1) * 128], hb_[f][0][:], f == 0, f == nf - 1,
                                   [wd_r[bi], hb_[f][1]], [py_r])
                            if pi == 0:
                                CP(yacc[:, oc, ts_], py[:], [py_r], [y_r[t][oc]], eng="act")
                            else:
                                TT(yacc[:, oc, ts_], yacc[:, oc, ts_], py[:], ALU.add, [y_r[t][oc], py_r], [y_r[t][oc]])
                        if t == NT - 1 and pi + 2 < len(pieces):
                            load_piece(pi + 2, "d")

                    unit_G(0)
                    for u in range(len(units)):
                        if u + 1 < len(units):
                            unit_G(u + 1)
                        unit_D(u)
                    for t in range(NT):
                        xb = xload[0] % 2; xload[0] += 1
                        xt, xt_r = xt2[xb], xt2_r[xb]
                        tok0 = btok + t * 512
                        DMA("sp", xt[:], src[:, tok0:tok0 + 512].rearrange("(k p) t -> p k t", p=128), W=xt_r)
                        for oc in range(KC):
                            g2 = mod[:, seq, li * 48 + 40 + oc: li * 48 + 41 + oc]
                            STT(xt[:, oc, :], yacc[:, oc, t * 512:(t + 1) * 512], g2, xt[:, oc, :], ALU.mult, ALU.add,
                                [y_r[t][oc], xt_r[oc]], [xt_r[oc]])
                        DMA("sp", Xs[:, tok0:tok0 + 512].rearrange("(k p) t -> p k t", p=128), xt[:], R=xt_r)
                first[0] = False
                P.flush()

        def phase_final():
            src = cur_src()
            with contextlib.ExitStack() as ph:
                def pb(name, shape, dt=F32):
                    return ph.enter_context(nc.sbuf_tensor(uname(name), list(shape), dt))
                pbank = Ring([(banks[i], bank_r[i]) for i in range(8)])
                xt2 = [pb("xtf%d" % i, [128, KC, 512]) for i in range(2)]
                xt2_r = [[Res() for _ in range(KC)] for _ in range(2)]
                ot2 = [pb("otf%d" % i, [128, KC, 512]) for i in range(2)]
                ot2_r = [[Res() for _ in range(KC)] for _ in range(2)]
                sq_ring = Ring([(pb("sqf%d" % i, [128, 512], BF16), Res()) for i in range(3)])
                tmp_ring = Ring([(pb("tmf%d" % i, [128, 512]), Res()) for i in range(3)])
                rstd = pb("rstdf", [128, 512]); rstd_r = Res()
                Af = AB[:, :, 64:72]
                for t in range(T // 512):
                    seq = (t * 512) // L
                    xt, xt_r = xt2[t % 2], xt2_r[t % 2]
                    ot, ot_r = ot2[t % 2], ot2_r[t % 2]
                    tok0 = t * 512
                    DMA("sp", xt[:], src[:, tok0:tok0 + 512].rearrange("(k p) t -> p k t", p=128), W=xt_r)
                    ps, ps_r = pbank.get()
                    for k in range(KC):
                        sq, sq_r = sq_ring.get()
                        ACT(sq[:], xt[:, k, :], AF.Square, [xt_r[k]], [sq_r])
                        MM(ps[:], ones_b, sq[:], k == 0, k == KC - 1, [sq_r, r_const], [ps_r])
                    ACT(rstd[:], ps[:], AF.Ln, [ps_r], [rstd_r], scale=1.0, bias=1024.0 * EPS)
                    ACT(rstd[:], rstd[:], AF.Exp, [rstd_r], [rstd_r], scale=-0.5)
                    for k in range(KC):
                        tm, tm_r = tmp_ring.get()
                        TT(tm[:], xt[:, k, :], rstd[:], ALU.mult, [xt_r[k], rstd_r], [tm_r])
                        ACT(ot[:, k, :], tm[:], AF.Identity, [tm_r], [ot_r[k]],
                            scale=Af[:, seq, k:k + 1], bias=mod[:, seq, 192 + k:193 + k])
                    DMA("sp", outT[:, tok0:tok0 + 512].rearrange("(k p) t -> p k t", p=128), ot[:], R=ot_r)
                P.flush()

        for li, l in enumerate(layers):
            phase_A(li, l)
            if CFG.get('upto', 99) >= 9:
                phase_B(li, l)
        phase_final()
    return nc


_PROG_CACHE = {}


def kernel(x, c, ada_w, ada_b, norm1_w, norm2_w, w_in, conv_w, a_log, dt_bias,
           dn_norm_w, gm_ln_w, gm_ln_b, gm_spatial_w, gm_spatial_b, w_out,
           ffn_w_gate, ffn_w_up, ffn_w_down, moe_router, moe_w_gate, moe_w_up,
           moe_w_down, final_ada_w, final_ada_b, final_norm_w):
    f = lambda a: np.ascontiguousarray(np.asarray(a, dtype=np.float32))
    x = f(x); c = f(c)
    B, L, _ = x.shape
    n_cores = CFG["n_cores"]
    NB = B // n_cores
    layers = list(CFG["layers"])
    key = (L, NB, tuple(layers))
    if key not in _PROG_CACHE:
        import time as _t, sys as _s
        _t0 = _t.time()
        _PROG_CACHE[key] = build(L, NB, layers)
        print("[kernel] build %.1fs" % (_t.time() - _t0), file=_s.stderr)
    nc = _PROG_CACHE[key]
    cons, sel = _consts()

    def pm(v, nchunk):
        v = f(v)
        lead = v.shape[:-1]
        return np.ascontiguousarray(np.moveaxis(v.reshape(*lead, nchunk, 128), -1, 0))

    ada_bT = pm(ada_b, 48).reshape(128, 4 * 48)
    fada_bT = pm(final_ada_b, 16).reshape(128, 16)
    nw = np.concatenate([np.stack([pm(norm1_w, 8), pm(norm2_w, 8)], axis=2).reshape(128, 64),
                         pm(final_norm_w, 8).reshape(128, 8)], axis=1)
    cwp = np.ascontiguousarray(np.transpose(pm(conv_w, 12), (0, 1, 3, 2))).reshape(128, 4 * 48)
    dnw = np.ascontiguousarray(f(dn_norm_w).T)
    alog_bc = np.ascontiguousarray(np.broadcast_to(f(a_log).reshape(1, 16), (128, 16)))
    dtb_bc = np.ascontiguousarray(np.broadcast_to(f(dt_bias).reshape(1, 16), (128, 16)))
    wsT = np.ascontiguousarray(np.transpose(f(gm_spatial_w), (0, 3, 1, 2))).reshape(4, 128, 512)
    gsb = f(gm_spatial_b).reshape(4, 512)
    shared = {
        "consts": cons.reshape(128, NCONST * 128), "sel": sel,
        "ada_w": f(ada_w), "ada_bT": ada_bT, "final_ada_w": f(final_ada_w), "fada_bT": fada_bT, "nw": nw,
        "w_in": f(w_in), "cw": cwp, "dnw": dnw, "alog_bc": alog_bc, "dtb_bc": dtb_bc,
        "gm_ln_w": f(gm_ln_w), "gm_ln_b": f(gm_ln_b), "wsT": wsT, "gsb": gsb, "w_out": f(w_out),
        "ffn_w_gate": f(ffn_w_gate), "ffn_w_up": f(ffn_w_up), "ffn_w_down": f(ffn_w_down),
        "moe_router": f(moe_router), "moe_w_gate": f(moe_w_gate), "moe_w_up": f(moe_w_up), "moe_w_down": f(moe_w_down),
    }
    if not any(l % 2 == 1 for l in layers):
        for k_ in ("moe_w_gate", "moe_w_up", "moe_w_down"):
            shared[k_] = np.zeros((1, 1, 1, 1), np.float32)
    in_maps = []
    for i in range(n_cores):
        xs = x[i * NB:(i + 1) * NB].reshape(NB * L, D)
        cs = c[i * NB:(i + 1) * NB]
        cTl = np.ascontiguousarray(np.transpose(cs.reshape(NB, KC, 128), (2, 1, 0))).reshape(128, KC * NB)
        m = dict(shared)
        m["xT"] = np.ascontiguousarray(xs.T)
        m["cT"] = cTl
        in_maps.append(m)
    import time as _t, sys as _s
    _t0 = _t.time()
    if CFG.get("sim"):
        return nc, in_maps
    res = run_bass_kernel_spmd(nc, in_maps, core_ids=list(range(n_cores)))
    print("[kernel] launch+transfer %.1fs" % (_t.time() - _t0), file=_s.stderr)
    out = np.empty((B, L, D), np.float32)
    for i in range(n_cores):
        out[i * NB:(i + 1) * NB] = res.results[i]["outT"].T.reshape(NB, L, D)
    return out
```
